# Optimizing a Trainium2 kernel written in Bass

```python
import jax, jax.numpy as jnp
from jax import lax
import numpy as np

D_MODEL = 1024
BATCH = 2
SEQ = 16384
DEPTH = 1

ATT_HEADS = 8
ATT_KV_HEADS = 2
ATT_HEAD_DIM = 64
WINDOW = 128
ATT_BLOCK = 128
ROPE_THETA = 10000.0
MLSTM_HEADS = 4
MLSTM_HEAD_DIM = 128
MLSTM_CHUNK = 128
CONV_WIDTH = 4
ATT_Q_W = ATT_HEADS * ATT_HEAD_DIM
ATT_KV_W = ATT_KV_HEADS * ATT_HEAD_DIM
MLSTM_W = MLSTM_HEADS * MLSTM_HEAD_DIM
N_BRANCHES = 2
IN_SPLITS = (ATT_Q_W, ATT_KV_W, ATT_KV_W, MLSTM_W, MLSTM_W, MLSTM_W, MLSTM_W,
             MLSTM_HEADS, MLSTM_HEADS, D_MODEL, D_MODEL)
IN_PROJ_W = sum(IN_SPLITS)
PEER_HEADS = 8
PEER_N_KEYS = 128
PEER_N_EXPERTS = PEER_N_KEYS * PEER_N_KEYS
PEER_QUERY_DIM = 256
PEER_HALF = PEER_QUERY_DIM // 2
PEER_TOPK = 16
PEER_BLOCK = 128
EPS = 1e-6

kernel_name = 'hybrid_swa_mlstm_peer_adaln'

F32 = jnp.float32


def rmsnorm(x, w):
    xf = x.astype(F32)
    y = xf * lax.rsqrt(jnp.mean(xf * xf, axis=-1, keepdims=True) + EPS)
    return (y * w.astype(F32)).astype(x.dtype)


def split_cols(t, sizes):
    offs, acc = [], 0
    for s_ in sizes[:-1]:
        acc += s_
        offs.append(acc)
    return jnp.split(t, offs, axis=-1)


def rope(x):
    s, hd = x.shape[1], x.shape[-1]
    half = hd // 2
    inv_freq = ROPE_THETA ** (-jnp.arange(half, dtype=F32) * 2.0 / hd)
    ang = jnp.arange(s, dtype=F32)[:, None] * inv_freq[None, :]
    cos, sin = jnp.cos(ang)[:, None, :], jnp.sin(ang)[:, None, :]
    xf = x.astype(F32)
    x1, x2 = xf[..., :half], xf[..., half:]
    return jnp.concatenate([x1 * cos - x2 * sin, x2 * cos + x1 * sin], axis=-1)


def sliding_window_attention(q, k, v, sinks):
    b, s, _, hd = q.shape
    nb = s // ATT_BLOCK
    g = ATT_HEADS // ATT_KV_HEADS
    qb = q.astype(F32).reshape(b, nb, ATT_BLOCK, ATT_KV_HEADS, g, hd)
    kb = k.astype(F32).reshape(b, nb, ATT_BLOCK, ATT_KV_HEADS, hd)
    vb = v.astype(F32).reshape(b, nb, ATT_BLOCK, ATT_KV_HEADS, hd)

    def with_prev(t):
        prev = jnp.pad(t, ((0, 0), (1, 0), (0, 0), (0, 0), (0, 0)))[:, :-1]
        return jnp.concatenate([prev, t], axis=2)

    kk, vv = with_prev(kb), with_prev(vb)
    scores = jnp.einsum('bnqhgd,bnkhd->bnhgqk', qb, kk) * (hd ** -0.5)
    qi = jnp.arange(ATT_BLOCK)[:, None]
    ki = jnp.arange(2 * ATT_BLOCK)[None, :]
    rel = qi + ATT_BLOCK - ki
    key_pos = jnp.arange(nb)[:, None, None] * ATT_BLOCK - ATT_BLOCK + ki[None]
    mask = (rel >= 0) & (rel < WINDOW) & (key_pos >= 0)
    scores = jnp.where(mask[None, :, None, None], scores, -jnp.inf)
    sink = sinks.astype(F32).reshape(1, 1, ATT_KV_HEADS, g, 1)
    m = jnp.maximum(scores.max(-1), sink)
    p = jnp.exp(scores - m[..., None])
    denom = p.sum(-1) + jnp.exp(sink - m)
    p = p / denom[..., None]
    out = jnp.einsum('bnhgqk,bnkhd->bnqhgd', p, vv)
    return out.reshape(b, s, ATT_HEADS * hd)


def causal_depthwise_conv(x, w):
    ch = x.shape[-1]
    return lax.conv_general_dilated(
        x, w[:, None, :].astype(x.dtype), window_strides=(1,),
        padding=[(CONV_WIDTH - 1, 0)], dimension_numbers=('NWC', 'WIO', 'NWC'),
        feature_group_count=ch)


def mlstm_chunkwise(q, k, v, i_pre, f_pre):
    b, s, h, d = q.shape
    L = MLSTM_CHUNK
    nc = s // L

    def to_chunks(t):
        t = t.astype(F32).reshape((b, nc, L, h) + t.shape[3:])
        return jnp.moveaxis(t, 3, 1)

    qc, kc, vc = to_chunks(q), to_chunks(k) * (d ** -0.5), to_chunks(v)
    ic = to_chunks(i_pre)
    lf = jax.nn.log_sigmoid(to_chunks(f_pre))
    bcum = jnp.cumsum(lf, axis=-1)
    g_tot = bcum[..., -1]
    a = g_tot[..., None] - bcum + ic

    def step(carry, xs):
        C, n, m = carry
        k_c, v_c, a_c, g_c = xs
        m_new = jnp.maximum(g_c + m, a_c.max(-1))
        decay = jnp.exp(g_c + m - m_new)
        wk = jnp.exp(a_c - m_new[..., None])[..., None] * k_c
        C_new = decay[..., None, None] * C + jnp.einsum('bhld,bhle->bhde', wk, v_c)
        n_new = decay[..., None] * n + wk.sum(axis=2)
        return (C_new, n_new, m_new), (C, n, m)

    init = (jnp.zeros((b, h, d, d), F32), jnp.zeros((b, h, d), F32), jnp.zeros((b, h), F32))
    xs = (jnp.moveaxis(kc, 2, 0), jnp.moveaxis(vc, 2, 0),
          jnp.moveaxis(a, 2, 0), jnp.moveaxis(g_tot, 2, 0))
    _, (C_st, n_st, m_st) = lax.scan(step, init, xs)
    C_st = jnp.moveaxis(C_st, 0, 2)
    n_st = jnp.moveaxis(n_st, 0, 2)
    m_st = jnp.moveaxis(m_st, 0, 2)

    causal = jnp.tril(jnp.ones((L, L), dtype=bool))
    D = bcum[..., :, None] - bcum[..., None, :] + ic[..., None, :]
    D = jnp.where(causal, D, -jnp.inf)
    m_inter = bcum + m_st[..., None]
    m_out = jnp.maximum(m_inter, D.max(-1))
    w_intra = jnp.exp(D - m_out[..., None]) * jnp.einsum('bhcld,bhcsd->bhcls', qc, kc)
    w_inter = jnp.exp(m_inter - m_out)
    num = (jnp.einsum('bhcls,bhcsd->bhcld', w_intra, vc)
           + w_inter[..., None] * jnp.einsum('bhcld,bhcde->bhcle', qc, C_st))
    den = w_intra.sum(-1) + w_inter * jnp.einsum('bhcld,bhcd->bhcl', qc, n_st)
    hid = num / jnp.maximum(jnp.abs(den), jnp.exp(-m_out))[..., None]
    return jnp.moveaxis(hid, 1, 3).reshape(b, s, h, d)


def token_mixer(u, w_in, conv_w, att_sinks, i_bias, f_bias, mlstm_norm_w,
                w_att_branch, w_mlstm_branch, w_out):
    b, s, _ = u.shape
    proj = u @ w_in
    aq, ak, av, mq, mk, mv, mo, mi, mf, ga, gm = split_cols(proj, IN_SPLITS)
    aq = rope(aq.reshape(b, s, ATT_HEADS, ATT_HEAD_DIM))
    ak = rope(ak.reshape(b, s, ATT_KV_HEADS, ATT_HEAD_DIM))
    av = av.reshape(b, s, ATT_KV_HEADS, ATT_HEAD_DIM)
    att = sliding_window_attention(aq, ak, av, att_sinks).astype(u.dtype)
    mqk = jax.nn.silu(causal_depthwise_conv(jnp.concatenate([mq, mk], axis=-1), conv_w))
    mq, mk = jnp.split(mqk, 2, axis=-1)
    shp = (b, s, MLSTM_HEADS, MLSTM_HEAD_DIM)
    hid = mlstm_chunkwise(mq.reshape(shp), mk.reshape(shp), mv.reshape(shp),
                          mi.astype(F32) + i_bias.astype(F32), mf.astype(F32) + f_bias.astype(F32))
    hid = jax.nn.sigmoid(mo.astype(F32)).reshape(shp) * hid
    hid = hid * lax.rsqrt(jnp.mean(hid * hid, axis=-1, keepdims=True) + EPS)
    mls = (hid.reshape(b, s, MLSTM_W) * mlstm_norm_w.astype(F32)).astype(u.dtype)
    merged = jax.nn.sigmoid(ga) * (att @ w_att_branch) + jax.nn.sigmoid(gm) * (mls @ w_mlstm_branch)
    return merged @ w_out


def peer_ffn(u, w_query, sub_keys, exp_u, exp_v):
    b, s, d = u.shape
    tokens = u.reshape(-1, PEER_BLOCK, d)

    def block(xb):
        t = xb.shape[0]
        qy = (xb @ w_query).reshape(t, PEER_HEADS, 2, PEER_HALF).astype(F32)
        scores = jnp.einsum('thpd,pnd->thpn', qy, sub_keys.astype(F32))
        top_s, top_i = lax.top_k(scores, PEER_TOPK)
        cand_s = top_s[:, :, 0, :, None] + top_s[:, :, 1, None, :]
        cand_i = top_i[:, :, 0, :, None] * PEER_N_KEYS + top_i[:, :, 1, None, :]
        kk = PEER_TOPK * PEER_TOPK
        best_s, best_j = lax.top_k(cand_s.reshape(t, PEER_HEADS, kk), PEER_TOPK)
        idx = jnp.take_along_axis(cand_i.reshape(t, PEER_HEADS, kk), best_j, axis=-1)
        gate = jax.nn.softmax(best_s, axis=-1)
        u_sel = jnp.take(exp_u, idx, axis=0)
        v_sel = jnp.take(exp_v, idx, axis=0)
        act = jax.nn.gelu(jnp.einsum('td,thkd->thk', xb, u_sel).astype(F32), approximate=False)
        return jnp.einsum('thk,thkd->td', (gate * act).astype(xb.dtype), v_sel)

    return lax.map(block, tokens).reshape(b, s, d)


def setup_inputs(seed: int = 0) -> dict:
    key = jax.random.key(seed)
    ks = jax.random.split(key, 24)

    def nrm(k, shape, scale):
        return jax.random.normal(k, shape, F32) * scale

    return {
        'x': nrm(ks[0], (BATCH, SEQ, D_MODEL), 1.0),
        'c': nrm(ks[1], (BATCH, D_MODEL), 1.0),
        'w_ada': nrm(ks[2], (DEPTH, D_MODEL, 6 * D_MODEL), 0.5 * D_MODEL ** -0.5),
        'b_ada': nrm(ks[3], (DEPTH, 6 * D_MODEL), 0.02),
        'norm1_w': 1.0 + nrm(ks[4], (DEPTH, D_MODEL), 0.02),
        'w_in': nrm(ks[5], (DEPTH, D_MODEL, IN_PROJ_W), D_MODEL ** -0.5),
        'conv_w': nrm(ks[6], (DEPTH, CONV_WIDTH, 2 * MLSTM_W), CONV_WIDTH ** -0.5),
        'att_sinks': nrm(ks[7], (DEPTH, ATT_HEADS), 1.0),
        'i_bias': nrm(ks[8], (DEPTH, MLSTM_HEADS), 0.1),
        'f_bias': jnp.linspace(3.0, 6.0, MLSTM_HEADS, dtype=F32)[None, :]
                  + nrm(ks[9], (DEPTH, MLSTM_HEADS), 0.1),
        'mlstm_norm_w': 1.0 + nrm(ks[10], (DEPTH, MLSTM_W), 0.02),
        'w_att_branch': nrm(ks[11], (DEPTH, ATT_Q_W, D_MODEL), ATT_Q_W ** -0.5),
        'w_mlstm_branch': nrm(ks[12], (DEPTH, MLSTM_W, D_MODEL), MLSTM_W ** -0.5),
        'w_out': nrm(ks[13], (DEPTH, D_MODEL, D_MODEL), D_MODEL ** -0.5),
        'norm2_w': 1.0 + nrm(ks[14], (DEPTH, D_MODEL), 0.02),
        'peer_w_query': nrm(ks[15], (DEPTH, D_MODEL, PEER_HEADS * PEER_QUERY_DIM), D_MODEL ** -0.5),
        'peer_sub_keys': nrm(ks[16], (DEPTH, 2, PEER_N_KEYS, PEER_HALF), PEER_HALF ** -0.5),
        'peer_u': nrm(ks[17], (DEPTH, PEER_N_EXPERTS, D_MODEL), D_MODEL ** -0.5),
        'peer_v': nrm(ks[18], (DEPTH, PEER_N_EXPERTS, D_MODEL), PEER_HEADS ** -0.5),
        'norm_f_w': 1.0 + nrm(ks[19], (D_MODEL,), 0.02),
    }


def reference(x, c, w_ada, b_ada, norm1_w, w_in, conv_w, att_sinks, i_bias, f_bias,
              mlstm_norm_w, w_att_branch, w_mlstm_branch, w_out, norm2_w,
              peer_w_query, peer_sub_keys, peer_u, peer_v, norm_f_w):
    h = x
    c_act = jax.nn.silu(c)
    for l in range(DEPTH):
        mod = (c_act @ w_ada[l] + b_ada[l])[:, None, :]
        sh1, sc1, g1, sh2, sc2, g2 = jnp.split(mod, 6, axis=-1)
        u = rmsnorm(h, norm1_w[l]) * (1 + sc1) + sh1
        h = h + g1 * token_mixer(u, w_in[l], conv_w[l], att_sinks[l], i_bias[l], f_bias[l],
                                 mlstm_norm_w[l], w_att_branch[l], w_mlstm_branch[l], w_out[l])
        u2 = rmsnorm(h, norm2_w[l]) * (1 + sc2) + sh2
        h = h + g2 * peer_ffn(u2, peer_w_query[l], peer_sub_keys[l], peer_u[l], peer_v[l])
    return rmsnorm(h, norm_f_w)
```

```python
import numpy as np
from contextlib import ExitStack
import concourse.bass as bass
import concourse.mybir as mybir
from concourse.bass_utils import run_bass_kernel_spmd

F32 = mybir.dt.float32
BF16 = mybir.dt.bfloat16
I32 = mybir.dt.int32
U32 = mybir.dt.uint32
AF = mybir.ActivationFunctionType
ALU = mybir.AluOpType
AX = mybir.AxisListType

D = 1024
NPROJ = 4872
EPS = 1e-6
NEG = -30000.0


class Buf:
    def __init__(self, t, name):
        self.t = t
        self.name = name
        self.w = None
        self.r = []
        self.dsem = None
        self.dcnt = 0

    def __getitem__(self, k):
        return self.t[k]


class Eng:
    def __init__(self, e, sem, name, is_pe=False):
        self.e, self.sem, self.name, self.cnt, self.is_pe = e, sem, name, 0, is_pe
        self.seen = {}


class Ctx:
    def __init__(self, nc, es):
        self.nc, self.es = nc, es
        self.engs = {}
        for nm, e in (("pe", nc.tensor), ("dve", nc.vector), ("act", nc.scalar),
                      ("pool", nc.gpsimd), ("sp", nc.sync)):
            sem = es.enter_context(nc.semaphore("s_" + nm))
            self.engs[nm] = Eng(e, sem, nm, is_pe=(nm == "pe"))
        self.bufs = []
        self.nsem = 0

    def sb(self, name, shape, dt, es=None):
        t = (es or self.es).enter_context(self.nc.sbuf_tensor("sb_" + name, shape, dt))
        b = Buf(t, name); self.bufs.append(b); return b

    def ps(self, name, shape, dt=F32, es=None):
        t = (es or self.es).enter_context(self.nc.psum_tensor("ps_" + name, shape, dt))
        b = Buf(t, name); self.bufs.append(b); return b

    def dram(self, name, shape, dt, kind="Internal"):
        t = self.nc.dram_tensor(name, shape, dt, kind=kind).ap()
        b = Buf(t, name); self.bufs.append(b); return b

    def view(self, buf, name):
        b = Buf(buf.t, name); self.bufs.append(b); return b

    def _wait(self, eng, deps):
        best = {}
        for (sem, val) in deps:
            k = id(sem)
            if k not in best or best[k][1] < val:
                best[k] = (sem, val)
        for k, (sem, val) in best.items():
            if eng.is_pe and sem is eng.sem:
                continue
            if eng.seen.get(k, 0) >= val:
                continue
            eng.e.wait_ge(sem, val)
            eng.seen[k] = val

    def _deps(self, reads, writes):
        deps = []
        for b in reads:
            if b.w: deps.append(b.w)
        for b in writes:
            if b.w: deps.append(b.w)
            deps.extend(b.r)
        return deps

    def op(self, en, fn, reads=(), writes=()):
        eng = self.engs[en]
        self._wait(eng, self._deps(reads, writes))
        ins = fn(eng.e)
        eng.cnt += 1
        ins.then_inc(eng.sem, 1)
        tok = (eng.sem, eng.cnt)
        for b in writes:
            b.w = tok; b.r = []
        for b in reads:
            if b not in writes:
                b.r.append(tok)

    def dma(self, en, fn, dst, srcs=(), waw=True):
        eng = self.engs[en]
        self._wait(eng, self._deps(srcs, [dst] if waw else []))
        if dst.dsem is None:
            dst.dsem = self.es.enter_context(self.nc.semaphore("d%d" % self.nsem))
            self.nsem += 1
        ins = fn(eng.e)
        dst.dcnt += 1
        ins.then_inc(dst.dsem, 16)
        tok = (dst.dsem, 16 * dst.dcnt)
        dst.w = tok; dst.r = []
        for b in srcs:
            b.r.append(tok)

    def barrier(self):
        toks = []
        for b in self.bufs:
            if b.w: toks.append(b.w)
            toks.extend(b.r)
        for e in self.engs.values():
            if e.cnt: toks.append((e.sem, e.cnt))
        for e in self.engs.values():
            self._wait(e, toks)
        for b in self.bufs:
            b.w = None; b.r = []

    def finish(self, bufs):
        eng = self.engs["sp"]
        self._wait(eng, [b.w for b in bufs if b.w])


def build(NPRE, NOWN, stage=2):
    nc = bass.Bass("TRN2", target_bir_lowering=False)
    es = ExitStack()
    with es:
        K = Ctx(nc, es)

        def din(name, shape, dt=F32):
            return K.dram(name, shape, dt, kind="ExternalInput")
        NP1 = max(NPRE, 1)
        xo = din("xo", [NOWN * 128, D]); xp = din("xp", [NP1 * 128, D])
        valid_d = din("valid", [128, NP1])
        mask0_d = din("mask0", [128, 256]); maskN_d = din("maskN", [128, 256])
        cos_d = din("cosd", [(NOWN + 1) * 128, 32]); sin_d = din("sind", [(NOWN + 1) * 128, 32])
        cT_d = din("cT", [128, 8])
        w_ada_d = din("w_ada", [D, 6 * D]); b_ada_d = din("b_ada", [6 * D])
        n1w_d = din("norm1_w", [D]); n2w_d = din("norm2_w", [D]); nfw_d = din("norm_f_w", [D])
        w_in_d = din("w_in", [D, NPROJ])
        cw_d = din("conv_w", [128, 8, 4])
        sink_d = din("att_sinks", [8]); ib_d = din("i_bias", [4, 1]); fb_d = din("f_bias", [4, 1])
        mnw_d = din("mlstm_norm_w", [512])
        wa_d = din("w_att", [512, D]); wb_d = din("w_ml", [512, D]); wo_d = din("w_out", [D, D])
        wq_d = din("wq", [D, 2048]); sk_d = din("subkT", [128, 2, 128])
        pu_d = din("pu", [16384, D]); pv_d = din("pv", [16384, D])
        ident_d = din("ident", [128, 128]); tri_d = din("tri", [128, 128])
        sel_d = din("sel", [4, 4, 128]); i4_d = din("i4", [4, 4]); iota_d = din("iota16", [128, 16])
        y = K.dram("y", [NOWN * 128, D], F32, kind="ExternalOutput")
        modbc_d = K.dram("modbc", [128, 6 * D], F32)
        hbuf_d = K.dram("hbuf", [NOWN * 128, D], F32)
        pub_d = K.dram("pub", [16384, D], BF16)
        pvb_d = K.dram("pvb", [16384, D], BF16)

        ident = K.sb("ident", [128, 128], F32); identb = K.sb("identb", [128, 128], BF16)
        K.dma("sp", lambda e: e.dma_start(out=ident[:], in_=ident_d[:]), ident, [ident_d])
        K.op("dve", lambda e: e.tensor_copy(out=identb[:], in_=ident[:]), [ident], [identb])
        ones = K.sb("ones", [128, 128], F32)
        K.op("dve", lambda e: e.memset(ones[:], 1.0), [], [ones])
        P = [K.ps("P%d" % i, [128, 512], F32) for i in range(8)]

        def bcast_load(dst, src_ap, src_buf):
            K.dma("sp", lambda e: e.dma_start(out=dst[:], in_=src_ap.partition_broadcast(128)), dst, [src_buf])

        with ExitStack() as e0:
            cT = K.sb("cT", [128, 8], F32, e0); cact = K.sb("cact", [128, 8], F32, e0)
            crep = K.sb("crep", [128, 8, 128], BF16, e0)
            K.dma("sp", lambda e: e.dma_start(out=cT[:], in_=cT_d[:]), cT, [cT_d])
            K.op("act", lambda e: e.activation(out=cact[:], in_=cT[:], func=AF.Silu), [cT], [cact])
            K.op("dve", lambda e: e.tensor_copy(out=crep[:], in_=cact[:].unsqueeze(2).to_broadcast([128, 8, 128])),
                 [cact], [crep])
            wst = [K.sb("wst%d" % i, [128, 8, 512], F32, e0) for i in range(2)]
            wsb = [K.sb("wsb%d" % i, [128, 8, 512], BF16, e0) for i in range(2)]
            badab = K.sb("badab", [128, 6 * D], F32, e0)
            bcast_load(badab, b_ada_d[:], b_ada_d)
            modsb = K.sb("modsb", [128, 6 * D], F32, e0)
            wv = w_ada_d.t.rearrange("(kc p) n -> p kc n", p=128)
            for j in range(12):
                ws = wst[j % 2]
                K.dma("sp", lambda e: e.dma_start(out=ws[:], in_=wv[:, :, j * 512:(j + 1) * 512]), ws, [w_ada_d])
                pj = P[j % 2]
                wb_ = wsb[j % 2]
                K.op("act" if j % 2 else "dve", (lambda e: e.activation(out=wb_[:], in_=ws[:], func=AF.Copy)) if j % 2 else
                     (lambda e: e.tensor_copy(out=wb_[:], in_=ws[:])), [ws], [wb_])
                for kc in range(8):
                    K.op("pe", lambda e: e.matmul(pj[:, :], lhsT=crep[:, kc, :], rhs=wb_[:, kc, :],
                                                  start=(kc == 0), stop=(kc == 7)), [crep, wb_], [pj])
                K.op("dve", lambda e: e.tensor_tensor(out=modsb[:, j * 512:(j + 1) * 512], in0=pj[:, :],
                                                      in1=badab[:, j * 512:(j + 1) * 512], op=ALU.add),
                     [pj, badab], [modsb])
            nwb = K.sb("nwb", [128, D], F32, e0)
            for (wd, slot) in ((n1w_d, 1), (n2w_d, 4)):
                bcast_load(nwb, wd[:], wd)
                K.op("dve", lambda e: e.scalar_tensor_tensor(out=modsb[:, slot * D:(slot + 1) * D],
                                                             in0=modsb[:, slot * D:(slot + 1) * D], scalar=1.0,
                                                             in1=nwb[:], op0=ALU.add, op1=ALU.mult),
                     [modsb, nwb], [modsb])
            K.dma("sp", lambda e: e.dma_start(out=modbc_d[:], in_=modsb[:]), modbc_d, [modsb])
        K.barrier()

        def load_bf16(dst, nrow_chunks, ncols, src_view, src_buf, stage):
            i = 0
            for kc in range(nrow_chunks):
                for c0 in range(0, ncols, 2048):
                    c1 = min(ncols, c0 + 2048)
                    st = stage[i % 2]; i += 1
                    pp = dst.t.shape[0]
                    K.dma("sp", lambda e: e.dma_start(out=st[0:pp, 0:c1 - c0], in_=src_view[:, kc, c0:c1]), st, [src_buf])
                    eng = "act" if (i % 2) else "dve"
                    if eng == "act":
                        K.op("act", lambda e: e.activation(out=dst[:, kc, c0:c1], in_=st[0:pp, 0:c1 - c0], func=AF.Copy), [st], [dst])
                    else:
                        K.op("dve", lambda e: e.tensor_copy(out=dst[:, kc, c0:c1], in_=st[0:pp, 0:c1 - c0]), [st], [dst])

        def rmsnorm_mod(xt, ut, A, B, sm):
            junk, ssq, rstd = sm
            K.op("act", lambda e: e.activation(out=junk[:].bitcast(BF16), in_=xt[:], func=AF.Square, accum_out=ssq[:, 0:1]), [xt], [junk, ssq])
            K.op("dve", lambda e: e.tensor_scalar(out=ssq[:, 1:2], in0=ssq[:, 0:1], scalar1=1.0 / D, scalar2=EPS,
                                                  op0=ALU.mult, op1=ALU.add), [ssq], [ssq])
            K.op("act", lambda e: e.activation(out=ssq[:, 2:3], in_=ssq[:, 1:2], func=AF.Ln), [ssq], [ssq])
            K.op("act", lambda e: e.activation(out=rstd[:], in_=ssq[:, 2:3], func=AF.Exp, scale=-0.5), [ssq], [rstd])
            K.op("dve", lambda e: e.scalar_tensor_tensor(out=ut[:], in0=xt[:], scalar=rstd[:, 0:1], in1=A[:],
                                                         op0=ALU.mult, op1=ALU.mult), [xt, rstd, A], [ut])
            if B is not None:
                K.op("pool", lambda e: e.tensor_tensor(out=ut[:], in0=ut[:], in1=B[:], op=ALU.add), [ut, B], [ut])

        def transpose_to(src, dstT, nk, pa, pb, dt32=True, srcbuf=None):
            srcbuf = srcbuf or src
            idn = ident if dt32 else identb
            for half in range((nk + 3) // 4):
                pb_ = (pa, pb)[half % 2]
                n = min(4, nk - half * 4)
                if dt32:
                    pv = pb_[:, :].rearrange("p (a b) -> p a b", b=128)
                else:
                    pv = pb_[:, :].bitcast(BF16).rearrange("p (a b) -> p a b", b=128)
                for q in range(n):
                    kc = half * 4 + q
                    K.op("pe", lambda e: e.transpose(out=pv[:, q, :], in_=src[:, kc * 128:(kc + 1) * 128], identity=idn[:]),
                         [srcbuf, idn], [pb_])
                K.op("act", lambda e: e.activation(out=dstT[:, half * 4:half * 4 + n, :], in_=pv[:, 0:n, :], func=AF.Copy),
                     [pb_], [dstT])

        with ExitStack() as e1:
            w_in = K.sb("w_in", [128, 8, NPROJ], BF16, e1)
            wa = K.sb("wa", [64, 8, D], BF16, e1)
            wb = K.sb("wb", [128, 4, D], BF16, e1)
            wo = K.sb("wo", [128, 8, D], BF16, e1)
            with ExitStack() as est:
                stage = [K.sb("stg%d" % i, [128, 2048], F32, est) for i in range(2)]
                load_bf16(w_in, 8, NPROJ, w_in_d.t.rearrange("(kc p) n -> p kc n", p=128), w_in_d, stage)
                load_bf16(wa, 8, D, wa_d.t.rearrange("(h p) n -> p h n", p=64), wa_d, stage)
                load_bf16(wb, 4, D, wb_d.t.rearrange("(kc p) n -> p kc n", p=128), wb_d, stage)
                load_bf16(wo, 8, D, wo_d.t.rearrange("(kc p) n -> p kc n", p=128), wo_d, stage)
                K.barrier()
            A1 = K.sb("A1", [128, D], F32, e1); B1 = K.sb("B1", [128, D], F32, e1); G1 = K.sb("G1", [128, D], F32, e1)
            K.dma("sp", lambda e: e.dma_start(out=A1[:], in_=modbc_d[:, 1 * D:2 * D]), A1, [modbc_d])
            K.dma("sp", lambda e: e.dma_start(out=B1[:], in_=modbc_d[:, 0:D]), B1, [modbc_d])
            K.dma("sp", lambda e: e.dma_start(out=G1[:], in_=modbc_d[:, 2 * D:3 * D]), G1, [modbc_d])
            cw = K.sb("cw", [128, 8, 4], F32, e1)
            K.dma("sp", lambda e: e.dma_start(out=cw[:], in_=cw_d[:]), cw, [cw_d])
            sinkb = K.sb("sinkb", [128, 8], F32, e1); bcast_load(sinkb, sink_d[:], sink_d)
            mnwb = K.sb("mnwb", [128, 512], F32, e1); bcast_load(mnwb, mnw_d[:], mnw_d)
            ibc = K.sb("ibc", [4, 1], F32, e1); fbc = K.sb("fbc", [4, 1], F32, e1)
            K.dma("sp", lambda e: e.dma_start(out=ibc[:], in_=ib_d[:]), ibc, [ib_d])
            K.dma("sp", lambda e: e.dma_start(out=fbc[:], in_=fb_d[:]), fbc, [fb_d])
            nfb = K.sb("nfb", [4, 1], F32, e1)
            K.op("dve", lambda e: e.tensor_scalar(out=nfb[:], in0=fbc[:], scalar1=-1.0, scalar2=None, op0=ALU.mult), [fbc], [nfb])
            valid = K.sb("valid", [128, NP1], F32, e1); pen = K.sb("pen", [128, NP1], F32, e1)
            K.dma("sp", lambda e: e.dma_start(out=valid[:], in_=valid_d[:]), valid, [valid_d])
            K.op("dve", lambda e: e.tensor_scalar(out=pen[:], in0=valid[:], scalar1=-1.0, scalar2=1e30, op0=ALU.add, op1=ALU.mult),
                 [valid], [pen])
            masks = [K.sb("mask0", [128, 256], F32, e1), K.sb("maskN", [128, 256], F32, e1)]
            K.dma("sp", lambda e: e.dma_start(out=masks[0][:], in_=mask0_d[:]), masks[0], [mask0_d])
            K.dma("sp", lambda e: e.dma_start(out=masks[1][:], in_=maskN_d[:]), masks[1], [maskN_d])
            tri = K.sb("tri", [128, 128], F32, e1)
            K.dma("sp", lambda e: e.dma_start(out=tri[:], in_=tri_d[:]), tri, [tri_d])
            sel = K.sb("sel", [4, 4, 128], F32, e1); i4 = K.sb("i4", [4, 4], F32, e1)
            K.dma("sp", lambda e: e.dma_start(out=sel[:], in_=sel_d[:]), sel, [sel_d])
            K.dma("sp", lambda e: e.dma_start(out=i4[:], in_=i4_d[:]), i4, [i4_d])

            xt = [K.sb("xt0", [128, D], F32, e1)] * 2
            ut = K.sb("ut", [128, D], F32, e1)
            junkF = K.sb("junk", [128, 512], F32, e1)
            ssq = K.sb("ssq", [128, 4], F32, e1); rstd = K.sb("rstd", [128, 1], F32, e1)
            uT = K.sb("uT", [128, 8, 128], BF16, e1)
            qk = K.sb("qk", [128, 640], F32, e1)
            vsb = [K.sb("vsb%d" % i, [128, 2, 64], BF16, e1) for i in range(2)]
            mv = K.sb("mv", [128, 4, 129], BF16, e1)
            K.op("dve", lambda e: e.memset(mv[:], 1.0), [], [mv])
            so = K.sb("so", [128, 512], F32, e1)
            sg = K.sb("sg", [128, 2048], BF16, e1)
            cin = K.sb("cin", [128, 8, 131], F32, e1)
            K.op("dve", lambda e: e.memset(cin[:], 0.0), [], [cin])
            qkT = K.sb("qkT", [128, 8, 128], BF16, e1)
            cs = K.sb("cs", [128, 32], F32, e1); sn = K.sb("sn", [128, 32], F32, e1)
            qkr = K.sb("qkr", [128, 10, 64], BF16, e1)
            qT = K.sb("qT", [64, 8, 128], BF16, e1)
            kT = [K.sb("kT%d" % i, [64, 2, 128], BF16, e1) for i in range(2)]
            s_sb = K.sb("s_sb", [128, 8, 256], F32, e1)
            p_bf = K.sb("p_bf", [128, 8, 256], BF16, e1)
            s_flat = s_sb[:].rearrange("p h k -> p (h k)")
            class _V:
                def __init__(s_, ap): s_.ap = ap
                def __getitem__(s_, k): return s_.ap[k]
            rtv = [_V(s_flat[:, i * 320:(i + 1) * 320].rearrange("p (h d) -> p h d", d=32)) for i in range(4)]
            m1 = _V(s_flat[:, 0:D]); m2 = _V(s_flat[:, D:2 * D])
            cy = _V(s_flat[:, 0:D].rearrange("p (j t) -> p j t", t=128))
            mg = _V(p_bf[:].rearrange("p h k -> p (h k)")[:, 0:D])
            sm8 = K.sb("sm8", [128, 40], F32, e1)
            pT = K.sb("pT", [128, 16, 128], BF16, e1)
            attT = K.sb("attT", [64, 8, 128], BF16, e1)
            g = {n: K.sb("g_" + n, [4, 128], F32, e1) for n in
                 ("i", "e", "lf", "b", "a", "ew", "imb", "r", "mm", "nmm", "wi", "mo", "em", "one")}
            K.op("dve", lambda e: e.memset(g["one"][:], 1.0), [], [g["one"]])
            gs = K.sb("gs", [4, 16], F32, e1)
            mst = K.sb("mst", [4, 1], F32, e1)
            K.op("dve", lambda e: e.memset(mst[:], 0.0), [], [mst])
            dg = K.sb("dg", [4, 4], F32, e1)
            tms = K.sb("tms", [128, 20], F32, e1)
            Cst = K.sb("Cst", [128, 4, 129], F32, e1)
            K.op("dve", lambda e: e.memset(Cst[:], 0.0), [], [Cst])
            Cbf = K.sb("Cbf", [128, 4, 129], BF16, e1)
            wk = K.sb("wk", [128, 4, 128], BF16, e1)
            Eb = K.sb("Eb", [128, 128], F32, e1)
            WT = K.sb("WT", [128, 128], BF16, e1)
            p1 = K.sb("p1", [128, 129], F32, e1); nd = K.sb("nd", [128, 129], F32, e1)
            hid = K.sb("hid", [128, 512], F32, e1); hsq = junkF
            hs = K.sb("hs", [128, 12], F32, e1)
            mls = K.sb("mls", [128, 512], BF16, e1)
            mlsT = K.sb("mlsT", [128, 4, 128], BF16, e1)
            mT = pT
            hh = ut

            def chunk(ci, prefix, kv, full):
                x = xt[ci % 2]
                srcd = xp if prefix else xo
                K.dma("sp", lambda e: e.dma_start(out=x[:], in_=srcd[ci * 128:(ci + 1) * 128, :]), x, [srcd])
                rmsnorm_mod(x, ut, A1, B1, (junkF, ssq, rstd))
                def dump(name, buf, ap, w):
                    if full and DBG == name:
                        K.dma("pool", lambda e: e.dma_start(out=y[ci * 128:(ci + 1) * 128, 0:w], in_=ap), y, [buf])
                dump("u", ut, ut[:], D)
                if DBG == "AB":
                    DBGs = "AB"
                    K.dma("pool", lambda e: e.dma_start(out=y[ci * 128:(ci + 1) * 128, :], in_=(A1 if ci == 0 else B1)[:]), y, [A1, B1]) if full else None
                transpose_to(ut, uT, 8, P[0], P[1])
                par = (ci % 2) if full else 1
                if kv and not full:
                    par = 1
                def tm_group(pb, c0, c1):
                    for kc in range(8):
                        K.op("pe", lambda e: e.matmul(pb[:, 0:c1 - c0], lhsT=uT[:, kc, :], rhs=w_in[:, kc, c0:c1],
                                                      start=(kc == 0), stop=(kc == 7)), [uT, w_in], [pb])
                if kv:
                    tm_group(P[2], 0, 512); tm_group(P[3], 512, 768)
                    K.op("act", lambda e: e.activation(out=qk[:, 0:512], in_=P[2][:, :], func=AF.Copy), [P[2]], [qk])
                    K.op("act", lambda e: e.activation(out=qk[:, 512:640], in_=P[3][:, 0:128], func=AF.Copy), [P[3]], [qk])
                    vdst = vsb[par]
                    K.op("dve", lambda e: e.tensor_copy(out=vdst[:].rearrange("p a b -> p (a b)"), in_=P[3][:, 128:256]),
                         [P[3]], [vdst])
                tm_group(P[4], 1792, 2304)
                K.op("act", lambda e: e.activation(out=mv[:, :, 0:128], in_=P[4][:, :].rearrange("p (a b) -> p a b", b=128),
                                                   func=AF.Copy), [P[4]], [mv])
                if full:
                    tm_group(P[5], 2304, 2816)
                    K.op("act", lambda e: e.activation(out=so[:], in_=P[5][:, :], func=AF.Sigmoid), [P[5]], [so])
                    for j in range(4):
                        pb = P[2 + (j % 2)]
                        tm_group(pb, 2824 + j * 512, 2824 + (j + 1) * 512)
                        K.op("act", lambda e: e.activation(out=sg[:, j * 512:(j + 1) * 512], in_=pb[:, :], func=AF.Sigmoid),
                             [pb], [sg])
                vcol = valid[:, ci:ci + 1] if prefix else ones[:, 0:1]
                vb = valid if prefix else ones
                j0 = 0 if (full or kv) else 4
                for j in range(j0, 8):
                    pb = P[6 + (j // 4) % 2]
                    q = j % 4
                    for kc in range(8):
                        K.op("pe", lambda e: e.matmul(pb[:, q * 128:(q + 1) * 128], lhsT=w_in[:, kc, 768 + j * 128:768 + (j + 1) * 128],
                                                      rhs=uT[:, kc, :], start=(kc == 0), stop=(kc == 7)), [uT, w_in], [pb])
                    K.op("dve", lambda e: e.tensor_scalar(out=cin[:, j, 3:131], in0=pb[:, q * 128:(q + 1) * 128],
                                                          scalar1=vcol, scalar2=None, op0=ALU.mult), [pb, vb], [cin])
                pgi = P[0]; pgf = P[1]
                for kc in range(8):
                    K.op("pe", lambda e: e.matmul(pgi[0:4, 0:128], lhsT=w_in[:, kc, 2816:2820], rhs=uT[:, kc, :],
                                                  start=(kc == 0), stop=(kc == 7)), [uT, w_in], [pgi])
                for kc in range(8):
                    K.op("pe", lambda e: e.matmul(pgf[0:4, 0:128], lhsT=w_in[:, kc, 2820:2824], rhs=uT[:, kc, :],
                                                  start=(kc == 0), stop=(kc == 7)), [uT, w_in], [pgf])
                for j in range(j0, 8):
                    K.op("dve", lambda e: e.tensor_scalar(out=cy[:, j, :], in0=cin[:, j, 0:128], scalar1=cw[:, j, 0:1],
                                                          scalar2=None, op0=ALU.mult), [cin, cw], [s_sb])
                    for t in range(1, 4):
                        K.op("dve", lambda e: e.scalar_tensor_tensor(out=cy[:, j, :], in0=cin[:, j, t:t + 128],
                                                                     scalar=cw[:, j, t:t + 1], in1=cy[:, j, :],
                                                                     op0=ALU.mult, op1=ALU.add), [cin, cw, s_sb], [s_sb])
                K.op("pool", lambda e: e.tensor_copy(out=cin[:, j0:8, 0:3], in_=cin[:, j0:8, 128:131]), [cin], [cin])
                K.op("act", lambda e: e.activation(out=qkT[:, j0:8, :], in_=cy[:, j0:8, :], func=AF.Silu), [s_sb], [qkT])
                K.op("pool", lambda e: e.tensor_scalar(out=qkT[:, 4:8, :], in0=qkT[:, 4:8, :], scalar1=128.0 ** -0.5,
                                                       scalar2=None, op0=ALU.mult), [qkT], [qkT])
                gi, ge, lf, gb, ga_, ew, imb, gr, mm, nmm, wi, gmo, em, gone = (g[n] for n in
                    ("i", "e", "lf", "b", "a", "ew", "imb", "r", "mm", "nmm", "wi", "mo", "em", "one"))
                if prefix:
                    K.op("dve", lambda e: e.tensor_scalar(out=gi[:], in0=pgi[0:4, 0:128], scalar1=ibc[:, 0:1], scalar2=valid[0:4, ci:ci + 1],
                                                          op0=ALU.add, op1=ALU.mult), [pgi, ibc, valid], [gi])
                    K.op("dve", lambda e: e.tensor_scalar(out=gi[:], in0=gi[:], scalar1=pen[0:4, ci:ci + 1], scalar2=None,
                                                          op0=ALU.add), [gi, pen], [gi])
                else:
                    K.op("dve", lambda e: e.tensor_scalar(out=gi[:], in0=pgi[0:4, 0:128], scalar1=ibc[:, 0:1], scalar2=None,
                                                          op0=ALU.add), [pgi, ibc], [gi])
                K.op("act", lambda e: e.activation(out=ge[:], in_=pgf[0:4, 0:128], func=AF.Exp, bias=nfb[:, 0:1], scale=-1.0),
                     [pgf, nfb], [ge])
                K.op("act", lambda e: e.activation(out=ge[:], in_=ge[:], func=AF.Ln, bias=1.0), [ge], [ge])
                v4 = valid[0:4, ci:ci + 1] if prefix else ones[0:4, 0:1]
                K.op("dve", lambda e: e.tensor_scalar(out=lf[:], in0=ge[:], scalar1=v4, scalar2=-1.0, op0=ALU.mult, op1=ALU.mult),
                     [ge, vb], [lf])
                K.op("dve", lambda e: e.tensor_tensor_scan(out=gb[:], data0=gone[:], data1=lf[:], initial=0.0,
                                                           op0=ALU.mult, op1=ALU.add), [gone, lf], [gb])
                K.op("dve", lambda e: e.tensor_tensor(out=imb[:], in0=gi[:], in1=gb[:], op=ALU.subtract), [gi, gb], [imb])
                K.op("dve", lambda e: e.tensor_scalar(out=ga_[:], in0=imb[:], scalar1=gb[:, 127:128], scalar2=None, op0=ALU.add),
                     [imb, gb], [ga_])
                K.op("dve", lambda e: e.tensor_reduce(out=gs[:, 0:1], in_=ga_[:], axis=AX.X, op=ALU.max), [ga_], [gs])
                K.op("dve", lambda e: e.tensor_tensor(out=gs[:, 1:2], in0=gb[:, 127:128], in1=mst[:], op=ALU.add), [gb, mst], [gs])
                K.op("dve", lambda e: e.tensor_tensor(out=gs[:, 2:3], in0=gs[:, 1:2], in1=gs[:, 0:1], op=ALU.max), [gs], [gs])
                K.op("dve", lambda e: e.tensor_scalar(out=gs[:, 3:4], in0=gs[:, 2:3], scalar1=-1.0, scalar2=None, op0=ALU.mult), [gs], [gs])
                K.op("dve", lambda e: e.tensor_tensor(out=gs[:, 4:5], in0=gs[:, 1:2], in1=gs[:, 2:3], op=ALU.subtract), [gs], [gs])
                K.op("act", lambda e: e.activation(out=gs[:, 5:6], in_=gs[:, 4:5], func=AF.Exp), [gs], [gs])
                K.op("act", lambda e: e.activation(out=ew[:], in_=ga_[:], func=AF.Exp, bias=gs[:, 3:4]), [ga_, gs], [ew])
                K.op("dve", lambda e: e.tensor_scalar(out=dg[:], in0=i4[:], scalar1=gs[:, 5:6], scalar2=None, op0=ALU.mult), [i4, gs], [dg])
                ptm = P[0]
                rows = [ew]
                if full:
                    K.op("dve", lambda e: e.tensor_tensor_scan(out=gr[:], data0=gone[:], data1=imb[:], initial=-1e30,
                                                               op0=ALU.mult, op1=ALU.max), [gone, imb], [gr])
                    K.op("dve", lambda e: e.tensor_scalar(out=mm[:], in0=gr[:], scalar1=mst[:, 0:1], scalar2=None, op0=ALU.max), [gr, mst], [mm])
                    K.op("dve", lambda e: e.tensor_scalar(out=nmm[:], in0=mm[:], scalar1=-1.0, scalar2=None, op0=ALU.mult), [mm], [nmm])
                    K.op("act", lambda e: e.activation(out=wi[:], in_=mm[:], func=AF.Exp, bias=mst[:, 0:1], scale=-1.0), [mm, mst], [wi])
                    K.op("dve", lambda e: e.tensor_tensor(out=gmo[:], in0=gb[:], in1=mm[:], op=ALU.add), [gb, mm], [gmo])
                    K.op("act", lambda e: e.activation(out=em[:], in_=gmo[:], func=AF.Exp, scale=-1.0), [gmo], [em])
                    rows = [ew, imb, wi, em]
                for r_i, rr in enumerate(rows):
                    K.op("pe", lambda e: e.transpose(out=ptm[:, r_i * 4:(r_i + 1) * 4], in_=rr[:, :], identity=ident[0:4, 0:4]),
                         [rr, ident], [ptm])
                K.op("pe", lambda e: e.matmul(ptm[:, 16:20], lhsT=ones[0:4, :], rhs=dg[:, :], start=True, stop=True), [ones, dg], [ptm])
                K.op("act", lambda e: e.activation(out=tms[:], in_=ptm[:, 0:20], func=AF.Copy), [ptm], [tms])

                if kv:
                    K.dma("sp", lambda e: e.dma_start(out=cs[:], in_=cos_d[(ci + 1 if full else 0) * 128:(ci + 2 if full else 1) * 128, :]), cs, [cos_d])
                    K.dma("sp", lambda e: e.dma_start(out=sn[:], in_=sin_d[(ci + 1 if full else 0) * 128:(ci + 2 if full else 1) * 128, :]), sn, [sin_d])
                    dump("qk", qk, qk[:], 640)
                    q4 = qk[:].rearrange("p (h two d) -> p h two d", two=2, d=32)
                    x1 = q4[:, :, 0, :]; x2 = q4[:, :, 1, :]
                    cb = cs[:].unsqueeze(1).to_broadcast([128, 10, 32]); sbb = sn[:].unsqueeze(1).to_broadcast([128, 10, 32])
                    qr4 = qkr[:].rearrange("p h (two d) -> p h two d", two=2)
                    K.op("dve", lambda e: e.tensor_tensor(out=rtv[0][:], in0=x1, in1=cb, op=ALU.mult), [qk, cs], [s_sb])
                    K.op("pool", lambda e: e.tensor_tensor(out=rtv[1][:], in0=x2, in1=sbb, op=ALU.mult), [qk, sn], [s_sb])
                    K.op("dve", lambda e: e.tensor_tensor(out=qr4[:, :, 0, :], in0=rtv[0][:], in1=rtv[1][:], op=ALU.subtract), [s_sb], [qkr])
                    K.op("pool", lambda e: e.tensor_tensor(out=rtv[2][:], in0=x2, in1=cb, op=ALU.mult), [qk, cs], [s_sb])
                    K.op("dve", lambda e: e.tensor_tensor(out=rtv[3][:], in0=x1, in1=sbb, op=ALU.mult), [qk, sn], [s_sb])
                    K.op("dve", lambda e: e.tensor_tensor(out=qr4[:, :, 1, :], in0=rtv[2][:], in1=rtv[3][:], op=ALU.add), [s_sb], [qkr])
                    pq = P[2]; pk = P[3]
                    pqv = pq[0:64, :].bitcast(BF16).rearrange("p (a b) -> p a b", b=128)
                    pkv = pk[0:64, :].bitcast(BF16).rearrange("p (a b) -> p a b", b=128)
                    h0 = 0 if full else 8
                    for h in range(h0, 10):
                        dst = pqv[:, h, :] if h < 8 else pkv[:, h - 8, :]
                        pbuf = pq if h < 8 else pk
                        K.op("pe", lambda e: e.transpose(out=dst, in_=qkr[:, h, :], identity=identb[:]), [qkr, identb], [pbuf])
                    if full:
                        K.op("act", lambda e: e.activation(out=qT[:], in_=pqv[:, 0:8, :], func=AF.Copy), [pq], [qT])
                    kdst = kT[par]
                    K.op("act", lambda e: e.activation(out=kdst[:], in_=pkv[:, 0:2, :], func=AF.Copy), [pk], [kdst])

                if full:
                    kprev, kcur = kT[1 - par], kT[par]
                    vprev, vcur = vsb[1 - par], vsb[par]
                    mk_ = masks[0] if ci == 0 else masks[1]
                    for h in range(8):
                        pb = P[2 + h // 2]
                        o = (h % 2) * 256
                        K.op("pe", lambda e: e.matmul(pb[:, o:o + 128], lhsT=qT[:, h, :], rhs=kprev[:, h // 4, :], start=True, stop=True),
                             [qT, kprev], [pb])
                        K.op("pe", lambda e: e.matmul(pb[:, o + 128:o + 256], lhsT=qT[:, h, :], rhs=kcur[:, h // 4, :], start=True, stop=True),
                             [qT, kcur], [pb])
                    for q in range(4):
                        pb = P[2 + q]
                        K.op("dve", lambda e: e.scalar_tensor_tensor(out=s_sb[:, 2 * q:2 * q + 2, :],
                                                                     in0=pb[:, :].rearrange("p (a b) -> p a b", b=256), scalar=0.125,
                                                                     in1=mk_[:].unsqueeze(1).to_broadcast([128, 2, 256]),
                                                                     op0=ALU.mult, op1=ALU.add), [pb, mk_], [s_sb])
                    K.op("dve", lambda e: e.tensor_reduce(out=sm8[:, 0:8], in_=s_sb[:], axis=AX.X, op=ALU.max), [s_sb], [sm8])
                    K.op("dve", lambda e: e.tensor_tensor(out=sm8[:, 0:8], in0=sm8[:, 0:8], in1=sinkb[:], op=ALU.max), [sm8, sinkb], [sm8])
                    K.op("dve", lambda e: e.tensor_tensor(out=s_sb[:], in0=s_sb[:], in1=sm8[:, 0:8].unsqueeze(2).to_broadcast([128, 8, 256]),
                                                          op=ALU.subtract), [s_sb, sm8], [s_sb])
                    K.op("act", lambda e: e.activation(out=s_sb[:], in_=s_sb[:], func=AF.Exp), [s_sb], [s_sb])
                    K.op("dve", lambda e: e.tensor_reduce(out=sm8[:, 16:24], in_=s_sb[:], axis=AX.X, op=ALU.add), [s_sb], [sm8])
                    K.op("dve", lambda e: e.tensor_tensor(out=sm8[:, 8:16], in0=sinkb[:], in1=sm8[:, 0:8], op=ALU.subtract), [sm8, sinkb], [sm8])
                    K.op("act", lambda e: e.activation(out=sm8[:, 24:32], in_=sm8[:, 8:16], func=AF.Exp), [sm8], [sm8])
                    K.op("dve", lambda e: e.tensor_tensor(out=sm8[:, 16:24], in0=sm8[:, 16:24], in1=sm8[:, 24:32], op=ALU.add), [sm8], [sm8])
                    K.op("dve", lambda e: e.reciprocal(out=sm8[:, 32:40], in_=sm8[:, 16:24]), [sm8], [sm8])
                    K.op("dve", lambda e: e.tensor_tensor(out=p_bf[:], in0=s_sb[:], in1=sm8[:, 32:40].unsqueeze(2).to_broadcast([128, 8, 256]),
                                                          op=ALU.mult), [s_sb, sm8], [p_bf])
                    for hh_ in range(2):
                        pb = P[2 + hh_]
                        pvw = pb[:, :].bitcast(BF16).rearrange("p (a b) -> p a b", b=128)
                        for h4 in range(4):
                            h = hh_ * 4 + h4
                            for half in range(2):
                                K.op("pe", lambda e: e.transpose(out=pvw[:, h4 * 2 + half, :], in_=p_bf[:, h, half * 128:(half + 1) * 128],
                                                                 identity=identb[:]), [p_bf, identb], [pb])
                        K.op("act", lambda e: e.activation(out=pT[:, hh_ * 8:(hh_ + 1) * 8, :], in_=pvw[:, 0:8, :], func=AF.Copy), [pb], [pT])
                    for hh_ in range(2):
                        pb = P[4 + hh_]
                        for h4 in range(4):
                            h = hh_ * 4 + h4
                            K.op("pe", lambda e: e.matmul(pb[0:64, h4 * 128:(h4 + 1) * 128], lhsT=vprev[:, h // 4, :], rhs=pT[:, 2 * h, :],
                                                          start=True, stop=False), [vprev, pT], [pb])
                            K.op("pe", lambda e: e.matmul(pb[0:64, h4 * 128:(h4 + 1) * 128], lhsT=vcur[:, h // 4, :], rhs=pT[:, 2 * h + 1, :],
                                                          start=False, stop=True), [vcur, pT], [pb])
                        K.op("act", lambda e: e.activation(out=attT[:, hh_ * 4:(hh_ + 1) * 4, :],
                                                           in_=pb[0:64, :].rearrange("p (a b) -> p a b", b=128), func=AF.Copy), [pb], [attT])

                    K.op("act", lambda e: e.activation(out=Cbf[:], in_=Cst[:], func=AF.Copy), [Cst], [Cbf])
                    for h in range(4):
                        pbc = P[6]; pst = P[7]
                        K.op("pe", lambda e: e.matmul(pbc[:, 0:128], lhsT=sel[:, h, :], rhs=nmm[:, :], start=True, stop=True), [sel, nmm], [pbc])
                        K.op("act", lambda e: e.activation(out=Eb[:], in_=pbc[:, 0:128], func=AF.Exp, bias=tms[:, 4 + h:5 + h]), [pbc, tms], [Eb])
                        K.op("pool", lambda e: e.tensor_tensor(out=Eb[:], in0=Eb[:], in1=tri[:], op=ALU.mult), [Eb, tri], [Eb])
                        K.op("pe", lambda e: e.matmul(pst[:, 0:128], lhsT=qkT[:, 4 + h, :], rhs=qkT[:, h, :], start=True, stop=True), [qkT], [pst])
                        K.op("dve", lambda e: e.tensor_tensor(out=WT[:], in0=pst[:, 0:128], in1=Eb[:], op=ALU.mult), [pst, Eb], [WT])
                        K.op("pe", lambda e: e.matmul(pbc[:, 128:257], lhsT=WT[:], rhs=mv[:, h, :], start=True, stop=True), [WT, mv], [pbc])
                        K.op("pe", lambda e: e.matmul(pst[:, 128:257], lhsT=qkT[:, h, :], rhs=Cbf[:, h, :], start=True, stop=True), [qkT, Cbf], [pst])
                        K.op("act", lambda e: e.activation(out=p1[:], in_=pbc[:, 128:257], func=AF.Copy), [pbc], [p1])
                        K.op("dve", lambda e: e.scalar_tensor_tensor(out=nd[:], in0=pst[:, 128:257], scalar=tms[:, 8 + h:9 + h], in1=p1[:],
                                                                     op0=ALU.mult, op1=ALU.add), [pst, tms, p1], [nd])
                        K.op("dve", lambda e: e.scalar_tensor_tensor(out=hs[:, 10:11], in0=nd[:, 128:129], scalar=-1.0, in1=nd[:, 128:129],
                                                                     op0=ALU.mult, op1=ALU.max), [nd], [hs])
                        K.op("dve", lambda e: e.tensor_scalar(out=hs[:, 8:9], in0=hs[:, 10:11], scalar1=tms[:, 12 + h:13 + h], scalar2=None,
                                                              op0=ALU.max), [hs, tms], [hs])
                        K.op("dve", lambda e: e.reciprocal(out=hs[:, 9:10], in_=hs[:, 8:9]), [hs], [hs])
                        K.op("dve", lambda e: e.scalar_tensor_tensor(out=hid[:, h * 128:(h + 1) * 128], in0=nd[:, 0:128], scalar=hs[:, 9:10],
                                                                     in1=so[:, h * 128:(h + 1) * 128], op0=ALU.mult, op1=ALU.mult), [nd, hs, so], [hid])
                    dump("hid", hid, hid[:], 512)
                    K.op("pool", lambda e: e.tensor_tensor(out=hsq[:], in0=hid[:], in1=hid[:], op=ALU.mult), [hid], [hsq])
                    K.op("dve", lambda e: e.tensor_reduce(out=hs[:, 0:4], in_=hsq[:].rearrange("p (a b) -> p a b", b=128), axis=AX.X, op=ALU.add),
                         [hsq], [hs])
                    K.op("dve", lambda e: e.tensor_scalar(out=hs[:, 0:4], in0=hs[:, 0:4], scalar1=1.0 / 128, scalar2=EPS, op0=ALU.mult, op1=ALU.add), [hs], [hs])
                    K.op("act", lambda e: e.activation(out=hs[:, 0:4], in_=hs[:, 0:4], func=AF.Sqrt), [hs], [hs])
                    K.op("dve", lambda e: e.reciprocal(out=hs[:, 4:8], in_=hs[:, 0:4]), [hs], [hs])
                    for h in range(4):
                        K.op("dve", lambda e: e.scalar_tensor_tensor(out=mls[:, h * 128:(h + 1) * 128], in0=hid[:, h * 128:(h + 1) * 128],
                                                                     scalar=hs[:, 4 + h:5 + h], in1=mnwb[:, h * 128:(h + 1) * 128],
                                                                     op0=ALU.mult, op1=ALU.mult), [hid, hs, mnwb], [mls])
                    transpose_to(mls, mlsT, 4, P[6], P[7], dt32=False)

                pkt = P[6]
                pktv = pkt[:, :].bitcast(BF16).rearrange("p (a b) -> p a b", b=128)
                for h in range(4):
                    K.op("pe", lambda e: e.transpose(out=pktv[:, h, :], in_=qkT[:, 4 + h, :], identity=identb[:]), [qkT, identb], [pkt])
                for h in range(4):
                    K.op("dve", lambda e: e.tensor_scalar(out=wk[:, h, :], in0=pktv[:, h, :], scalar1=tms[:, h:h + 1], scalar2=None, op0=ALU.mult),
                         [pkt, tms], [wk])
                pu_ = P[7]
                for h in range(4):
                    K.op("pe", lambda e: e.matmul(pu_[:, h * 129:(h + 1) * 129] if h < 3 else P[6][:, 0:129], lhsT=wk[:, h, :], rhs=mv[:, h, :],
                                                  start=True, stop=True), [wk, mv], [pu_ if h < 3 else P[6]])
                for h in range(4):
                    src = pu_[:, h * 129:(h + 1) * 129] if h < 3 else P[6][:, 0:129]
                    sb_ = pu_ if h < 3 else P[6]
                    K.op("dve", lambda e: e.scalar_tensor_tensor(out=Cst[:, h, :], in0=Cst[:, h, :], scalar=tms[:, 16 + h:17 + h], in1=src,
                                                                 op0=ALU.mult, op1=ALU.add), [Cst, tms, sb_], [Cst])
                K.op("dve", lambda e: e.tensor_copy(out=mst[:], in_=gs[:, 2:3]), [gs], [mst])

                if full:
                    for nh in range(2):
                        pa_ = P[2 + nh]; pb_ = P[4 + nh]
                        for h in range(8):
                            K.op("pe", lambda e: e.matmul(pa_[:, :], lhsT=attT[:, h, :], rhs=wa[:, h, nh * 512:(nh + 1) * 512],
                                                          start=(h == 0), stop=(h == 7)), [attT, wa], [pa_])
                        for kc in range(4):
                            K.op("pe", lambda e: e.matmul(pb_[:, :], lhsT=mlsT[:, kc, :], rhs=wb[:, kc, nh * 512:(nh + 1) * 512],
                                                          start=(kc == 0), stop=(kc == 3)), [mlsT, wb], [pb_])
                        K.op("dve", lambda e: e.tensor_tensor(out=m1[:, nh * 512:(nh + 1) * 512], in0=pa_[:, :], in1=sg[:, nh * 512:(nh + 1) * 512],
                                                              op=ALU.mult), [pa_, sg], [s_sb])
                        K.op("dve", lambda e: e.tensor_tensor(out=m2[:, nh * 512:(nh + 1) * 512], in0=pb_[:, :], in1=sg[:, D + nh * 512:D + (nh + 1) * 512],
                                                              op=ALU.mult), [pb_, sg], [s_sb])
                    K.op("pool", lambda e: e.tensor_tensor(out=mg[:], in0=(m2 if ONLY == "b" else m1)[:], in1=(m1 if ONLY == "a" else m2)[:], op=ALU.add), [s_sb], [p_bf])
                    transpose_to(mg, mT, 8, P[2], P[3], dt32=False, srcbuf=p_bf)
                    for nh in range(2):
                        po = P[4 + nh]
                        for kc in range(8):
                            K.op("pe", lambda e: e.matmul(po[:, :], lhsT=mT[:, kc, :], rhs=wo[:, kc, nh * 512:(nh + 1) * 512],
                                                          start=(kc == 0), stop=(kc == 7)), [mT, wo], [po])
                        K.op("dve", lambda e: e.tensor_tensor(out=m1[:, nh * 512:(nh + 1) * 512], in0=po[:, :], in1=G1[:, nh * 512:(nh + 1) * 512],
                                                              op=ALU.mult), [po, G1], [s_sb])
                    K.op("pool", lambda e: e.tensor_tensor(out=hh[:], in0=m1[:], in1=x[:], op=ALU.add), [s_sb, x], [hh])
                    K.dma("pool", lambda e: e.dma_start(out=hbuf_d[ci * 128:(ci + 1) * 128, :], in_=hh[:]), hbuf_d, [hh])

            class _V2:
                def __init__(s_, ap): s_.ap = ap
                def __getitem__(s_, k): return s_.ap[k]
            sgf = sg[:].bitcast(F32)
            pbf32 = p_bf[:].rearrange("p h k -> p (h k)").bitcast(F32)
            hidb = hid[:].bitcast(BF16)
            mlsf = mls[:].bitcast(F32)
            PX = [(xt[0], _V2(xt[0][:])), (s_sb, _V2(s_flat[:, D:2 * D]))]
            PU = [(ut, _V2(ut[:])), (p_bf, _V2(pbf32))]
            PUT = [(uT, _V2(uT[:])), (pT, _V2(pT[:, 0:8, :]))]
            PCIN = [(cin, _V2(cin[:, 4:8, :])), (sg, _V2(sgf[:, 0:524].rearrange("p (j t) -> p j t", t=131)))]
            PCY = [(s_sb, _V2(cy[:, 4:8, :])), (s_sb, _V2(cy[:, 0:4, :]))]
            PKT = [(qkT, _V2(qkT[:, 4:8, :])), (qkT, _V2(qkT[:, 0:4, :]))]
            PMV = [(mv, _V2(mv[:])), (hid, _V2(hidb[:, 0:516].rearrange("p (h e) -> p h e", e=129)))]
            PWK = [(wk, _V2(wk[:])), (qkr, _V2(qkr[:].rearrange("p h d -> p (h d)")[:, 0:512].rearrange("p (h d) -> p h d", d=128)))]
            g1n = ("i", "e", "lf", "b", "a", "ew", "imb")
            PG = [{n: (g[n], _V2(g[n][:])) for n in g1n},
                  {n: ((so, _V2(so[0:4, k * 128:(k + 1) * 128])) if k < 4 else (mls, _V2(mlsf[0:4, (k - 4) * 128:(k - 3) * 128])))
                   for k, n in enumerate(("i", "e", "lf", "b", "a", "ew"))}]
            PG[1]["imb"] = (qk, _V2(qk[0:4, 128:256]))
            PGS = [(gs, _V2(gs[:])), (qk, _V2(qk[0:4, 0:16]))]
            PDG = [(dg, _V2(dg[:])), (qk, _V2(qk[0:4, 16:20]))]
            PTMS = [(tms, _V2(tms[:])), (qk, _V2(qk[:, 32:52]))]
            PSQ = [((ssq, _V2(ssq[:])), (rstd, _V2(rstd[:]))), ((qk, _V2(qk[:, 60:64])), (qk, _V2(qk[:, 64:65])))]
            tEb = K.view(pT, "tEb")
            if NPRE > 1:
                K.op("dve", lambda e: e.memset(PMV[1][1][:], 1.0), [], [hid])
                K.op("dve", lambda e: e.memset(PCIN[1][1][:], 0.0), [], [sg])

            def pfA(ci, p):
                xb, xv = PX[p]; ub, uv = PU[p]; utb, utv = PUT[p]; cb_, cv = PCIN[p]; mb, mvv = PMV[p]
                (sqb, sqv), (rsb, rsv) = PSQ[p]
                Q0, Q1, Q2, Q3 = P[4 * p:4 * p + 4]
                K.dma("sp", lambda e: e.dma_start(out=xv[:], in_=xp[ci * 128:(ci + 1) * 128, :]), xb, [xp])
                K.op("act", lambda e: e.activation(out=junkF[:].bitcast(BF16), in_=xv[:], func=AF.Square, accum_out=sqv[:, 0:1]), [xb], [junkF, sqb]); yield
                K.op("dve", lambda e: e.tensor_scalar(out=sqv[:, 1:2], in0=sqv[:, 0:1], scalar1=1.0 / D, scalar2=EPS, op0=ALU.mult, op1=ALU.add), [sqb], [sqb]); yield
                K.op("act", lambda e: e.activation(out=sqv[:, 2:3], in_=sqv[:, 1:2], func=AF.Ln), [sqb], [sqb]); yield
                K.op("act", lambda e: e.activation(out=rsv[:], in_=sqv[:, 2:3], func=AF.Exp, scale=-0.5), [sqb], [rsb]); yield
                K.op("dve", lambda e: e.scalar_tensor_tensor(out=uv[:], in0=xv[:], scalar=rsv[:, 0:1], in1=A1[:], op0=ALU.mult, op1=ALU.mult), [xb, rsb, A1], [ub]); yield
                K.op("pool", lambda e: e.tensor_tensor(out=uv[:], in0=uv[:], in1=B1[:], op=ALU.add), [ub, B1], [ub]); yield
                for half in range(2):
                    pb_ = (Q0, Q1)[half]
                    pv = pb_[:, :].rearrange("p (a b) -> p a b", b=128)
                    for q in range(4):
                        kc = half * 4 + q
                        K.op("pe", lambda e: e.transpose(out=pv[:, q, :], in_=uv[:, kc * 128:(kc + 1) * 128], identity=ident[:]), [ub, ident], [pb_])
                    K.op("act", lambda e: e.activation(out=utv[:, half * 4:half * 4 + 4, :], in_=pv[:, 0:4, :], func=AF.Copy), [pb_], [utb]); yield
                for kc in range(8):
                    K.op("pe", lambda e: e.matmul(Q2[:, 0:512], lhsT=utv[:, kc, :], rhs=w_in[:, kc, 1792:2304], start=(kc == 0), stop=(kc == 7)), [utb, w_in], [Q2])
                K.op("act", lambda e: e.activation(out=mvv[:, :, 0:128], in_=Q2[:, :].rearrange("p (a b) -> p a b", b=128), func=AF.Copy), [Q2], [mb]); yield
                for j in range(4):
                    for kc in range(8):
                        K.op("pe", lambda e: e.matmul(Q3[:, j * 128:(j + 1) * 128], lhsT=w_in[:, kc, 1280 + j * 128:1280 + (j + 1) * 128], rhs=utv[:, kc, :],
                                                      start=(kc == 0), stop=(kc == 7)), [utb, w_in], [Q3])
                K.op("dve", lambda e: e.tensor_scalar(out=cv[:, :, 3:131], in0=Q3[:, :].rearrange("p (a b) -> p a b", b=128), scalar1=valid[:, ci:ci + 1],
                                                      scalar2=None, op0=ALU.mult), [Q3, valid], [cb_]); yield
                ob, ov = PCIN[1 - p]
                K.op("pool", lambda e: e.tensor_copy(out=ov[:, :, 0:3], in_=cv[:, :, 128:131]), [cb_], [ob]); yield
                for kc in range(8):
                    K.op("pe", lambda e: e.matmul(Q0[0:4, 0:128], lhsT=w_in[:, kc, 2816:2820], rhs=utv[:, kc, :], start=(kc == 0), stop=(kc == 7)), [utb, w_in], [Q0])
                for kc in range(8):
                    K.op("pe", lambda e: e.matmul(Q0[0:4, 128:256], lhsT=w_in[:, kc, 2820:2824], rhs=utv[:, kc, :], start=(kc == 0), stop=(kc == 7)), [utb, w_in], [Q0])
                yield

            def pfB(ci, p):
                cb_, cv = PCIN[p]; cyb, cyv = PCY[p]; kb, kv_ = PKT[p]; mb, mvv = PMV[p]; wkb, wkv = PWK[p]
                G = PG[p]; gsb, gsv = PGS[p]; dgb, dgv = PDG[p]; tmb, tmv = PTMS[p]
                Q0, Q1, Q2, Q3 = P[4 * p:4 * p + 4]
                for j in range(4):
                    K.op("dve", lambda e: e.tensor_scalar(out=cyv[:, j, :], in0=cv[:, j, 0:128], scalar1=cw[:, 4 + j, 0:1], scalar2=None, op0=ALU.mult), [cb_, cw], [cyb]); yield
                    for t in range(1, 4):
                        K.op("dve", lambda e: e.scalar_tensor_tensor(out=cyv[:, j, :], in0=cv[:, j, t:t + 128], scalar=cw[:, 4 + j, t:t + 1], in1=cyv[:, j, :],
                                                                     op0=ALU.mult, op1=ALU.add), [cb_, cw, cyb], [cyb]); yield
                tE = pT[:, 8:16, :].rearrange("p a b -> p (a b)").bitcast(F32).rearrange("p (j t) -> p j t", t=128)
                K.op("act", lambda e: e.activation(out=tE, in_=cyv[:], func=AF.Exp, scale=-1.0), [cyb], [tEb]); yield
                K.op("dve", lambda e: e.tensor_scalar(out=tE, in0=tE, scalar1=1.0, scalar2=None, op0=ALU.add), [tEb], [tEb]); yield
                K.op("dve", lambda e: e.reciprocal(out=tE, in_=tE), [tEb], [tEb]); yield
                K.op("dve", lambda e: e.scalar_tensor_tensor(out=kv_[:], in0=cyv[:], scalar=128.0 ** -0.5, in1=tE, op0=ALU.mult, op1=ALU.mult), [cyb, tEb], [kb]); yield
                (gib, giv), (geb, gev), (lfb, lfv), (gbb, gbv), (gab, gav), (ewb, ewv), (imbb, imbv) = (G[n] for n in g1n)
                K.op("dve", lambda e: e.tensor_scalar(out=giv[:], in0=Q0[0:4, 0:128], scalar1=ibc[:, 0:1], scalar2=valid[0:4, ci:ci + 1], op0=ALU.add, op1=ALU.mult), [Q0, ibc, valid], [gib]); yield
                K.op("dve", lambda e: e.tensor_scalar(out=giv[:], in0=giv[:], scalar1=pen[0:4, ci:ci + 1], scalar2=None, op0=ALU.add), [gib, pen], [gib]); yield
                K.op("act", lambda e: e.activation(out=gev[:], in_=Q0[0:4, 128:256], func=AF.Exp, bias=nfb[:, 0:1], scale=-1.0), [Q0, nfb], [geb]); yield
                K.op("act", lambda e: e.activation(out=gev[:], in_=gev[:], func=AF.Ln, bias=1.0), [geb], [geb]); yield
                K.op("dve", lambda e: e.tensor_scalar(out=lfv[:], in0=gev[:], scalar1=valid[0:4, ci:ci + 1], scalar2=-1.0, op0=ALU.mult, op1=ALU.mult), [geb, valid], [lfb]); yield
                K.op("dve", lambda e: e.tensor_tensor_scan(out=gbv[:], data0=g["one"][:], data1=lfv[:], initial=0.0, op0=ALU.mult, op1=ALU.add), [g["one"], lfb], [gbb]); yield
                K.op("dve", lambda e: e.tensor_tensor(out=imbv[:], in0=giv[:], in1=gbv[:], op=ALU.subtract), [gib, gbb], [imbb]); yield
                K.op("dve", lambda e: e.tensor_scalar(out=gav[:], in0=imbv[:], scalar1=gbv[:, 127:128], scalar2=None, op0=ALU.add), [imbb, gbb], [gab]); yield
                K.op("dve", lambda e: e.tensor_reduce(out=gsv[:, 0:1], in_=gav[:], axis=AX.X, op=ALU.max), [gab], [gsb]); yield
                K.op("dve", lambda e: e.tensor_tensor(out=gsv[:, 1:2], in0=gbv[:, 127:128], in1=mst[:], op=ALU.add), [gbb, mst], [gsb]); yield
                K.op("dve", lambda e: e.tensor_tensor(out=gsv[:, 2:3], in0=gsv[:, 1:2], in1=gsv[:, 0:1], op=ALU.max), [gsb], [gsb]); yield
                K.op("dve", lambda e: e.tensor_copy(out=mst[:], in_=gsv[:, 2:3]), [gsb], [mst]); yield
                K.op("dve", lambda e: e.tensor_scalar(out=gsv[:, 3:4], in0=gsv[:, 2:3], scalar1=-1.0, scalar2=None, op0=ALU.mult), [gsb], [gsb]); yield
                K.op("dve", lambda e: e.tensor_tensor(out=gsv[:, 4:5], in0=gsv[:, 1:2], in1=gsv[:, 2:3], op=ALU.subtract), [gsb], [gsb]); yield
                K.op("act", lambda e: e.activation(out=gsv[:, 5:6], in_=gsv[:, 4:5], func=AF.Exp), [gsb], [gsb]); yield
                K.op("act", lambda e: e.activation(out=ewv[:], in_=gav[:], func=AF.Exp, bias=gsv[:, 3:4]), [gab, gsb], [ewb]); yield
                K.op("dve", lambda e: e.tensor_scalar(out=dgv[:], in0=i4[:], scalar1=gsv[:, 5:6], scalar2=None, op0=ALU.mult), [i4, gsb], [dgb]); yield
                K.op("pe", lambda e: e.transpose(out=Q0[:, 256:260], in_=ewv[:, :], identity=ident[0:4, 0:4]), [ewb, ident], [Q0])
                K.op("pe", lambda e: e.matmul(Q0[:, 272:276], lhsT=ones[0:4, :], rhs=dgv[:, :], start=True, stop=True), [ones, dgb], [Q0])
                K.op("act", lambda e: e.activation(out=tmv[:, 0:20], in_=Q0[:, 256:276], func=AF.Copy), [Q0], [tmb]); yield
                pktv = Q1[:, :].bitcast(BF16).rearrange("p (a b) -> p a b", b=128)
                for h in range(4):
                    K.op("pe", lambda e: e.transpose(out=pktv[:, h, :], in_=kv_[:, h, :], identity=identb[:]), [kb, identb], [Q1])
                for h in range(4):
                    K.op("dve", lambda e: e.tensor_scalar(out=wkv[:, h, :], in0=pktv[:, h, :], scalar1=tmv[:, h:h + 1], scalar2=None, op0=ALU.mult), [Q1, tmb], [wkb]); yield
                for h in range(4):
                    dst = Q2[:, h * 129:(h + 1) * 129] if h < 3 else Q3[:, 0:129]
                    K.op("pe", lambda e: e.matmul(dst, lhsT=wkv[:, h, :], rhs=mvv[:, h, :], start=True, stop=True), [wkb, mb], [Q2 if h < 3 else Q3])
                for h in range(4):
                    src = Q2[:, h * 129:(h + 1) * 129] if h < 3 else Q3[:, 0:129]
                    K.op("dve", lambda e: e.scalar_tensor_tensor(out=Cst[:, h, :], in0=Cst[:, h, :], scalar=tmv[:, 16 + h:17 + h], in1=src,
                                                                 op0=ALU.mult, op1=ALU.add), [Cst, tmb, Q2 if h < 3 else Q3], [Cst]); yield

            def step(gn):
                if gn is None:
                    return None
                try:
                    next(gn); return gn
                except StopIteration:
                    return None

            NPP = max(NPRE - 1, 0)
            if NPP > 0:
                ga_gen = pfA(0, 0)
                while ga_gen is not None:
                    ga_gen = step(ga_gen)
                for ci in range(NPP):
                    gb_gen = pfB(ci, ci % 2)
                    ga_gen = pfA(ci + 1, (ci + 1) % 2) if ci + 1 < NPP else None
                    while gb_gen is not None or ga_gen is not None:
                        gb_gen = step(gb_gen)
                        ga_gen = step(ga_gen)
                lb, lv = PCIN[(NPP - 1) % 2]
                if (NPP - 1) % 2 == 1:
                    K.op("pool", lambda e: e.tensor_copy(out=cin[:, 4:8, 0:3], in_=lv[:, :, 128:131]), [lb], [cin])
                K.barrier()
                K.op("dve", lambda e: e.memset(mv[:], 1.0), [], [mv])
            if NPRE > 0:
                chunk(NPRE - 1, True, True, False)
            if NPRE == 0:
                K.op("dve", lambda e: e.memset(kT[1][:], 0.0), [], [kT[1]])
                K.op("dve", lambda e: e.memset(vsb[1][:], 0.0), [], [vsb[1]])
            for c in range(NOWN):
                chunk(c, False, True, True)
        K.barrier()
        if stage == 1:
            with ExitStack() as ed:
                tt = K.sb("dbgt", [128, D], F32, ed)
                for c in range(NOWN if DBG is None else 0):
                    K.dma("sp", lambda e: e.dma_start(out=tt[:], in_=hbuf_d[c * 128:(c + 1) * 128, :]), tt, [hbuf_d])
                    K.dma("sp", lambda e: e.dma_start(out=y[c * 128:(c + 1) * 128, :], in_=tt[:]), y, [tt])
                K.finish([y])
            return nc

        with ExitStack() as e2:
            stage = [K.sb("stg2_%d" % i, [128, 2048], F32, e2) for i in range(2)]
            wq = K.sb("wq", [128, 8, 2048], BF16, e2)
            load_bf16(wq, 8, 2048, wq_d.t.rearrange("(kc p) n -> p kc n", p=128), wq_d, stage)
            skT = K.sb("skT", [128, 2, 128], F32, e2)
            K.dma("sp", lambda e: e.dma_start(out=skT[:], in_=sk_d[:]), skT, [sk_d])
            A2 = K.sb("A2", [128, D], F32, e2); B2 = K.sb("B2", [128, D], F32, e2); G2 = K.sb("G2", [128, D], F32, e2)
            K.dma("sp", lambda e: e.dma_start(out=A2[:], in_=modbc_d[:, 4 * D:5 * D]), A2, [modbc_d])
            K.dma("sp", lambda e: e.dma_start(out=B2[:], in_=modbc_d[:, 3 * D:4 * D]), B2, [modbc_d])
            K.dma("sp", lambda e: e.dma_start(out=G2[:], in_=modbc_d[:, 5 * D:6 * D]), G2, [modbc_d])
            NF = K.sb("NF", [128, D], F32, e2); bcast_load(NF, nfw_d[:], nfw_d)
            iota16 = K.sb("iota16", [128, 16], F32, e2)
            K.dma("sp", lambda e: e.dma_start(out=iota16[:], in_=iota_d[:]), iota16, [iota_d])
            ht = [K.sb("ht%d" % i, [128, D], F32, e2) for i in range(2)]
            u2 = K.sb("u2", [128, D], F32, e2)
            junk = K.sb("junk2", [128, 512], F32, e2); junkf = K.sb("junkf", [128, D], F32, e2)
            ssq = K.sb("ssq2", [128, 4], F32, e2); rstd = K.sb("rstd2", [128, 1], F32, e2)
            u2T = K.sb("u2T", [128, 8, 128], BF16, e2)
            qyT = K.sb("qyT", [128, 16, 128], F32, e2)
            sc = K.sb("sc", [128, 16, 128], F32, e2); sc2 = K.sb("sc2", [128, 16, 128], F32, e2)
            tv = K.sb("tv", [128, 16, 16], F32, e2); ti = K.sb("ti", [128, 16, 16], U32, e2)
            tif = K.sb("tif", [128, 16, 16], F32, e2)
            cand = K.sb("cand", [128, 8, 256], F32, e2); cand2 = K.sb("cand2", [128, 8, 256], F32, e2)
            bs = K.sb("bs", [128, 8, 16], F32, e2); bp = K.sb("bp", [128, 8, 16], U32, e2)
            au = K.sb("au", [128, 8, 16], U32, e2); bu = K.sb("bu", [128, 8, 16], U32, e2)
            af = K.sb("af", [128, 8, 16], F32, e2); bfl = K.sb("bfl", [128, 8, 16], F32, e2)
            eq = K.sb("eq", [128, 8, 16, 16], F32, e2)
            isl = K.sb("isl", [128, 8, 16], F32, e2); jsl = K.sb("jsl", [128, 8, 16], F32, e2)
            idxf = K.sb("idxf", [128, 128], F32, e2); idx = K.sb("idx", [128, 128], I32, e2)
            gsm = K.sb("gsm", [128, 24], F32, e2)
            gate = K.sb("gate", [128, 8, 16], F32, e2)
            dots = K.sb("dots", [128, 128], F32, e2); wsl = K.sb("wsl", [128, 128], F32, e2)
            acc = K.sb("acc", [128, D], F32, e2)
            R = 6
            ring = [K.sb("ring%d" % i, [128, D], F32, e2) for i in range(R)]
            yt = K.sb("yt", [128, D], F32, e2)
            tmpr = [K.sb("tmpr%d" % i, [128, D], BF16, e2) for i in range(3)]
            class _RB:
                def __init__(s_, b): s_.b = b
                def __getitem__(s_, k): return s_.b[:].bitcast(BF16)[:, 0:D][k]
            rcount = [0]
            blk = 0
            for (src_t, dst_t) in ((pu_d, pub_d), (pv_d, pvb_d)):
                sv_ = src_t.t.rearrange("(p q) d -> p q d", q=128)
                dv_ = dst_t.t.rearrange("(p q) d -> p q d", q=128)
                for q in range(128):
                    rbf = ring[blk % R]; tbf = tmpr[blk % 3]
                    K.dma("sp", lambda e: e.dma_start(out=rbf[:], in_=sv_[:, q, :]), rbf, [src_t])
                    engs = ("dve", "act", "pool")
                    en = engs[blk % 3]
                    if en == "act":
                        K.op("act", lambda e: e.activation(out=tbf[:], in_=rbf[:], func=AF.Copy), [rbf], [tbf])
                    else:
                        K.op(en, lambda e: e.tensor_copy(out=tbf[:], in_=rbf[:]), [rbf], [tbf])
                    K.dma("sp", lambda e: e.dma_start(out=dv_[:, q, :], in_=tbf[:]), dst_t, [tbf], waw=False)
                    blk += 1

            def gather(src_d, col, idx):
                rb = ring[rcount[0] % R]; rcount[0] += 1
                K.dma("pool", lambda e: e.indirect_dma_start(out=rb[:].bitcast(BF16)[:, 0:D], out_offset=None, in_=src_d[:, :],
                                                             in_offset=bass.IndirectOffsetOnAxis(ap=idx[:, col:col + 1], axis=0)),
                      rb, [src_d, idx])
                return rb

            idxs = [idx, K.sb("idx_b", [128, 128], I32, e2)]

            def front(c):
                h_ = ht[c % 2]
                idx = idxs[c % 2]
                K.dma("sp", lambda e: e.dma_start(out=h_[:], in_=hbuf_d[c * 128:(c + 1) * 128, :]), h_, [hbuf_d])
                rmsnorm_mod(h_, u2, A2, B2, (junk, ssq, rstd))
                transpose_to(u2, u2T, 8, P[0], P[1])
                for hp in range(16):
                    pb = P[2 + hp // 4]
                    q = hp % 4
                    for kc in range(8):
                        K.op("pe", lambda e: e.matmul(pb[:, q * 128:(q + 1) * 128], lhsT=wq[:, kc, hp * 128:(hp + 1) * 128], rhs=u2T[:, kc, :],
                                                      start=(kc == 0), stop=(kc == 7)), [wq, u2T], [pb])
                    if q == 3:
                        K.op("act", lambda e: e.activation(out=qyT[:, hp - 3:hp + 1, :], in_=pb[:, :].rearrange("p (a b) -> p a b", b=128),
                                                           func=AF.Copy), [pb], [qyT])
                        yield
                for hp in range(16):
                    pb = P[2 + hp // 4]
                    q = hp % 4
                    K.op("pe", lambda e: e.matmul(pb[:, q * 128:(q + 1) * 128], lhsT=qyT[:, hp, :], rhs=skT[:, hp % 2, :], start=True, stop=True),
                         [qyT, skT], [pb])
                    if q == 3:
                        K.op("act", lambda e: e.activation(out=sc[:, hp - 3:hp + 1, :], in_=pb[:, :].rearrange("p (a b) -> p a b", b=128),
                                                           func=AF.Copy), [pb], [sc])
                        yield
                for hp in range(16):
                    K.op("dve", lambda e: e.max(out=tv[:, hp, 0:8], in_=sc[:, hp, :]), [sc], [tv])
                    yield
                    K.op("dve", lambda e: e.max_index(out=ti[:, hp, 0:8], in_max=tv[:, hp, 0:8], in_values=sc[:, hp, :]), [sc, tv], [ti])
                    yield
                    K.op("dve", lambda e: e.match_replace(out=sc2[:, hp, :], in_to_replace=tv[:, hp, 0:8], in_values=sc[:, hp, :], imm_value=-1e30),
                         [sc, tv], [sc2])
                    yield
                    K.op("dve", lambda e: e.max(out=tv[:, hp, 8:16], in_=sc2[:, hp, :]), [sc2], [tv])
                    yield
                    K.op("dve", lambda e: e.max_index(out=ti[:, hp, 8:16], in_max=tv[:, hp, 8:16], in_values=sc2[:, hp, :]), [sc2, tv], [ti])
                    yield
                tv4 = tv[:].rearrange("p (h s) k -> p h s k", s=2)
                K.op("dve", lambda e: e.tensor_tensor(out=cand[:].rearrange("p h (a b) -> p h a b", b=16),
                                                      in0=tv4[:, :, 0, :].unsqueeze(3).to_broadcast([128, 8, 16, 16]),
                                                      in1=tv4[:, :, 1, :].unsqueeze(2).to_broadcast([128, 8, 16, 16]), op=ALU.add), [tv], [cand])
                yield
                for h in range(8):
                    K.op("dve", lambda e: e.max(out=bs[:, h, 0:8], in_=cand[:, h, :]), [cand], [bs])
                    yield
                    K.op("dve", lambda e: e.max_index(out=bp[:, h, 0:8], in_max=bs[:, h, 0:8], in_values=cand[:, h, :]), [cand, bs], [bp])
                    yield
                    K.op("dve", lambda e: e.match_replace(out=cand2[:, h, :], in_to_replace=bs[:, h, 0:8], in_values=cand[:, h, :], imm_value=-1e30),
                         [cand, bs], [cand2])
                    yield
                    K.op("dve", lambda e: e.max(out=bs[:, h, 8:16], in_=cand2[:, h, :]), [cand2], [bs])
                    yield
                    K.op("dve", lambda e: e.max_index(out=bp[:, h, 8:16], in_max=bs[:, h, 8:16], in_values=cand2[:, h, :]), [cand2, bs], [bp])
                    yield
                K.op("dve", lambda e: e.tensor_scalar(out=au[:], in0=bp[:], scalar1=4, scalar2=None, op0=ALU.logical_shift_right), [bp], [au])
                yield
                K.op("dve", lambda e: e.tensor_scalar(out=bu[:], in0=bp[:], scalar1=15, scalar2=None, op0=ALU.bitwise_and), [bp], [bu])
                yield
                K.op("dve", lambda e: e.tensor_copy(out=af[:], in_=au[:]), [au], [af])
                yield
                K.op("dve", lambda e: e.tensor_copy(out=bfl[:], in_=bu[:]), [bu], [bfl])
                yield
                K.op("dve", lambda e: e.tensor_copy(out=tif[:], in_=ti[:]), [ti], [tif])
                yield
                tif4 = tif[:].rearrange("p (h s) k -> p h s k", s=2)
                for (srcf, side, dst) in ((af, 0, isl), (bfl, 1, jsl)):
                    K.op("dve", lambda e: e.tensor_tensor(out=eq[:], in0=iota16[:].unsqueeze(1).unsqueeze(1).to_broadcast([128, 8, 16, 16]),
                                                          in1=srcf[:].unsqueeze(3).to_broadcast([128, 8, 16, 16]), op=ALU.is_equal), [iota16, srcf], [eq])
                    yield
                    K.op("dve", lambda e: e.tensor_tensor(out=eq[:], in0=eq[:], in1=tif4[:, :, side, :].unsqueeze(2).to_broadcast([128, 8, 16, 16]),
                                                          op=ALU.mult), [eq, tif], [eq])
                    yield
                    K.op("dve", lambda e: e.tensor_reduce(out=dst[:], in_=eq[:], axis=AX.X, op=ALU.add), [eq], [dst])
                    yield
                K.op("dve", lambda e: e.scalar_tensor_tensor(out=idxf[:], in0=isl[:].rearrange("p h k -> p (h k)"), scalar=128.0,
                                                             in1=jsl[:].rearrange("p h k -> p (h k)"), op0=ALU.mult, op1=ALU.add), [isl, jsl], [idxf])
                yield
                K.op("dve", lambda e: e.tensor_scalar(out=idxf[:], in0=idxf[:], scalar1=0.0, scalar2=16383.0, op0=ALU.max, op1=ALU.min), [idxf], [idxf])
                yield
                K.op("dve", lambda e: e.tensor_copy(out=idx[:], in_=idxf[:]), [idxf], [idx])
                yield
                K.op("dve", lambda e: e.tensor_tensor(out=gate[:], in0=bs[:], in1=bs[:, :, 0:1].to_broadcast([128, 8, 16]), op=ALU.subtract), [bs], [gate])
                yield
                K.op("act", lambda e: e.activation(out=gate[:], in_=gate[:], func=AF.Exp), [gate], [gate])
                yield
                K.op("dve", lambda e: e.tensor_reduce(out=gsm[:, 0:8], in_=gate[:], axis=AX.X, op=ALU.add), [gate], [gsm])
                yield
                K.op("dve", lambda e: e.reciprocal(out=gsm[:, 8:16], in_=gsm[:, 0:8]), [gsm], [gsm])
                yield
                K.op("dve", lambda e: e.tensor_tensor(out=gate[:], in0=gate[:], in1=gsm[:, 8:16].unsqueeze(2).to_broadcast([128, 8, 16]), op=ALU.mult),
                     [gate, gsm], [gate])
                yield
                yield

            def drain(g, n=None):
                k = 0
                while g is not None and (n is None or k < n):
                    try:
                        next(g)
                    except StopIteration:
                        return None
                    k += 1
                return g

            gen = drain(front(0))
            for c in range(NOWN):
                h_ = ht[c % 2]
                idx = idxs[c % 2]
                for s in range(128):
                    rb = gather(pub_d, s, idx); rbv = _RB(rb)
                    K.op("dve", lambda e: e.scalar_tensor_tensor(out=junkf[:], in0=u2[:], scalar=1.0, in1=rbv[:], op0=ALU.mult, op1=ALU.mult,
                                                                 accum_out=dots[:, s:s + 1]), [u2, rb], [junkf, dots])
                K.op("act", lambda e: e.activation(out=wsl[:], in_=dots[:], func=AF.Gelu), [dots], [wsl])
                K.op("dve", lambda e: e.tensor_tensor(out=wsl[:], in0=wsl[:], in1=gate[:].rearrange("p h k -> p (h k)"), op=ALU.mult), [wsl, gate], [wsl])
                gen = front(c + 1) if c + 1 < NOWN else None
                for s in range(128):
                    rb = gather(pvb_d, s, idx); rbv = _RB(rb)
                    tb = tmpr[s % 3]
                    K.op("act", lambda e: e.activation(out=tb[:], in_=rbv[:], func=AF.Copy, scale=wsl[:, s:s + 1]), [rb, wsl], [tb])
                    for nh in range(2):
                        K.op("pe", lambda e: e.matmul(P[6 + nh][:, :], lhsT=identb[:], rhs=tb[:, nh * 512:(nh + 1) * 512],
                                                      start=(s == 0), stop=(s == 127)), [identb, tb], [P[6 + nh]])
                    gen = drain(gen, 2)
                gen = drain(gen)
                for nh in range(2):
                    K.op("dve", lambda e: e.tensor_tensor(out=acc[:, nh * 512:(nh + 1) * 512], in0=P[6 + nh][:, :], in1=G2[:, nh * 512:(nh + 1) * 512],
                                                          op=ALU.mult), [P[6 + nh], G2], [acc])
                K.op("pool", lambda e: e.tensor_tensor(out=acc[:], in0=acc[:], in1=h_[:], op=ALU.add), [acc, h_], [acc])
                rmsnorm_mod(acc, yt, NF, None, (junk, ssq, rstd))
                K.dma("sp", lambda e: e.dma_start(out=y[c * 128:(c + 1) * 128, :], in_=yt[:]), y, [yt])
            K.finish([y])
    return nc


SEQ_FULL = 16384
ONLY = None
DBG = None
_cache = {}


def _consts():
    ident = np.eye(128, dtype=np.float32)
    tri = np.triu(np.ones((128, 128), np.float32))
    sel = np.zeros((4, 4, 128), np.float32)
    for h in range(4):
        sel[h, h, :] = 1.0
    i4 = np.eye(4, dtype=np.float32)
    iota16 = np.tile(np.arange(16, dtype=np.float32)[None, :], (128, 1))
    qi = np.arange(128)[:, None]; ki = np.arange(256)[None, :]
    rel = qi + 128 - ki
    ok = (rel >= 0) & (rel < 128)
    maskN = np.where(ok, 0.0, NEG).astype(np.float32)
    mask0 = np.where(ok & (ki >= 128), 0.0, NEG).astype(np.float32)
    return dict(ident=ident, tri=tri, sel=sel, i4=i4, iota16=iota16, maskN=maskN, mask0=mask0)


def run(inputs, S, ncore_per_seq, NOWN, stage=2):
    x = np.asarray(inputs["x"], np.float32)
    B = x.shape[0]
    NPRE = (ncore_per_seq - 1) * NOWN
    key = (NPRE, NOWN, stage)
    if key not in _cache:
        _cache[key] = build(NPRE, NOWN, stage)
    nc = _cache[key]
    f = lambda k: np.ascontiguousarray(np.asarray(inputs[k], np.float32))
    cst = _consts()
    half = 32
    inv_freq = (np.float32(10000.0) ** (-np.arange(half, dtype=np.float32) * np.float32(2.0) / np.float32(64))).astype(np.float32)
    shared = dict(
        w_ada=f("w_ada")[0], b_ada=f("b_ada")[0], norm1_w=f("norm1_w")[0], norm2_w=f("norm2_w")[0], norm_f_w=f("norm_f_w"),
        w_in=f("w_in")[0],
        conv_w=np.ascontiguousarray(f("conv_w")[0].reshape(4, 8, 128).transpose(2, 1, 0)),
        att_sinks=f("att_sinks")[0], i_bias=f("i_bias")[0].reshape(4, 1), f_bias=f("f_bias")[0].reshape(4, 1),
        mlstm_norm_w=f("mlstm_norm_w")[0], w_att=f("w_att_branch")[0], w_ml=f("w_mlstm_branch")[0], w_out=f("w_out")[0],
        wq=f("peer_w_query")[0], subkT=np.ascontiguousarray(f("peer_sub_keys")[0].transpose(2, 0, 1)),
        pu=f("peer_u")[0], pv=f("peer_v")[0],
        ident=cst["ident"], tri=cst["tri"], sel=cst["sel"], i4=cst["i4"], iota16=cst["iota16"], maskN=cst["maskN"],
    )
    c = f("c")
    in_maps = []
    T = NOWN * 128
    for b in range(B):
        for j in range(ncore_per_seq):
            start = j * T
            xo = np.ascontiguousarray(x[b, start:start + T])
            NP1 = max(NPRE, 1)
            xp = np.zeros((NP1 * 128, D), np.float32)
            valid = np.zeros((128, NP1), np.float32)
            if NPRE > 0 and start > 0:
                xp[NPRE * 128 - start:] = x[b, :start]
                valid[:, NPRE - start // 128:] = 1.0
            pos = (np.arange(start - 128, start + T, dtype=np.float32))
            ang = (pos[:, None] * inv_freq[None, :]).astype(np.float32)
            m = dict(shared)
            m.update(xo=xo, xp=xp, valid=valid, mask0=(cst["mask0"] if j == 0 else cst["maskN"]),
                     cosd=np.cos(ang).astype(np.float32), sind=np.sin(ang).astype(np.float32),
                     cT=np.ascontiguousarray(c[b].reshape(8, 128).T))
            in_maps.append(m)
    n = len(in_maps)
    res = run_bass_kernel_spmd(nc, in_maps, core_ids=list(range(n)))
    out = np.zeros((B, S, D), np.float32)
    k = 0
    for b in range(B):
        for j in range(ncore_per_seq):
            out[b, j * T:(j + 1) * T] = res.results[k]["y"]
            k += 1
    return out


def kernel(**inputs):
    return run(inputs, SEQ_FULL, 4, 32)
```

```python
import numpy as np
from contextlib import ExitStack
import concourse.bass as bass
import concourse.mybir as mybir
from concourse.bass_utils import run_bass_kernel_spmd

F32 = mybir.dt.float32
BF16 = mybir.dt.bfloat16
I32 = mybir.dt.int32
U32 = mybir.dt.uint32
AF = mybir.ActivationFunctionType
ALU = mybir.AluOpType
AX = mybir.AxisListType

D = 1024
NPROJ = 4872
EPS = 1e-6
NEG = -30000.0


class Buf:
    def __init__(self, t, name):
        self.t = t
        self.name = name
        self.w = None
        self.r = []
        self.dsem = None
        self.dcnt = 0

    def __getitem__(self, k):
        return self.t[k]


class Eng:
    def __init__(self, e, sem, name, is_pe=False):
        self.e, self.sem, self.name, self.cnt, self.is_pe = e, sem, name, 0, is_pe
        self.seen = {}


class Ctx:
    def __init__(self, nc, es):
        self.nc, self.es = nc, es
        self.engs = {}
        for nm, e in (("pe", nc.tensor), ("dve", nc.vector), ("act", nc.scalar),
                      ("pool", nc.gpsimd), ("sp", nc.sync)):
            sem = es.enter_context(nc.semaphore("s_" + nm))
            self.engs[nm] = Eng(e, sem, nm, is_pe=(nm == "pe"))
        self.bufs = []
        self.nsem = 0

    def sb(self, name, shape, dt, es=None):
        t = (es or self.es).enter_context(self.nc.sbuf_tensor("sb_" + name, shape, dt))
        b = Buf(t, name); self.bufs.append(b); return b

    def ps(self, name, shape, dt=F32, es=None):
        t = (es or self.es).enter_context(self.nc.psum_tensor("ps_" + name, shape, dt))
        b = Buf(t, name); self.bufs.append(b); return b

    def dram(self, name, shape, dt, kind="Internal"):
        t = self.nc.dram_tensor(name, shape, dt, kind=kind).ap()
        b = Buf(t, name); self.bufs.append(b); return b

    def view(self, buf, name):
        b = Buf(buf.t, name); self.bufs.append(b); return b

    def _wait(self, eng, deps):
        best = {}
        for (sem, val) in deps:
            k = id(sem)
            if k not in best or best[k][1] < val:
                best[k] = (sem, val)
        for k, (sem, val) in best.items():
            if eng.is_pe and sem is eng.sem:
                continue
            if eng.seen.get(k, 0) >= val:
                continue
            eng.e.wait_ge(sem, val)
            eng.seen[k] = val

    def _deps(self, reads, writes):
        deps = []
        for b in reads:
            if b.w: deps.append(b.w)
        for b in writes:
            if b.w: deps.append(b.w)
            deps.extend(b.r)
        return deps

    def op(self, en, fn, reads=(), writes=()):
        eng = self.engs[en]
        self._wait(eng, self._deps(reads, writes))
        ins = fn(eng.e)
        eng.cnt += 1
        ins.then_inc(eng.sem, 1)
        tok = (eng.sem, eng.cnt)
        for b in writes:
            b.w = tok; b.r = []
        for b in reads:
            if b not in writes:
                b.r.append(tok)

    def dma(self, en, fn, dst, srcs=(), waw=True):
        eng = self.engs[en]
        self._wait(eng, self._deps(srcs, [dst] if waw else []))
        if dst.dsem is None:
            dst.dsem = self.es.enter_context(self.nc.semaphore("d%d" % self.nsem))
            self.nsem += 1
        ins = fn(eng.e)
        dst.dcnt += 1
        ins.then_inc(dst.dsem, 16)
        tok = (dst.dsem, 16 * dst.dcnt)
        dst.w = tok; dst.r = []
        for b in srcs:
            b.r.append(tok)

    def barrier(self):
        toks = []
        for b in self.bufs:
            if b.w: toks.append(b.w)
            toks.extend(b.r)
        for e in self.engs.values():
            if e.cnt: toks.append((e.sem, e.cnt))
        for e in self.engs.values():
            self._wait(e, toks)
        for b in self.bufs:
            b.w = None; b.r = []

    def finish(self, bufs):
        eng = self.engs["sp"]
        self._wait(eng, [b.w for b in bufs if b.w])


def build(NPRE, NOWN, stage=2):
    nc = bass.Bass("TRN2", target_bir_lowering=False)
    es = ExitStack()
    with es:
        K = Ctx(nc, es)

        def din(name, shape, dt=F32):
            return K.dram(name, shape, dt, kind="ExternalInput")
        NP1 = max(NPRE, 1)
        xo = din("xo", [NOWN * 128, D]); xp = din("xp", [NP1 * 128, D])
        valid_d = din("valid", [128, NP1])
        mask0_d = din("mask0", [128, 256]); maskN_d = din("maskN", [128, 256])
        cos_d = din("cosd", [(NOWN + 1) * 128, 32]); sin_d = din("sind", [(NOWN + 1) * 128, 32])
        cT_d = din("cT", [128, 8])
        w_ada_d = din("w_ada", [D, 6 * D]); b_ada_d = din("b_ada", [6 * D])
        n1w_d = din("norm1_w", [D]); n2w_d = din("norm2_w", [D]); nfw_d = din("norm_f_w", [D])
        w_in_d = din("w_in", [D, NPROJ])
        cw_d = din("conv_w", [128, 8, 4])
        sink_d = din("att_sinks", [8]); ib_d = din("i_bias", [4, 1]); fb_d = din("f_bias", [4, 1])
        mnw_d = din("mlstm_norm_w", [512])
        wa_d = din("w_att", [512, D]); wb_d = din("w_ml", [512, D]); wo_d = din("w_out", [D, D])
        wq_d = din("wq", [D, 2048]); sk_d = din("subkT", [128, 2, 128])
        pu_d = din("pu", [16384, D]); pv_d = din("pv", [16384, D])
        ident_d = din("ident", [128, 128]); tri_d = din("tri", [128, 128])
        sel_d = din("sel", [4, 4, 128]); i4_d = din("i4", [4, 4]); iota_d = din("iota16", [128, 16])
        y = K.dram("y", [NOWN * 128, D], F32, kind="ExternalOutput")
        modbc_d = K.dram("modbc", [128, 6 * D], F32)
        hbuf_d = K.dram("hbuf", [NOWN * 128, D], F32)
        pub_d = K.dram("pub", [16384, D], BF16)
        pvb_d = K.dram("pvb", [16384, D], BF16)

        ident = K.sb("ident", [128, 128], F32); identb = K.sb("identb", [128, 128], BF16)
        K.dma("sp", lambda e: e.dma_start(out=ident[:], in_=ident_d[:]), ident, [ident_d])
        K.op("dve", lambda e: e.tensor_copy(out=identb[:], in_=ident[:]), [ident], [identb])
        ones = K.sb("ones", [128, 128], F32)
        K.op("dve", lambda e: e.memset(ones[:], 1.0), [], [ones])
        P = [K.ps("P%d" % i, [128, 512], F32) for i in range(8)]

        def bcast_load(dst, src_ap, src_buf):
            K.dma("sp", lambda e: e.dma_start(out=dst[:], in_=src_ap.partition_broadcast(128)), dst, [src_buf])

        with ExitStack() as e0:
            cT = K.sb("cT", [128, 8], F32, e0); cact = K.sb("cact", [128, 8], F32, e0)
            crep = K.sb("crep", [128, 8, 128], BF16, e0)
            K.dma("sp", lambda e: e.dma_start(out=cT[:], in_=cT_d[:]), cT, [cT_d])
            K.op("act", lambda e: e.activation(out=cact[:], in_=cT[:], func=AF.Silu), [cT], [cact])
            K.op("dve", lambda e: e.tensor_copy(out=crep[:], in_=cact[:].unsqueeze(2).to_broadcast([128, 8, 128])),
                 [cact], [crep])
            wst = [K.sb("wst%d" % i, [128, 8, 512], F32, e0) for i in range(2)]
            wsb = [K.sb("wsb%d" % i, [128, 8, 512], BF16, e0) for i in range(2)]
            badab = K.sb("badab", [128, 6 * D], F32, e0)
            bcast_load(badab, b_ada_d[:], b_ada_d)
            modsb = K.sb("modsb", [128, 6 * D], F32, e0)
            wv = w_ada_d.t.rearrange("(kc p) n -> p kc n", p=128)
            for j in range(12):
                ws = wst[j % 2]
                K.dma("sp", lambda e: e.dma_start(out=ws[:], in_=wv[:, :, j * 512:(j + 1) * 512]), ws, [w_ada_d])
                pj = P[j % 2]
                wb_ = wsb[j % 2]
                K.op("act" if j % 2 else "dve", (lambda e: e.activation(out=wb_[:], in_=ws[:], func=AF.Copy)) if j % 2 else
                     (lambda e: e.tensor_copy(out=wb_[:], in_=ws[:])), [ws], [wb_])
                for kc in range(8):
                    K.op("pe", lambda e: e.matmul(pj[:, :], lhsT=crep[:, kc, :], rhs=wb_[:, kc, :],
                                                  start=(kc == 0), stop=(kc == 7)), [crep, wb_], [pj])
                K.op("dve", lambda e: e.tensor_tensor(out=modsb[:, j * 512:(j + 1) * 512], in0=pj[:, :],
                                                      in1=badab[:, j * 512:(j + 1) * 512], op=ALU.add),
                     [pj, badab], [modsb])
            nwb = K.sb("nwb", [128, D], F32, e0)
            for (wd, slot) in ((n1w_d, 1), (n2w_d, 4)):
                bcast_load(nwb, wd[:], wd)
                K.op("dve", lambda e: e.scalar_tensor_tensor(out=modsb[:, slot * D:(slot + 1) * D],
                                                             in0=modsb[:, slot * D:(slot + 1) * D], scalar=1.0,
                                                             in1=nwb[:], op0=ALU.add, op1=ALU.mult),
                     [modsb, nwb], [modsb])
            K.dma("sp", lambda e: e.dma_start(out=modbc_d[:], in_=modsb[:]), modbc_d, [modsb])
        K.barrier()

        def load_bf16(dst, nrow_chunks, ncols, src_view, src_buf, stage):
            i = 0
            for kc in range(nrow_chunks):
                for c0 in range(0, ncols, 2048):
                    c1 = min(ncols, c0 + 2048)
                    st = stage[i % 2]; i += 1
                    pp = dst.t.shape[0]
                    K.dma("sp", lambda e: e.dma_start(out=st[0:pp, 0:c1 - c0], in_=src_view[:, kc, c0:c1]), st, [src_buf])
                    eng = "act" if (i % 2) else "dve"
                    if eng == "act":
                        K.op("act", lambda e: e.activation(out=dst[:, kc, c0:c1], in_=st[0:pp, 0:c1 - c0], func=AF.Copy), [st], [dst])
                    else:
                        K.op("dve", lambda e: e.tensor_copy(out=dst[:, kc, c0:c1], in_=st[0:pp, 0:c1 - c0]), [st], [dst])

        def rmsnorm_mod(xt, ut, A, B, sm):
            junk, ssq, rstd = sm
            K.op("act", lambda e: e.activation(out=junk[:].bitcast(BF16), in_=xt[:], func=AF.Square, accum_out=ssq[:, 0:1]), [xt], [junk, ssq])
            K.op("dve", lambda e: e.tensor_scalar(out=ssq[:, 1:2], in0=ssq[:, 0:1], scalar1=1.0 / D, scalar2=EPS,
                                                  op0=ALU.mult, op1=ALU.add), [ssq], [ssq])
            K.op("act", lambda e: e.activation(out=ssq[:, 2:3], in_=ssq[:, 1:2], func=AF.Ln), [ssq], [ssq])
            K.op("act", lambda e: e.activation(out=rstd[:], in_=ssq[:, 2:3], func=AF.Exp, scale=-0.5), [ssq], [rstd])
            K.op("dve", lambda e: e.scalar_tensor_tensor(out=ut[:], in0=xt[:], scalar=rstd[:, 0:1], in1=A[:],
                                                         op0=ALU.mult, op1=ALU.mult), [xt, rstd, A], [ut])
            if B is not None:
                K.op("pool", lambda e: e.tensor_tensor(out=ut[:], in0=ut[:], in1=B[:], op=ALU.add), [ut, B], [ut])

        def transpose_to(src, dstT, nk, pa, pb, dt32=True, srcbuf=None):
            srcbuf = srcbuf or src
            idn = ident if dt32 else identb
            for half in range((nk + 3) // 4):
                pb_ = (pa, pb)[half % 2]
                n = min(4, nk - half * 4)
                if dt32:
                    pv = pb_[:, :].rearrange("p (a b) -> p a b", b=128)
                else:
                    pv = pb_[:, :].bitcast(BF16).rearrange("p (a b) -> p a b", b=128)
                for q in range(n):
                    kc = half * 4 + q
                    K.op("pe", lambda e: e.transpose(out=pv[:, q, :], in_=src[:, kc * 128:(kc + 1) * 128], identity=idn[:]),
                         [srcbuf, idn], [pb_])
                K.op("act", lambda e: e.activation(out=dstT[:, half * 4:half * 4 + n, :], in_=pv[:, 0:n, :], func=AF.Copy),
                     [pb_], [dstT])

        with ExitStack() as e1:
            w_in = K.sb("w_in", [128, 8, NPROJ], BF16, e1)
            wa = K.sb("wa", [64, 8, D], BF16, e1)
            wb = K.sb("wb", [128, 4, D], BF16, e1)
            wo = K.sb("wo", [128, 8, D], BF16, e1)
            with ExitStack() as est:
                stage = [K.sb("stg%d" % i, [128, 2048], F32, est) for i in range(2)]
                load_bf16(w_in, 8, NPROJ, w_in_d.t.rearrange("(kc p) n -> p kc n", p=128), w_in_d, stage)
                load_bf16(wa, 8, D, wa_d.t.rearrange("(h p) n -> p h n", p=64), wa_d, stage)
                load_bf16(wb, 4, D, wb_d.t.rearrange("(kc p) n -> p kc n", p=128), wb_d, stage)
                load_bf16(wo, 8, D, wo_d.t.rearrange("(kc p) n -> p kc n", p=128), wo_d, stage)
                K.barrier()
            A1 = K.sb("A1", [128, D], F32, e1); B1 = K.sb("B1", [128, D], F32, e1); G1 = K.sb("G1", [128, D], F32, e1)
            K.dma("sp", lambda e: e.dma_start(out=A1[:], in_=modbc_d[:, 1 * D:2 * D]), A1, [modbc_d])
            K.dma("sp", lambda e: e.dma_start(out=B1[:], in_=modbc_d[:, 0:D]), B1, [modbc_d])
            K.dma("sp", lambda e: e.dma_start(out=G1[:], in_=modbc_d[:, 2 * D:3 * D]), G1, [modbc_d])
            cw = K.sb("cw", [128, 8, 4], F32, e1)
            K.dma("sp", lambda e: e.dma_start(out=cw[:], in_=cw_d[:]), cw, [cw_d])
            sinkb = K.sb("sinkb", [128, 8], F32, e1); bcast_load(sinkb, sink_d[:], sink_d)
            mnwb = K.sb("mnwb", [128, 512], F32, e1); bcast_load(mnwb, mnw_d[:], mnw_d)
            ibc = K.sb("ibc", [4, 1], F32, e1); fbc = K.sb("fbc", [4, 1], F32, e1)
            K.dma("sp", lambda e: e.dma_start(out=ibc[:], in_=ib_d[:]), ibc, [ib_d])
            K.dma("sp", lambda e: e.dma_start(out=fbc[:], in_=fb_d[:]), fbc, [fb_d])
            nfb = K.sb("nfb", [4, 1], F32, e1)
            K.op("dve", lambda e: e.tensor_scalar(out=nfb[:], in0=fbc[:], scalar1=-1.0, scalar2=None, op0=ALU.mult), [fbc], [nfb])
            valid = K.sb("valid", [128, NP1], F32, e1); pen = K.sb("pen", [128, NP1], F32, e1)
            K.dma("sp", lambda e: e.dma_start(out=valid[:], in_=valid_d[:]), valid, [valid_d])
            K.op("dve", lambda e: e.tensor_scalar(out=pen[:], in0=valid[:], scalar1=-1.0, scalar2=1e30, op0=ALU.add, op1=ALU.mult),
                 [valid], [pen])
            masks = [K.sb("mask0", [128, 256], F32, e1), K.sb("maskN", [128, 256], F32, e1)]
            K.dma("sp", lambda e: e.dma_start(out=masks[0][:], in_=mask0_d[:]), masks[0], [mask0_d])
            K.dma("sp", lambda e: e.dma_start(out=masks[1][:], in_=maskN_d[:]), masks[1], [maskN_d])
            tri = K.sb("tri", [128, 128], F32, e1)
            K.dma("sp", lambda e: e.dma_start(out=tri[:], in_=tri_d[:]), tri, [tri_d])
            sel = K.sb("sel", [4, 4, 128], F32, e1); i4 = K.sb("i4", [4, 4], F32, e1)
            K.dma("sp", lambda e: e.dma_start(out=sel[:], in_=sel_d[:]), sel, [sel_d])
            K.dma("sp", lambda e: e.dma_start(out=i4[:], in_=i4_d[:]), i4, [i4_d])

            xt = [K.sb("xt0", [128, D], F32, e1)] * 2
            ut = K.sb("ut", [128, D], F32, e1)
            junkF = K.sb("junk", [128, 512], F32, e1)
            ssq = K.sb("ssq", [128, 4], F32, e1); rstd = K.sb("rstd", [128, 1], F32, e1)
            uT = K.sb("uT", [128, 8, 128], BF16, e1)
            qk = K.sb("qk", [128, 640], F32, e1)
            vsb = [K.sb("vsb%d" % i, [128, 2, 64], BF16, e1) for i in range(2)]
            mv = K.sb("mv", [128, 4, 129], BF16, e1)
            K.op("dve", lambda e: e.memset(mv[:], 1.0), [], [mv])
            so = K.sb("so", [128, 512], F32, e1)
            sg = K.sb("sg", [128, 2048], BF16, e1)
            cin = K.sb("cin", [128, 8, 131], F32, e1)
            K.op("dve", lambda e: e.memset(cin[:], 0.0), [], [cin])
            qkT = K.sb("qkT", [128, 8, 128], BF16, e1)
            cs = K.sb("cs", [128, 32], F32, e1); sn = K.sb("sn", [128, 32], F32, e1)
            qkr = K.sb("qkr", [128, 10, 64], BF16, e1)
            qT = K.sb("qT", [64, 8, 128], BF16, e1)
            kT = [K.sb("kT%d" % i, [64, 2, 128], BF16, e1) for i in range(2)]
            s_sb = K.sb("s_sb", [128, 8, 256], F32, e1)
            p_bf = K.sb("p_bf", [128, 8, 256], BF16, e1)
            s_flat = s_sb[:].rearrange("p h k -> p (h k)")
            class _V:
                def __init__(s_, ap): s_.ap = ap
                def __getitem__(s_, k): return s_.ap[k]
            rtv = [_V(s_flat[:, i * 320:(i + 1) * 320].rearrange("p (h d) -> p h d", d=32)) for i in range(4)]
            m1 = _V(s_flat[:, 0:D]); m2 = _V(s_flat[:, D:2 * D])
            cy = _V(s_flat[:, 0:D].rearrange("p (j t) -> p j t", t=128))
            mg = _V(p_bf[:].rearrange("p h k -> p (h k)")[:, 0:D])
            sm8 = K.sb("sm8", [128, 40], F32, e1)
            pT = K.sb("pT", [128, 16, 128], BF16, e1)
            attT = K.sb("attT", [64, 8, 128], BF16, e1)
            g = {n: K.sb("g_" + n, [4, 128], F32, e1) for n in
                 ("i", "e", "lf", "b", "a", "ew", "imb", "r", "mm", "nmm", "wi", "mo", "em", "one")}
            K.op("dve", lambda e: e.memset(g["one"][:], 1.0), [], [g["one"]])
            gs = K.sb("gs", [4, 16], F32, e1)
            mst = K.sb("mst", [4, 1], F32, e1)
            K.op("dve", lambda e: e.memset(mst[:], 0.0), [], [mst])
            dg = K.sb("dg", [4, 4], F32, e1)
            tms = K.sb("tms", [128, 20], F32, e1)
            Cst = K.sb("Cst", [128, 4, 129], F32, e1)
            K.op("dve", lambda e: e.memset(Cst[:], 0.0), [], [Cst])
            Cbf = K.sb("Cbf", [128, 4, 129], BF16, e1)
            wk = K.sb("wk", [128, 4, 128], BF16, e1)
            Eb = K.sb("Eb", [128, 128], F32, e1)
            WT = K.sb("WT", [128, 128], BF16, e1)
            p1 = K.sb("p1", [128, 129], F32, e1); nd = K.sb("nd", [128, 129], F32, e1)
            hid = K.sb("hid", [128, 512], F32, e1); hsq = junkF
            hs = K.sb("hs", [128, 12], F32, e1)
            mls = K.sb("mls", [128, 512], BF16, e1)
            mlsT = K.sb("mlsT", [128, 4, 128], BF16, e1)
            mT = pT
            hh = ut

            def chunk(ci, prefix, kv, full):
                x = xt[ci % 2]
                srcd = xp if prefix else xo
                K.dma("sp", lambda e: e.dma_start(out=x[:], in_=srcd[ci * 128:(ci + 1) * 128, :]), x, [srcd])
                rmsnorm_mod(x, ut, A1, B1, (junkF, ssq, rstd))
                def dump(name, buf, ap, w):
                    if full and DBG == name:
                        K.dma("pool", lambda e: e.dma_start(out=y[ci * 128:(ci + 1) * 128, 0:w], in_=ap), y, [buf])
                dump("u", ut, ut[:], D)
                if DBG == "AB":
                    DBGs = "AB"
                    K.dma("pool", lambda e: e.dma_start(out=y[ci * 128:(ci + 1) * 128, :], in_=(A1 if ci == 0 else B1)[:]), y, [A1, B1]) if full else None
                transpose_to(ut, uT, 8, P[0], P[1])
                par = (ci % 2) if full else 1
                if kv and not full:
                    par = 1
                def tm_group(pb, c0, c1):
                    for kc in range(8):
                        K.op("pe", lambda e: e.matmul(pb[:, 0:c1 - c0], lhsT=uT[:, kc, :], rhs=w_in[:, kc, c0:c1],
                                                      start=(kc == 0), stop=(kc == 7)), [uT, w_in], [pb])
                if kv:
                    tm_group(P[2], 0, 512); tm_group(P[3], 512, 768)
                    K.op("act", lambda e: e.activation(out=qk[:, 0:512], in_=P[2][:, :], func=AF.Copy), [P[2]], [qk])
                    K.op("act", lambda e: e.activation(out=qk[:, 512:640], in_=P[3][:, 0:128], func=AF.Copy), [P[3]], [qk])
                    vdst = vsb[par]
                    K.op("dve", lambda e: e.tensor_copy(out=vdst[:].rearrange("p a b -> p (a b)"), in_=P[3][:, 128:256]),
                         [P[3]], [vdst])
                tm_group(P[4], 1792, 2304)
                K.op("act", lambda e: e.activation(out=mv[:, :, 0:128], in_=P[4][:, :].rearrange("p (a b) -> p a b", b=128),
                                                   func=AF.Copy), [P[4]], [mv])
                if full:
                    tm_group(P[5], 2304, 2816)
                    K.op("act", lambda e: e.activation(out=so[:], in_=P[5][:, :], func=AF.Sigmoid), [P[5]], [so])
                    for j in range(4):
                        pb = P[2 + (j % 2)]
                        tm_group(pb, 2824 + j * 512, 2824 + (j + 1) * 512)
                        K.op("act", lambda e: e.activation(out=sg[:, j * 512:(j + 1) * 512], in_=pb[:, :], func=AF.Sigmoid),
                             [pb], [sg])
                vcol = valid[:, ci:ci + 1] if prefix else ones[:, 0:1]
                vb = valid if prefix else ones
                j0 = 0 if (full or kv) else 4
                for j in range(j0, 8):
                    pb = P[6 + (j // 4) % 2]
                    q = j % 4
                    for kc in range(8):
                        K.op("pe", lambda e: e.matmul(pb[:, q * 128:(q + 1) * 128], lhsT=w_in[:, kc, 768 + j * 128:768 + (j + 1) * 128],
                                                      rhs=uT[:, kc, :], start=(kc == 0), stop=(kc == 7)), [uT, w_in], [pb])
                    K.op("dve", lambda e: e.tensor_scalar(out=cin[:, j, 3:131], in0=pb[:, q * 128:(q + 1) * 128],
                                                          scalar1=vcol, scalar2=None, op0=ALU.mult), [pb, vb], [cin])
                pgi = P[0]; pgf = P[1]
                for kc in range(8):
                    K.op("pe", lambda e: e.matmul(pgi[0:4, 0:128], lhsT=w_in[:, kc, 2816:2820], rhs=uT[:, kc, :],
                                                  start=(kc == 0), stop=(kc == 7)), [uT, w_in], [pgi])
                for kc in range(8):
                    K.op("pe", lambda e: e.matmul(pgf[0:4, 0:128], lhsT=w_in[:, kc, 2820:2824], rhs=uT[:, kc, :],
                                                  start=(kc == 0), stop=(kc == 7)), [uT, w_in], [pgf])
                for j in range(j0, 8):
                    K.op("dve", lambda e: e.tensor_scalar(out=cy[:, j, :], in0=cin[:, j, 0:128], scalar1=cw[:, j, 0:1],
                                                          scalar2=None, op0=ALU.mult), [cin, cw], [s_sb])
                    for t in range(1, 4):
                        K.op("dve", lambda e: e.scalar_tensor_tensor(out=cy[:, j, :], in0=cin[:, j, t:t + 128],
                                                                     scalar=cw[:, j, t:t + 1], in1=cy[:, j, :],
                                                                     op0=ALU.mult, op1=ALU.add), [cin, cw, s_sb], [s_sb])
                K.op("pool", lambda e: e.tensor_copy(out=cin[:, j0:8, 0:3], in_=cin[:, j0:8, 128:131]), [cin], [cin])
                K.op("act", lambda e: e.activation(out=qkT[:, j0:8, :], in_=cy[:, j0:8, :], func=AF.Silu), [s_sb], [qkT])
                K.op("pool", lambda e: e.tensor_scalar(out=qkT[:, 4:8, :], in0=qkT[:, 4:8, :], scalar1=128.0 ** -0.5,
                                                       scalar2=None, op0=ALU.mult), [qkT], [qkT])
                gi, ge, lf, gb, ga_, ew, imb, gr, mm, nmm, wi, gmo, em, gone = (g[n] for n in
                    ("i", "e", "lf", "b", "a", "ew", "imb", "r", "mm", "nmm", "wi", "mo", "em", "one"))
                if prefix:
                    K.op("dve", lambda e: e.tensor_scalar(out=gi[:], in0=pgi[0:4, 0:128], scalar1=ibc[:, 0:1], scalar2=valid[0:4, ci:ci + 1],
                                                          op0=ALU.add, op1=ALU.mult), [pgi, ibc, valid], [gi])
                    K.op("dve", lambda e: e.tensor_scalar(out=gi[:], in0=gi[:], scalar1=pen[0:4, ci:ci + 1], scalar2=None,
                                                          op0=ALU.add), [gi, pen], [gi])
                else:
                    K.op("dve", lambda e: e.tensor_scalar(out=gi[:], in0=pgi[0:4, 0:128], scalar1=ibc[:, 0:1], scalar2=None,
                                                          op0=ALU.add), [pgi, ibc], [gi])
                K.op("act", lambda e: e.activation(out=ge[:], in_=pgf[0:4, 0:128], func=AF.Exp, bias=nfb[:, 0:1], scale=-1.0),
                     [pgf, nfb], [ge])
                K.op("act", lambda e: e.activation(out=ge[:], in_=ge[:], func=AF.Ln, bias=1.0), [ge], [ge])
                v4 = valid[0:4, ci:ci + 1] if prefix else ones[0:4, 0:1]
                K.op("dve", lambda e: e.tensor_scalar(out=lf[:], in0=ge[:], scalar1=v4, scalar2=-1.0, op0=ALU.mult, op1=ALU.mult),
                     [ge, vb], [lf])
                K.op("dve", lambda e: e.tensor_tensor_scan(out=gb[:], data0=gone[:], data1=lf[:], initial=0.0,
                                                           op0=ALU.mult, op1=ALU.add), [gone, lf], [gb])
                K.op("dve", lambda e: e.tensor_tensor(out=imb[:], in0=gi[:], in1=gb[:], op=ALU.subtract), [gi, gb], [imb])
                K.op("dve", lambda e: e.tensor_scalar(out=ga_[:], in0=imb[:], scalar1=gb[:, 127:128], scalar2=None, op0=ALU.add),
                     [imb, gb], [ga_])
                K.op("dve", lambda e: e.tensor_reduce(out=gs[:, 0:1], in_=ga_[:], axis=AX.X, op=ALU.max), [ga_], [gs])
                K.op("dve", lambda e: e.tensor_tensor(out=gs[:, 1:2], in0=gb[:, 127:128], in1=mst[:], op=ALU.add), [gb, mst], [gs])
                K.op("dve", lambda e: e.tensor_tensor(out=gs[:, 2:3], in0=gs[:, 1:2], in1=gs[:, 0:1], op=ALU.max), [gs], [gs])
                K.op("dve", lambda e: e.tensor_scalar(out=gs[:, 3:4], in0=gs[:, 2:3], scalar1=-1.0, scalar2=None, op0=ALU.mult), [gs], [gs])
                K.op("dve", lambda e: e.tensor_tensor(out=gs[:, 4:5], in0=gs[:, 1:2], in1=gs[:, 2:3], op=ALU.subtract), [gs], [gs])
                K.op("act", lambda e: e.activation(out=gs[:, 5:6], in_=gs[:, 4:5], func=AF.Exp), [gs], [gs])
                K.op("act", lambda e: e.activation(out=ew[:], in_=ga_[:], func=AF.Exp, bias=gs[:, 3:4]), [ga_, gs], [ew])
                K.op("dve", lambda e: e.tensor_scalar(out=dg[:], in0=i4[:], scalar1=gs[:, 5:6], scalar2=None, op0=ALU.mult), [i4, gs], [dg])
                ptm = P[0]
                rows = [ew]
                if full:
                    K.op("dve", lambda e: e.tensor_tensor_scan(out=gr[:], data0=gone[:], data1=imb[:], initial=-1e30,
                                                               op0=ALU.mult, op1=ALU.max), [gone, imb], [gr])
                    K.op("dve", lambda e: e.tensor_scalar(out=mm[:], in0=gr[:], scalar1=mst[:, 0:1], scalar2=None, op0=ALU.max), [gr, mst], [mm])
                    K.op("dve", lambda e: e.tensor_scalar(out=nmm[:], in0=mm[:], scalar1=-1.0, scalar2=None, op0=ALU.mult), [mm], [nmm])
                    K.op("act", lambda e: e.activation(out=wi[:], in_=mm[:], func=AF.Exp, bias=mst[:, 0:1], scale=-1.0), [mm, mst], [wi])
                    K.op("dve", lambda e: e.tensor_tensor(out=gmo[:], in0=gb[:], in1=mm[:], op=ALU.add), [gb, mm], [gmo])
                    K.op("act", lambda e: e.activation(out=em[:], in_=gmo[:], func=AF.Exp, scale=-1.0), [gmo], [em])
                    rows = [ew, imb, wi, em]
                for r_i, rr in enumerate(rows):
                    K.op("pe", lambda e: e.transpose(out=ptm[:, r_i * 4:(r_i + 1) * 4], in_=rr[:, :], identity=ident[0:4, 0:4]),
                         [rr, ident], [ptm])
                K.op("pe", lambda e: e.matmul(ptm[:, 16:20], lhsT=ones[0:4, :], rhs=dg[:, :], start=True, stop=True), [ones, dg], [ptm])
                K.op("act", lambda e: e.activation(out=tms[:], in_=ptm[:, 0:20], func=AF.Copy), [ptm], [tms])

                if kv:
                    K.dma("sp", lambda e: e.dma_start(out=cs[:], in_=cos_d[(ci + 1 if full else 0) * 128:(ci + 2 if full else 1) * 128, :]), cs, [cos_d])
                    K.dma("sp", lambda e: e.dma_start(out=sn[:], in_=sin_d[(ci + 1 if full else 0) * 128:(ci + 2 if full else 1) * 128, :]), sn, [sin_d])
                    dump("qk", qk, qk[:], 640)
                    q4 = qk[:].rearrange("p (h two d) -> p h two d", two=2, d=32)
                    x1 = q4[:, :, 0, :]; x2 = q4[:, :, 1, :]
                    cb = cs[:].unsqueeze(1).to_broadcast([128, 10, 32]); sbb = sn[:].unsqueeze(1).to_broadcast([128, 10, 32])
                    qr4 = qkr[:].rearrange("p h (two d) -> p h two d", two=2)
                    K.op("dve", lambda e: e.tensor_tensor(out=rtv[0][:], in0=x1, in1=cb, op=ALU.mult), [qk, cs], [s_sb])
                    K.op("pool", lambda e: e.tensor_tensor(out=rtv[1][:], in0=x2, in1=sbb, op=ALU.mult), [qk, sn], [s_sb])
                    K.op("dve", lambda e: e.tensor_tensor(out=qr4[:, :, 0, :], in0=rtv[0][:], in1=rtv[1][:], op=ALU.subtract), [s_sb], [qkr])
                    K.op("pool", lambda e: e.tensor_tensor(out=rtv[2][:], in0=x2, in1=cb, op=ALU.mult), [qk, cs], [s_sb])
                    K.op("dve", lambda e: e.tensor_tensor(out=rtv[3][:], in0=x1, in1=sbb, op=ALU.mult), [qk, sn], [s_sb])
                    K.op("dve", lambda e: e.tensor_tensor(out=qr4[:, :, 1, :], in0=rtv[2][:], in1=rtv[3][:], op=ALU.add), [s_sb], [qkr])
                    pq = P[2]; pk = P[3]
                    pqv = pq[0:64, :].bitcast(BF16).rearrange("p (a b) -> p a b", b=128)
                    pkv = pk[0:64, :].bitcast(BF16).rearrange("p (a b) -> p a b", b=128)
                    h0 = 0 if full else 8
                    for h in range(h0, 10):
                        dst = pqv[:, h, :] if h < 8 else pkv[:, h - 8, :]
                        pbuf = pq if h < 8 else pk
                        K.op("pe", lambda e: e.transpose(out=dst, in_=qkr[:, h, :], identity=identb[:]), [qkr, identb], [pbuf])
                    if full:
                        K.op("act", lambda e: e.activation(out=qT[:], in_=pqv[:, 0:8, :], func=AF.Copy), [pq], [qT])
                    kdst = kT[par]
                    K.op("act", lambda e: e.activation(out=kdst[:], in_=pkv[:, 0:2, :], func=AF.Copy), [pk], [kdst])

                if full:
                    kprev, kcur = kT[1 - par], kT[par]
                    vprev, vcur = vsb[1 - par], vsb[par]
                    mk_ = masks[0] if ci == 0 else masks[1]
                    for h in range(8):
                        pb = P[2 + h // 2]
                        o = (h % 2) * 256
                        K.op("pe", lambda e: e.matmul(pb[:, o:o + 128], lhsT=qT[:, h, :], rhs=kprev[:, h // 4, :], start=True, stop=True),
                             [qT, kprev], [pb])
                        K.op("pe", lambda e: e.matmul(pb[:, o + 128:o + 256], lhsT=qT[:, h, :], rhs=kcur[:, h // 4, :], start=True, stop=True),
                             [qT, kcur], [pb])
                    for q in range(4):
                        pb = P[2 + q]
                        K.op("dve", lambda e: e.scalar_tensor_tensor(out=s_sb[:, 2 * q:2 * q + 2, :],
                                                                     in0=pb[:, :].rearrange("p (a b) -> p a b", b=256), scalar=0.125,
                                                                     in1=mk_[:].unsqueeze(1).to_broadcast([128, 2, 256]),
                                                                     op0=ALU.mult, op1=ALU.add), [pb, mk_], [s_sb])
                    K.op("dve", lambda e: e.tensor_reduce(out=sm8[:, 0:8], in_=s_sb[:], axis=AX.X, op=ALU.max), [s_sb], [sm8])
                    K.op("dve", lambda e: e.tensor_tensor(out=sm8[:, 0:8], in0=sm8[:, 0:8], in1=sinkb[:], op=ALU.max), [sm8, sinkb], [sm8])
                    K.op("dve", lambda e: e.tensor_tensor(out=s_sb[:], in0=s_sb[:], in1=sm8[:, 0:8].unsqueeze(2).to_broadcast([128, 8, 256]),
                                                          op=ALU.subtract), [s_sb, sm8], [s_sb])
                    K.op("act", lambda e: e.activation(out=s_sb[:], in_=s_sb[:], func=AF.Exp), [s_sb], [s_sb])
                    K.op("dve", lambda e: e.tensor_reduce(out=sm8[:, 16:24], in_=s_sb[:], axis=AX.X, op=ALU.add), [s_sb], [sm8])
                    K.op("dve", lambda e: e.tensor_tensor(out=sm8[:, 8:16], in0=sinkb[:], in1=sm8[:, 0:8], op=ALU.subtract), [sm8, sinkb], [sm8])
                    K.op("act", lambda e: e.activation(out=sm8[:, 24:32], in_=sm8[:, 8:16], func=AF.Exp), [sm8], [sm8])
                    K.op("dve", lambda e: e.tensor_tensor(out=sm8[:, 16:24], in0=sm8[:, 16:24], in1=sm8[:, 24:32], op=ALU.add), [sm8], [sm8])
                    K.op("dve", lambda e: e.reciprocal(out=sm8[:, 32:40], in_=sm8[:, 16:24]), [sm8], [sm8])
                    K.op("dve", lambda e: e.tensor_tensor(out=p_bf[:], in0=s_sb[:], in1=sm8[:, 32:40].unsqueeze(2).to_broadcast([128, 8, 256]),
                                                          op=ALU.mult), [s_sb, sm8], [p_bf])
                    for hh_ in range(2):
                        pb = P[2 + hh_]
                        pvw = pb[:, :].bitcast(BF16).rearrange("p (a b) -> p a b", b=128)
                        for h4 in range(4):
                            h = hh_ * 4 + h4
                            for half in range(2):
                                K.op("pe", lambda e: e.transpose(out=pvw[:, h4 * 2 + half, :], in_=p_bf[:, h, half * 128:(half + 1) * 128],
                                                                 identity=identb[:]), [p_bf, identb], [pb])
                        K.op("act", lambda e: e.activation(out=pT[:, hh_ * 8:(hh_ + 1) * 8, :], in_=pvw[:, 0:8, :], func=AF.Copy), [pb], [pT])
                    for hh_ in range(2):
                        pb = P[4 + hh_]
                        for h4 in range(4):
                            h = hh_ * 4 + h4
                            K.op("pe", lambda e: e.matmul(pb[0:64, h4 * 128:(h4 + 1) * 128], lhsT=vprev[:, h // 4, :], rhs=pT[:, 2 * h, :],
                                                          start=True, stop=False), [vprev, pT], [pb])
                            K.op("pe", lambda e: e.matmul(pb[0:64, h4 * 128:(h4 + 1) * 128], lhsT=vcur[:, h // 4, :], rhs=pT[:, 2 * h + 1, :],
                                                          start=False, stop=True), [vcur, pT], [pb])
                        K.op("act", lambda e: e.activation(out=attT[:, hh_ * 4:(hh_ + 1) * 4, :],
                                                           in_=pb[0:64, :].rearrange("p (a b) -> p a b", b=128), func=AF.Copy), [pb], [attT])

                    K.op("act", lambda e: e.activation(out=Cbf[:], in_=Cst[:], func=AF.Copy), [Cst], [Cbf])
                    for h in range(4):
                        pbc = P[6]; pst = P[7]
                        K.op("pe", lambda e: e.matmul(pbc[:, 0:128], lhsT=sel[:, h, :], rhs=nmm[:, :], start=True, stop=True), [sel, nmm], [pbc])
                        K.op("act", lambda e: e.activation(out=Eb[:], in_=pbc[:, 0:128], func=AF.Exp, bias=tms[:, 4 + h:5 + h]), [pbc, tms], [Eb])
                        K.op("pool", lambda e: e.tensor_tensor(out=Eb[:], in0=Eb[:], in1=tri[:], op=ALU.mult), [Eb, tri], [Eb])
                        K.op("pe", lambda e: e.matmul(pst[:, 0:128], lhsT=qkT[:, 4 + h, :], rhs=qkT[:, h, :], start=True, stop=True), [qkT], [pst])
                        K.op("dve", lambda e: e.tensor_tensor(out=WT[:], in0=pst[:, 0:128], in1=Eb[:], op=ALU.mult), [pst, Eb], [WT])
                        K.op("pe", lambda e: e.matmul(pbc[:, 128:257], lhsT=WT[:], rhs=mv[:, h, :], start=True, stop=True), [WT, mv], [pbc])
                        K.op("pe", lambda e: e.matmul(pst[:, 128:257], lhsT=qkT[:, h, :], rhs=Cbf[:, h, :], start=True, stop=True), [qkT, Cbf], [pst])
                        K.op("act", lambda e: e.activation(out=p1[:], in_=pbc[:, 128:257], func=AF.Copy), [pbc], [p1])
                        K.op("dve", lambda e: e.scalar_tensor_tensor(out=nd[:], in0=pst[:, 128:257], scalar=tms[:, 8 + h:9 + h], in1=p1[:],
                                                                     op0=ALU.mult, op1=ALU.add), [pst, tms, p1], [nd])
                        K.op("dve", lambda e: e.scalar_tensor_tensor(out=hs[:, 10:11], in0=nd[:, 128:129], scalar=-1.0, in1=nd[:, 128:129],
                                                                     op0=ALU.mult, op1=ALU.max), [nd], [hs])
                        K.op("dve", lambda e: e.tensor_scalar(out=hs[:, 8:9], in0=hs[:, 10:11], scalar1=tms[:, 12 + h:13 + h], scalar2=None,
                                                              op0=ALU.max), [hs, tms], [hs])
                        K.op("dve", lambda e: e.reciprocal(out=hs[:, 9:10], in_=hs[:, 8:9]), [hs], [hs])
                        K.op("dve", lambda e: e.scalar_tensor_tensor(out=hid[:, h * 128:(h + 1) * 128], in0=nd[:, 0:128], scalar=hs[:, 9:10],
                                                                     in1=so[:, h * 128:(h + 1) * 128], op0=ALU.mult, op1=ALU.mult), [nd, hs, so], [hid])
                    dump("hid", hid, hid[:], 512)
                    K.op("pool", lambda e: e.tensor_tensor(out=hsq[:], in0=hid[:], in1=hid[:], op=ALU.mult), [hid], [hsq])
                    K.op("dve", lambda e: e.tensor_reduce(out=hs[:, 0:4], in_=hsq[:].rearrange("p (a b) -> p a b", b=128), axis=AX.X, op=ALU.add),
                         [hsq], [hs])
                    K.op("dve", lambda e: e.tensor_scalar(out=hs[:, 0:4], in0=hs[:, 0:4], scalar1=1.0 / 128, scalar2=EPS, op0=ALU.mult, op1=ALU.add), [hs], [hs])
                    K.op("act", lambda e: e.activation(out=hs[:, 0:4], in_=hs[:, 0:4], func=AF.Sqrt), [hs], [hs])
                    K.op("dve", lambda e: e.reciprocal(out=hs[:, 4:8], in_=hs[:, 0:4]), [hs], [hs])
                    for h in range(4):
                        K.op("dve", lambda e: e.scalar_tensor_tensor(out=mls[:, h * 128:(h + 1) * 128], in0=hid[:, h * 128:(h + 1) * 128],
                                                                     scalar=hs[:, 4 + h:5 + h], in1=mnwb[:, h * 128:(h + 1) * 128],
                                                                     op0=ALU.mult, op1=ALU.mult), [hid, hs, mnwb], [mls])
                    transpose_to(mls, mlsT, 4, P[6], P[7], dt32=False)

                pkt = P[6]
                pktv = pkt[:, :].bitcast(BF16).rearrange("p (a b) -> p a b", b=128)
                for h in range(4):
                    K.op("pe", lambda e: e.transpose(out=pktv[:, h, :], in_=qkT[:, 4 + h, :], identity=identb[:]), [qkT, identb], [pkt])
                for h in range(4):
                    K.op("dve", lambda e: e.tensor_scalar(out=wk[:, h, :], in0=pktv[:, h, :], scalar1=tms[:, h:h + 1], scalar2=None, op0=ALU.mult),
                         [pkt, tms], [wk])
                pu_ = P[7]
                for h in range(4):
                    K.op("pe", lambda e: e.matmul(pu_[:, h * 129:(h + 1) * 129] if h < 3 else P[6][:, 0:129], lhsT=wk[:, h, :], rhs=mv[:, h, :],
                                                  start=True, stop=True), [wk, mv], [pu_ if h < 3 else P[6]])
                for h in range(4):
                    src = pu_[:, h * 129:(h + 1) * 129] if h < 3 else P[6][:, 0:129]
                    sb_ = pu_ if h < 3 else P[6]
                    K.op("dve", lambda e: e.scalar_tensor_tensor(out=Cst[:, h, :], in0=Cst[:, h, :], scalar=tms[:, 16 + h:17 + h], in1=src,
                                                                 op0=ALU.mult, op1=ALU.add), [Cst, tms, sb_], [Cst])
                K.op("dve", lambda e: e.tensor_copy(out=mst[:], in_=gs[:, 2:3]), [gs], [mst])

                if full:
                    for nh in range(2):
                        pa_ = P[2 + nh]; pb_ = P[4 + nh]
                        for h in range(8):
                            K.op("pe", lambda e: e.matmul(pa_[:, :], lhsT=attT[:, h, :], rhs=wa[:, h, nh * 512:(nh + 1) * 512],
                                                          start=(h == 0), stop=(h == 7)), [attT, wa], [pa_])
                        for kc in range(4):
                            K.op("pe", lambda e: e.matmul(pb_[:, :], lhsT=mlsT[:, kc, :], rhs=wb[:, kc, nh * 512:(nh + 1) * 512],
                                                          start=(kc == 0), stop=(kc == 3)), [mlsT, wb], [pb_])
                        K.op("dve", lambda e: e.tensor_tensor(out=m1[:, nh * 512:(nh + 1) * 512], in0=pa_[:, :], in1=sg[:, nh * 512:(nh + 1) * 512],
                                                              op=ALU.mult), [pa_, sg], [s_sb])
                        K.op("dve", lambda e: e.tensor_tensor(out=m2[:, nh * 512:(nh + 1) * 512], in0=pb_[:, :], in1=sg[:, D + nh * 512:D + (nh + 1) * 512],
                                                              op=ALU.mult), [pb_, sg], [s_sb])
                    K.op("pool", lambda e: e.tensor_tensor(out=mg[:], in0=(m2 if ONLY == "b" else m1)[:], in1=(m1 if ONLY == "a" else m2)[:], op=ALU.add), [s_sb], [p_bf])
                    transpose_to(mg, mT, 8, P[2], P[3], dt32=False, srcbuf=p_bf)
                    for nh in range(2):
                        po = P[4 + nh]
                        for kc in range(8):
                            K.op("pe", lambda e: e.matmul(po[:, :], lhsT=mT[:, kc, :], rhs=wo[:, kc, nh * 512:(nh + 1) * 512],
                                                          start=(kc == 0), stop=(kc == 7)), [mT, wo], [po])
                        K.op("dve", lambda e: e.tensor_tensor(out=m1[:, nh * 512:(nh + 1) * 512], in0=po[:, :], in1=G1[:, nh * 512:(nh + 1) * 512],
                                                              op=ALU.mult), [po, G1], [s_sb])
                    K.op("pool", lambda e: e.tensor_tensor(out=hh[:], in0=m1[:], in1=x[:], op=ALU.add), [s_sb, x], [hh])
                    K.dma("pool", lambda e: e.dma_start(out=hbuf_d[ci * 128:(ci + 1) * 128, :], in_=hh[:]), hbuf_d, [hh])

            class _V2:
                def __init__(s_, ap): s_.ap = ap
                def __getitem__(s_, k): return s_.ap[k]
            sgf = sg[:].bitcast(F32)
            pbf32 = p_bf[:].rearrange("p h k -> p (h k)").bitcast(F32)
            hidb = hid[:].bitcast(BF16)
            mlsf = mls[:].bitcast(F32)
            PX = [(xt[0], _V2(xt[0][:])), (s_sb, _V2(s_flat[:, D:2 * D]))]
            PU = [(ut, _V2(ut[:])), (p_bf, _V2(pbf32))]
            PUT = [(uT, _V2(uT[:])), (pT, _V2(pT[:, 0:8, :]))]
            PCIN = [(cin, _V2(cin[:, 4:8, :])), (sg, _V2(sgf[:, 0:524].rearrange("p (j t) -> p j t", t=131)))]
            PCY = [(s_sb, _V2(cy[:, 4:8, :])), (s_sb, _V2(cy[:, 0:4, :]))]
            PKT = [(qkT, _V2(qkT[:, 4:8, :])), (qkT, _V2(qkT[:, 0:4, :]))]
            PMV = [(mv, _V2(mv[:])), (hid, _V2(hidb[:, 0:516].rearrange("p (h e) -> p h e", e=129)))]
            PWK = [(wk, _V2(wk[:])), (qkr, _V2(qkr[:].rearrange("p h d -> p (h d)")[:, 0:512].rearrange("p (h d) -> p h d", d=128)))]
            g1n = ("i", "e", "lf", "b", "a", "ew", "imb")
            PG = [{n: (g[n], _V2(g[n][:])) for n in g1n},
                  {n: ((so, _V2(so[0:4, k * 128:(k + 1) * 128])) if k < 4 else (mls, _V2(mlsf[0:4, (k - 4) * 128:(k - 3) * 128])))
                   for k, n in enumerate(("i", "e", "lf", "b", "a", "ew"))}]
            PG[1]["imb"] = (qk, _V2(qk[0:4, 128:256]))
            PGS = [(gs, _V2(gs[:])), (qk, _V2(qk[0:4, 0:16]))]
            PDG = [(dg, _V2(dg[:])), (qk, _V2(qk[0:4, 16:20]))]
            PTMS = [(tms, _V2(tms[:])), (qk, _V2(qk[:, 32:52]))]
            PSQ = [((ssq, _V2(ssq[:])), (rstd, _V2(rstd[:]))), ((qk, _V2(qk[:, 60:64])), (qk, _V2(qk[:, 64:65])))]
            tEb = K.view(pT, "tEb")
            if NPRE > 1:
                K.op("dve", lambda e: e.memset(PMV[1][1][:], 1.0), [], [hid])
                K.op("dve", lambda e: e.memset(PCIN[1][1][:], 0.0), [], [sg])

            def pfA(ci, p):
                xb, xv = PX[p]; ub, uv = PU[p]; utb, utv = PUT[p]; cb_, cv = PCIN[p]; mb, mvv = PMV[p]
                (sqb, sqv), (rsb, rsv) = PSQ[p]
                Q0, Q1, Q2, Q3 = P[4 * p:4 * p + 4]
                K.dma("sp", lambda e: e.dma_start(out=xv[:], in_=xp[ci * 128:(ci + 1) * 128, :]), xb, [xp])
                K.op("act", lambda e: e.activation(out=junkF[:].bitcast(BF16), in_=xv[:], func=AF.Square, accum_out=sqv[:, 0:1]), [xb], [junkF, sqb]); yield
                K.op("dve", lambda e: e.tensor_scalar(out=sqv[:, 1:2], in0=sqv[:, 0:1], scalar1=1.0 / D, scalar2=EPS, op0=ALU.mult, op1=ALU.add), [sqb], [sqb]); yield
                K.op("act", lambda e: e.activation(out=sqv[:, 2:3], in_=sqv[:, 1:2], func=AF.Ln), [sqb], [sqb]); yield
                K.op("act", lambda e: e.activation(out=rsv[:], in_=sqv[:, 2:3], func=AF.Exp, scale=-0.5), [sqb], [rsb]); yield
                K.op("dve", lambda e: e.scalar_tensor_tensor(out=uv[:], in0=xv[:], scalar=rsv[:, 0:1], in1=A1[:], op0=ALU.mult, op1=ALU.mult), [xb, rsb, A1], [ub]); yield
                K.op("pool", lambda e: e.tensor_tensor(out=uv[:], in0=uv[:], in1=B1[:], op=ALU.add), [ub, B1], [ub]); yield
                for half in range(2):
                    pb_ = (Q0, Q1)[half]
                    pv = pb_[:, :].rearrange("p (a b) -> p a b", b=128)
                    for q in range(4):
                        kc = half * 4 + q
                        K.op("pe", lambda e: e.transpose(out=pv[:, q, :], in_=uv[:, kc * 128:(kc + 1) * 128], identity=ident[:]), [ub, ident], [pb_])
                    K.op("act", lambda e: e.activation(out=utv[:, half * 4:half * 4 + 4, :], in_=pv[:, 0:4, :], func=AF.Copy), [pb_], [utb]); yield
                for kc in range(8):
                    K.op("pe", lambda e: e.matmul(Q2[:, 0:512], lhsT=utv[:, kc, :], rhs=w_in[:, kc, 1792:2304], start=(kc == 0), stop=(kc == 7)), [utb, w_in], [Q2])
                K.op("act", lambda e: e.activation(out=mvv[:, :, 0:128], in_=Q2[:, :].rearrange("p (a b) -> p a b", b=128), func=AF.Copy), [Q2], [mb]); yield
                for j in range(4):
                    for kc in range(8):
                        K.op("pe", lambda e: e.matmul(Q3[:, j * 128:(j + 1) * 128], lhsT=w_in[:, kc, 1280 + j * 128:1280 + (j + 1) * 128], rhs=utv[:, kc, :],
                                                      start=(kc == 0), stop=(kc == 7)), [utb, w_in], [Q3])
                K.op("dve", lambda e: e.tensor_scalar(out=cv[:, :, 3:131], in0=Q3[:, :].rearrange("p (a b) -> p a b", b=128), scalar1=valid[:, ci:ci + 1],
                                                      scalar2=None, op0=ALU.mult), [Q3, valid], [cb_]); yield
                ob, ov = PCIN[1 - p]
                K.op("pool", lambda e: e.tensor_copy(out=ov[:, :, 0:3], in_=cv[:, :, 128:131]), [cb_], [ob]); yield
                for kc in range(8):
                    K.op("pe", lambda e: e.matmul(Q0[0:4, 0:128], lhsT=w_in[:, kc, 2816:2820], rhs=utv[:, kc, :], start=(kc == 0), stop=(kc == 7)), [utb, w_in], [Q0])
                for kc in range(8):
                    K.op("pe", lambda e: e.matmul(Q0[0:4, 128:256], lhsT=w_in[:, kc, 2820:2824], rhs=utv[:, kc, :], start=(kc == 0), stop=(kc == 7)), [utb, w_in], [Q0])
                yield

            def pfB(ci, p):
                cb_, cv = PCIN[p]; cyb, cyv = PCY[p]; kb, kv_ = PKT[p]; mb, mvv = PMV[p]; wkb, wkv = PWK[p]
                G = PG[p]; gsb, gsv = PGS[p]; dgb, dgv = PDG[p]; tmb, tmv = PTMS[p]
                Q0, Q1, Q2, Q3 = P[4 * p:4 * p + 4]
                for j in range(4):
                    K.op("dve", lambda e: e.tensor_scalar(out=cyv[:, j, :], in0=cv[:, j, 0:128], scalar1=cw[:, 4 + j, 0:1], scalar2=None, op0=ALU.mult), [cb_, cw], [cyb]); yield
                    for t in range(1, 4):
                        K.op("dve", lambda e: e.scalar_tensor_tensor(out=cyv[:, j, :], in0=cv[:, j, t:t + 128], scalar=cw[:, 4 + j, t:t + 1], in1=cyv[:, j, :],
                                                                     op0=ALU.mult, op1=ALU.add), [cb_, cw, cyb], [cyb]); yield
                tE = pT[:, 8:16, :].rearrange("p a b -> p (a b)").bitcast(F32).rearrange("p (j t) -> p j t", t=128)
                K.op("act", lambda e: e.activation(out=tE, in_=cyv[:], func=AF.Exp, scale=-1.0), [cyb], [tEb]); yield
                K.op("dve", lambda e: e.tensor_scalar(out=tE, in0=tE, scalar1=1.0, scalar2=None, op0=ALU.add), [tEb], [tEb]); yield
                K.op("dve", lambda e: e.reciprocal(out=tE, in_=tE), [tEb], [tEb]); yield
                K.op("dve", lambda e: e.scalar_tensor_tensor(out=kv_[:], in0=cyv[:], scalar=128.0 ** -0.5, in1=tE, op0=ALU.mult, op1=ALU.mult), [cyb, tEb], [kb]); yield
                (gib, giv), (geb, gev), (lfb, lfv), (gbb, gbv), (gab, gav), (ewb, ewv), (imbb, imbv) = (G[n] for n in g1n)
                K.op("dve", lambda e: e.tensor_scalar(out=giv[:], in0=Q0[0:4, 0:128], scalar1=ibc[:, 0:1], scalar2=valid[0:4, ci:ci + 1], op0=ALU.add, op1=ALU.mult), [Q0, ibc, valid], [gib]); yield
                K.op("dve", lambda e: e.tensor_scalar(out=giv[:], in0=giv[:], scalar1=pen[0:4, ci:ci + 1], scalar2=None, op0=ALU.add), [gib, pen], [gib]); yield
                K.op("act", lambda e: e.activation(out=gev[:], in_=Q0[0:4, 128:256], func=AF.Exp, bias=nfb[:, 0:1], scale=-1.0), [Q0, nfb], [geb]); yield
                K.op("act", lambda e: e.activation(out=gev[:], in_=gev[:], func=AF.Ln, bias=1.0), [geb], [geb]); yield
                K.op("dve", lambda e: e.tensor_scalar(out=lfv[:], in0=gev[:], scalar1=valid[0:4, ci:ci + 1], scalar2=-1.0, op0=ALU.mult, op1=ALU.mult), [geb, valid], [lfb]); yield
                K.op("dve", lambda e: e.tensor_tensor_scan(out=gbv[:], data0=g["one"][:], data1=lfv[:], initial=0.0, op0=ALU.mult, op1=ALU.add), [g["one"], lfb], [gbb]); yield
                K.op("dve", lambda e: e.tensor_tensor(out=imbv[:], in0=giv[:], in1=gbv[:], op=ALU.subtract), [gib, gbb], [imbb]); yield
                K.op("dve", lambda e: e.tensor_scalar(out=gav[:], in0=imbv[:], scalar1=gbv[:, 127:128], scalar2=None, op0=ALU.add), [imbb, gbb], [gab]); yield
                K.op("dve", lambda e: e.tensor_reduce(out=gsv[:, 0:1], in_=gav[:], axis=AX.X, op=ALU.max), [gab], [gsb]); yield
                K.op("dve", lambda e: e.tensor_tensor(out=gsv[:, 1:2], in0=gbv[:, 127:128], in1=mst[:], op=ALU.add), [gbb, mst], [gsb]); yield
                K.op("dve", lambda e: e.tensor_tensor(out=gsv[:, 2:3], in0=gsv[:, 1:2], in1=gsv[:, 0:1], op=ALU.max), [gsb], [gsb]); yield
                K.op("dve", lambda e: e.tensor_copy(out=mst[:], in_=gsv[:, 2:3]), [gsb], [mst]); yield
                K.op("dve", lambda e: e.tensor_scalar(out=gsv[:, 3:4], in0=gsv[:, 2:3], scalar1=-1.0, scalar2=None, op0=ALU.mult), [gsb], [gsb]); yield
                K.op("dve", lambda e: e.tensor_tensor(out=gsv[:, 4:5], in0=gsv[:, 1:2], in1=gsv[:, 2:3], op=ALU.subtract), [gsb], [gsb]); yield
                K.op("act", lambda e: e.activation(out=gsv[:, 5:6], in_=gsv[:, 4:5], func=AF.Exp), [gsb], [gsb]); yield
                K.op("act", lambda e: e.activation(out=ewv[:], in_=gav[:], func=AF.Exp, bias=gsv[:, 3:4]), [gab, gsb], [ewb]); yield
                K.op("dve", lambda e: e.tensor_scalar(out=dgv[:], in0=i4[:], scalar1=gsv[:, 5:6], scalar2=None, op0=ALU.mult), [i4, gsb], [dgb]); yield
                K.op("pe", lambda e: e.transpose(out=Q0[:, 256:260], in_=ewv[:, :], identity=ident[0:4, 0:4]), [ewb, ident], [Q0])
                K.op("pe", lambda e: e.matmul(Q0[:, 272:276], lhsT=ones[0:4, :], rhs=dgv[:, :], start=True, stop=True), [ones, dgb], [Q0])
                K.op("act", lambda e: e.activation(out=tmv[:, 0:20], in_=Q0[:, 256:276], func=AF.Copy), [Q0], [tmb]); yield
                pktv = Q1[:, :].bitcast(BF16).rearrange("p (a b) -> p a b", b=128)
                for h in range(4):
                    K.op("pe", lambda e: e.transpose(out=pktv[:, h, :], in_=kv_[:, h, :], identity=identb[:]), [kb, identb], [Q1])
                for h in range(4):
                    K.op("dve", lambda e: e.tensor_scalar(out=wkv[:, h, :], in0=pktv[:, h, :], scalar1=tmv[:, h:h + 1], scalar2=None, op0=ALU.mult), [Q1, tmb], [wkb]); yield
                for h in range(4):
                    dst = Q2[:, h * 129:(h + 1) * 129] if h < 3 else Q3[:, 0:129]
                    K.op("pe", lambda e: e.matmul(dst, lhsT=wkv[:, h, :], rhs=mvv[:, h, :], start=True, stop=True), [wkb, mb], [Q2 if h < 3 else Q3])
                for h in range(4):
                    src = Q2[:, h * 129:(h + 1) * 129] if h < 3 else Q3[:, 0:129]
                    K.op("dve", lambda e: e.scalar_tensor_tensor(out=Cst[:, h, :], in0=Cst[:, h, :], scalar=tmv[:, 16 + h:17 + h], in1=src,
                                                                 op0=ALU.mult, op1=ALU.add), [Cst, tmb, Q2 if h < 3 else Q3], [Cst]); yield

            def step(gn):
                if gn is None:
                    return None
                try:
                    next(gn); return gn
                except StopIteration:
                    return None

            NPP = max(NPRE - 1, 0)
            if NPP > 0:
                ga_gen = pfA(0, 0)
                while ga_gen is not None:
                    ga_gen = step(ga_gen)
                for ci in range(NPP):
                    gb_gen = pfB(ci, ci % 2)
                    ga_gen = pfA(ci + 1, (ci + 1) % 2) if ci + 1 < NPP else None
                    while gb_gen is not None or ga_gen is not None:
                        gb_gen = step(gb_gen)
                        ga_gen = step(ga_gen)
                lb, lv = PCIN[(NPP - 1) % 2]
                if (NPP - 1) % 2 == 1:
                    K.op("pool", lambda e: e.tensor_copy(out=cin[:, 4:8, 0:3], in_=lv[:, :, 128:131]), [lb], [cin])
                K.barrier()
                K.op("dve", lambda e: e.memset(mv[:], 1.0), [], [mv])
            if NPRE > 0:
                chunk(NPRE - 1, True, True, False)
            if NPRE == 0:
                K.op("dve", lambda e: e.memset(kT[1][:], 0.0), [], [kT[1]])
                K.op("dve", lambda e: e.memset(vsb[1][:], 0.0), [], [vsb[1]])
            for c in range(NOWN):
                chunk(c, False, True, True)
        K.barrier()
        if stage == 1:
            with ExitStack() as ed:
                tt = K.sb("dbgt", [128, D], F32, ed)
                for c in range(NOWN if DBG is None else 0):
                    K.dma("sp", lambda e: e.dma_start(out=tt[:], in_=hbuf_d[c * 128:(c + 1) * 128, :]), tt, [hbuf_d])
                    K.dma("sp", lambda e: e.dma_start(out=y[c * 128:(c + 1) * 128, :], in_=tt[:]), y, [tt])
                K.finish([y])
            return nc

        with ExitStack() as e2:
            stage = [K.sb("stg2_%d" % i, [128, 2048], F32, e2) for i in range(2)]
            wq = K.sb("wq", [128, 8, 2048], BF16, e2)
            load_bf16(wq, 8, 2048, wq_d.t.rearrange("(kc p) n -> p kc n", p=128), wq_d, stage)
            skT = K.sb("skT", [128, 2, 128], F32, e2)
            K.dma("sp", lambda e: e.dma_start(out=skT[:], in_=sk_d[:]), skT, [sk_d])
            A2 = K.sb("A2", [128, D], F32, e2); B2 = K.sb("B2", [128, D], F32, e2); G2 = K.sb("G2", [128, D], F32, e2)
            K.dma("sp", lambda e: e.dma_start(out=A2[:], in_=modbc_d[:, 4 * D:5 * D]), A2, [modbc_d])
            K.dma("sp", lambda e: e.dma_start(out=B2[:], in_=modbc_d[:, 3 * D:4 * D]), B2, [modbc_d])
            K.dma("sp", lambda e: e.dma_start(out=G2[:], in_=modbc_d[:, 5 * D:6 * D]), G2, [modbc_d])
            NF = K.sb("NF", [128, D], F32, e2); bcast_load(NF, nfw_d[:], nfw_d)
            iota16 = K.sb("iota16", [128, 16], F32, e2)
            K.dma("sp", lambda e: e.dma_start(out=iota16[:], in_=iota_d[:]), iota16, [iota_d])
            ht = [K.sb("ht%d" % i, [128, D], F32, e2) for i in range(2)]
            u2 = K.sb("u2", [128, D], F32, e2)
            junk = K.sb("junk2", [128, 512], F32, e2); junkf = K.sb("junkf", [128, D], F32, e2)
            ssq = K.sb("ssq2", [128, 4], F32, e2); rstd = K.sb("rstd2", [128, 1], F32, e2)
            u2T = K.sb("u2T", [128, 8, 128], BF16, e2)
            qyT = K.sb("qyT", [128, 16, 128], F32, e2)
            sc = K.sb("sc", [128, 16, 128], F32, e2); sc2 = K.sb("sc2", [128, 16, 128], F32, e2)
            tv = K.sb("tv", [128, 16, 16], F32, e2); ti = K.sb("ti", [128, 16, 16], U32, e2)
            tif = K.sb("tif", [128, 16, 16], F32, e2)
            cand = K.sb("cand", [128, 8, 256], F32, e2); cand2 = K.sb("cand2", [128, 8, 256], F32, e2)
            bs = K.sb("bs", [128, 8, 16], F32, e2); bp = K.sb("bp", [128, 8, 16], U32, e2)
            au = K.sb("au", [128, 8, 16], U32, e2); bu = K.sb("bu", [128, 8, 16], U32, e2)
            af = K.sb("af", [128, 8, 16], F32, e2); bfl = K.sb("bfl", [128, 8, 16], F32, e2)
            eq = K.sb("eq", [128, 8, 16, 16], F32, e2)
            isl = K.sb("isl", [128, 8, 16], F32, e2); jsl = K.sb("jsl", [128, 8, 16], F32, e2)
            idxf = K.sb("idxf", [128, 128], F32, e2); idx = K.sb("idx", [128, 128], I32, e2)
            gsm = K.sb("gsm", [128, 24], F32, e2)
            gate = K.sb("gate", [128, 8, 16], F32, e2)
            dots = K.sb("dots", [128, 128], F32, e2); wsl = K.sb("wsl", [128, 128], F32, e2)
            acc = K.sb("acc", [128, D], F32, e2)
            R = 6
            ring = [K.sb("ring%d" % i, [128, D], F32, e2) for i in range(R)]
            yt = K.sb("yt", [128, D], F32, e2)
            tmpr = [K.sb("tmpr%d" % i, [128, D], BF16, e2) for i in range(3)]
            class _RB:
                def __init__(s_, b): s_.b = b
                def __getitem__(s_, k): return s_.b[:].bitcast(BF16)[:, 0:D][k]
            rcount = [0]
            blk = 0
            for (src_t, dst_t) in ((pu_d, pub_d), (pv_d, pvb_d)):
                sv_ = src_t.t.rearrange("(p q) d -> p q d", q=128)
                dv_ = dst_t.t.rearrange("(p q) d -> p q d", q=128)
                for q in range(128):
                    rbf = ring[blk % R]; tbf = tmpr[blk % 3]
                    K.dma("sp", lambda e: e.dma_start(out=rbf[:], in_=sv_[:, q, :]), rbf, [src_t])
                    engs = ("dve", "act")
                    en = engs[blk % 2]
                    if en == "act":
                        K.op("act", lambda e: e.activation(out=tbf[:], in_=rbf[:], func=AF.Copy), [rbf], [tbf])
                    else:
                        K.op(en, lambda e: e.tensor_copy(out=tbf[:], in_=rbf[:]), [rbf], [tbf])
                    K.dma("pool", lambda e: e.dma_start(out=dv_[:, q, :], in_=tbf[:]), dst_t, [tbf], waw=False)
                    blk += 1

            def gather(src_d, col, idx):
                rb = ring[rcount[0] % R]; rcount[0] += 1
                K.dma("pool", lambda e: e.indirect_dma_start(out=rb[:].bitcast(BF16)[:, 0:D], out_offset=None, in_=src_d[:, :],
                                                             in_offset=bass.IndirectOffsetOnAxis(ap=idx[:, col:col + 1], axis=0)),
                      rb, [src_d, idx])
                return rb

            idxs = [idx, K.sb("idx_b", [128, 128], I32, e2)]

            def front(c):
                h_ = ht[c % 2]
                idx = idxs[c % 2]
                K.dma("sp", lambda e: e.dma_start(out=h_[:], in_=hbuf_d[c * 128:(c + 1) * 128, :]), h_, [hbuf_d])
                rmsnorm_mod(h_, u2, A2, B2, (junk, ssq, rstd))
                transpose_to(u2, u2T, 8, P[0], P[1])
                for hp in range(16):
                    pb = P[2 + hp // 4]
                    q = hp % 4
                    for kc in range(8):
                        K.op("pe", lambda e: e.matmul(pb[:, q * 128:(q + 1) * 128], lhsT=wq[:, kc, hp * 128:(hp + 1) * 128], rhs=u2T[:, kc, :],
                                                      start=(kc == 0), stop=(kc == 7)), [wq, u2T], [pb])
                    if q == 3:
                        K.op("act", lambda e: e.activation(out=qyT[:, hp - 3:hp + 1, :], in_=pb[:, :].rearrange("p (a b) -> p a b", b=128),
                                                           func=AF.Copy), [pb], [qyT])
                        yield
                for hp in range(16):
                    pb = P[2 + hp // 4]
                    q = hp % 4
                    K.op("pe", lambda e: e.matmul(pb[:, q * 128:(q + 1) * 128], lhsT=qyT[:, hp, :], rhs=skT[:, hp % 2, :], start=True, stop=True),
                         [qyT, skT], [pb])
                    if q == 3:
                        K.op("act", lambda e: e.activation(out=sc[:, hp - 3:hp + 1, :], in_=pb[:, :].rearrange("p (a b) -> p a b", b=128),
                                                           func=AF.Copy), [pb], [sc])
                        yield
                for hp in range(16):
                    K.op("dve", lambda e: e.max(out=tv[:, hp, 0:8], in_=sc[:, hp, :]), [sc], [tv])
                    yield
                    K.op("dve", lambda e: e.max_index(out=ti[:, hp, 0:8], in_max=tv[:, hp, 0:8], in_values=sc[:, hp, :]), [sc, tv], [ti])
                    yield
                    K.op("dve", lambda e: e.match_replace(out=sc2[:, hp, :], in_to_replace=tv[:, hp, 0:8], in_values=sc[:, hp, :], imm_value=-1e30),
                         [sc, tv], [sc2])
                    yield
                    K.op("dve", lambda e: e.max(out=tv[:, hp, 8:16], in_=sc2[:, hp, :]), [sc2], [tv])
                    yield
                    K.op("dve", lambda e: e.max_index(out=ti[:, hp, 8:16], in_max=tv[:, hp, 8:16], in_values=sc2[:, hp, :]), [sc2, tv], [ti])
                    yield
                tv4 = tv[:].rearrange("p (h s) k -> p h s k", s=2)
                K.op("dve", lambda e: e.tensor_tensor(out=cand[:].rearrange("p h (a b) -> p h a b", b=16),
                                                      in0=tv4[:, :, 0, :].unsqueeze(3).to_broadcast([128, 8, 16, 16]),
                                                      in1=tv4[:, :, 1, :].unsqueeze(2).to_broadcast([128, 8, 16, 16]), op=ALU.add), [tv], [cand])
                yield
                for h in range(8):
                    K.op("dve", lambda e: e.max(out=bs[:, h, 0:8], in_=cand[:, h, :]), [cand], [bs])
                    yield
                    K.op("dve", lambda e: e.max_index(out=bp[:, h, 0:8], in_max=bs[:, h, 0:8], in_values=cand[:, h, :]), [cand, bs], [bp])
                    yield
                    K.op("dve", lambda e: e.match_replace(out=cand2[:, h, :], in_to_replace=bs[:, h, 0:8], in_values=cand[:, h, :], imm_value=-1e30),
                         [cand, bs], [cand2])
                    yield
                    K.op("dve", lambda e: e.max(out=bs[:, h, 8:16], in_=cand2[:, h, :]), [cand2], [bs])
                    yield
                    K.op("dve", lambda e: e.max_index(out=bp[:, h, 8:16], in_max=bs[:, h, 8:16], in_values=cand2[:, h, :]), [cand2, bs], [bp])
                    yield
                K.op("dve", lambda e: e.tensor_scalar(out=au[:], in0=bp[:], scalar1=4, scalar2=None, op0=ALU.logical_shift_right), [bp], [au])
                yield
                K.op("dve", lambda e: e.tensor_scalar(out=bu[:], in0=bp[:], scalar1=15, scalar2=None, op0=ALU.bitwise_and), [bp], [bu])
                yield
                K.op("dve", lambda e: e.tensor_copy(out=af[:], in_=au[:]), [au], [af])
                yield
                K.op("dve", lambda e: e.tensor_copy(out=bfl[:], in_=bu[:]), [bu], [bfl])
                yield
                K.op("dve", lambda e: e.tensor_copy(out=tif[:], in_=ti[:]), [ti], [tif])
                yield
                tif4 = tif[:].rearrange("p (h s) k -> p h s k", s=2)
                for (srcf, side, dst) in ((af, 0, isl), (bfl, 1, jsl)):
                    K.op("dve", lambda e: e.tensor_tensor(out=eq[:], in0=iota16[:].unsqueeze(1).unsqueeze(1).to_broadcast([128, 8, 16, 16]),
                                                          in1=srcf[:].unsqueeze(3).to_broadcast([128, 8, 16, 16]), op=ALU.is_equal), [iota16, srcf], [eq])
                    yield
                    K.op("dve", lambda e: e.tensor_tensor(out=eq[:], in0=eq[:], in1=tif4[:, :, side, :].unsqueeze(2).to_broadcast([128, 8, 16, 16]),
                                                          op=ALU.mult), [eq, tif], [eq])
                    yield
                    K.op("dve", lambda e: e.tensor_reduce(out=dst[:], in_=eq[:], axis=AX.X, op=ALU.add), [eq], [dst])
                    yield
                K.op("dve", lambda e: e.scalar_tensor_tensor(out=idxf[:], in0=isl[:].rearrange("p h k -> p (h k)"), scalar=128.0,
                                                             in1=jsl[:].rearrange("p h k -> p (h k)"), op0=ALU.mult, op1=ALU.add), [isl, jsl], [idxf])
                yield
                K.op("dve", lambda e: e.tensor_scalar(out=idxf[:], in0=idxf[:], scalar1=0.0, scalar2=16383.0, op0=ALU.max, op1=ALU.min), [idxf], [idxf])
                yield
                K.op("dve", lambda e: e.tensor_copy(out=idx[:], in_=idxf[:]), [idxf], [idx])
                yield
                K.op("dve", lambda e: e.tensor_tensor(out=gate[:], in0=bs[:], in1=bs[:, :, 0:1].to_broadcast([128, 8, 16]), op=ALU.subtract), [bs], [gate])
                yield
                K.op("act", lambda e: e.activation(out=gate[:], in_=gate[:], func=AF.Exp), [gate], [gate])
                yield
                K.op("dve", lambda e: e.tensor_reduce(out=gsm[:, 0:8], in_=gate[:], axis=AX.X, op=ALU.add), [gate], [gsm])
                yield
                K.op("dve", lambda e: e.reciprocal(out=gsm[:, 8:16], in_=gsm[:, 0:8]), [gsm], [gsm])
                yield
                K.op("dve", lambda e: e.tensor_tensor(out=gate[:], in0=gate[:], in1=gsm[:, 8:16].unsqueeze(2).to_broadcast([128, 8, 16]), op=ALU.mult),
                     [gate, gsm], [gate])
                yield
                yield

            def drain(g, n=None):
                k = 0
                while g is not None and (n is None or k < n):
                    try:
                        next(g)
                    except StopIteration:
                        return None
                    k += 1
                return g

            gen = drain(front(0))
            for c in range(NOWN):
                h_ = ht[c % 2]
                idx = idxs[c % 2]
                for s in range(128):
                    rb = gather(pub_d, s, idx); rbv = _RB(rb)
                    K.op("dve", lambda e: e.scalar_tensor_tensor(out=junkf[:], in0=u2[:], scalar=1.0, in1=rbv[:], op0=ALU.mult, op1=ALU.mult,
                                                                 accum_out=dots[:, s:s + 1]), [u2, rb], [junkf, dots])
                K.op("act", lambda e: e.activation(out=wsl[:], in_=dots[:], func=AF.Gelu), [dots], [wsl])
                K.op("dve", lambda e: e.tensor_tensor(out=wsl[:], in0=wsl[:], in1=gate[:].rearrange("p h k -> p (h k)"), op=ALU.mult), [wsl, gate], [wsl])
                gen = front(c + 1) if c + 1 < NOWN else None
                for s in range(128):
                    rb = gather(pvb_d, s, idx); rbv = _RB(rb)
                    tb = tmpr[s % 3]
                    K.op("act", lambda e: e.activation(out=tb[:], in_=rbv[:], func=AF.Copy, scale=wsl[:, s:s + 1]), [rb, wsl], [tb])
                    for nh in range(2):
                        K.op("pe", lambda e: e.matmul(P[6 + nh][:, :], lhsT=identb[:], rhs=tb[:, nh * 512:(nh + 1) * 512],
                                                      start=(s == 0), stop=(s == 127)), [identb, tb], [P[6 + nh]])
                    gen = drain(gen, 2)
                gen = drain(gen)
                for nh in range(2):
                    K.op("dve", lambda e: e.tensor_tensor(out=acc[:, nh * 512:(nh + 1) * 512], in0=P[6 + nh][:, :], in1=G2[:, nh * 512:(nh + 1) * 512],
                                                          op=ALU.mult), [P[6 + nh], G2], [acc])
                K.op("pool", lambda e: e.tensor_tensor(out=acc[:], in0=acc[:], in1=h_[:], op=ALU.add), [acc, h_], [acc])
                rmsnorm_mod(acc, yt, NF, None, (junk, ssq, rstd))
                K.dma("sp", lambda e: e.dma_start(out=y[c * 128:(c + 1) * 128, :], in_=yt[:]), y, [yt])
            K.finish([y])
    return nc


SEQ_FULL = 16384
ONLY = None
DBG = None
_cache = {}


def _consts():
    ident = np.eye(128, dtype=np.float32)
    tri = np.triu(np.ones((128, 128), np.float32))
    sel = np.zeros((4, 4, 128), np.float32)
    for h in range(4):
        sel[h, h, :] = 1.0
    i4 = np.eye(4, dtype=np.float32)
    iota16 = np.tile(np.arange(16, dtype=np.float32)[None, :], (128, 1))
    qi = np.arange(128)[:, None]; ki = np.arange(256)[None, :]
    rel = qi + 128 - ki
    ok = (rel >= 0) & (rel < 128)
    maskN = np.where(ok, 0.0, NEG).astype(np.float32)
    mask0 = np.where(ok & (ki >= 128), 0.0, NEG).astype(np.float32)
    return dict(ident=ident, tri=tri, sel=sel, i4=i4, iota16=iota16, maskN=maskN, mask0=mask0)


def run(inputs, S, ncore_per_seq, NOWN, stage=2):
    x = np.asarray(inputs["x"], np.float32)
    B = x.shape[0]
    NPRE = (ncore_per_seq - 1) * NOWN
    key = (NPRE, NOWN, stage)
    if key not in _cache:
        _cache[key] = build(NPRE, NOWN, stage)
    nc = _cache[key]
    f = lambda k: np.ascontiguousarray(np.asarray(inputs[k], np.float32))
    cst = _consts()
    half = 32
    inv_freq = (np.float32(10000.0) ** (-np.arange(half, dtype=np.float32) * np.float32(2.0) / np.float32(64))).astype(np.float32)
    shared = dict(
        w_ada=f("w_ada")[0], b_ada=f("b_ada")[0], norm1_w=f("norm1_w")[0], norm2_w=f("norm2_w")[0], norm_f_w=f("norm_f_w"),
        w_in=f("w_in")[0],
        conv_w=np.ascontiguousarray(f("conv_w")[0].reshape(4, 8, 128).transpose(2, 1, 0)),
        att_sinks=f("att_sinks")[0], i_bias=f("i_bias")[0].reshape(4, 1), f_bias=f("f_bias")[0].reshape(4, 1),
        mlstm_norm_w=f("mlstm_norm_w")[0], w_att=f("w_att_branch")[0], w_ml=f("w_mlstm_branch")[0], w_out=f("w_out")[0],
        wq=f("peer_w_query")[0], subkT=np.ascontiguousarray(f("peer_sub_keys")[0].transpose(2, 0, 1)),
        pu=f("peer_u")[0], pv=f("peer_v")[0],
        ident=cst["ident"], tri=cst["tri"], sel=cst["sel"], i4=cst["i4"], iota16=cst["iota16"], maskN=cst["maskN"],
    )
    c = f("c")
    in_maps = []
    T = NOWN * 128
    for b in range(B):
        for j in range(ncore_per_seq):
            start = j * T
            xo = np.ascontiguousarray(x[b, start:start + T])
            NP1 = max(NPRE, 1)
            xp = np.zeros((NP1 * 128, D), np.float32)
            valid = np.zeros((128, NP1), np.float32)
            if NPRE > 0 and start > 0:
                xp[NPRE * 128 - start:] = x[b, :start]
                valid[:, NPRE - start // 128:] = 1.0
            pos = (np.arange(start - 128, start + T, dtype=np.float32))
            ang = (pos[:, None] * inv_freq[None, :]).astype(np.float32)
            m = dict(shared)
            m.update(xo=xo, xp=xp, valid=valid, mask0=(cst["mask0"] if j == 0 else cst["maskN"]),
                     cosd=np.cos(ang).astype(np.float32), sind=np.sin(ang).astype(np.float32),
                     cT=np.ascontiguousarray(c[b].reshape(8, 128).T))
            in_maps.append(m)
    n = len(in_maps)
    res = run_bass_kernel_spmd(nc, in_maps, core_ids=list(range(n)))
    out = np.zeros((B, S, D), np.float32)
    k = 0
    for b in range(B):
        for j in range(ncore_per_seq):
            out[b, j * T:(j + 1) * T] = res.results[k]["y"]
            k += 1
    return out


def kernel(**inputs):
    return run(inputs, SEQ_FULL, 4, 32)
```

```python
import numpy as np
from contextlib import ExitStack
import concourse.bass as bass
import concourse.mybir as mybir
from concourse.bass_utils import run_bass_kernel_spmd

F32 = mybir.dt.float32
BF16 = mybir.dt.bfloat16
I32 = mybir.dt.int32
U32 = mybir.dt.uint32
AF = mybir.ActivationFunctionType
ALU = mybir.AluOpType
AX = mybir.AxisListType

D = 1024
NPROJ = 4872
EPS = 1e-6
NEG = -30000.0


class Buf:
    def __init__(self, t, name):
        self.t = t
        self.name = name
        self.w = None
        self.r = []
        self.dsem = None
        self.dcnt = 0

    def __getitem__(self, k):
        return self.t[k]


class Eng:
    def __init__(self, e, sem, name, is_pe=False):
        self.e, self.sem, self.name, self.cnt, self.is_pe = e, sem, name, 0, is_pe
        self.seen = {}


class Ctx:
    def __init__(self, nc, es):
        self.nc, self.es = nc, es
        self.engs = {}
        for nm, e in (("pe", nc.tensor), ("dve", nc.vector), ("act", nc.scalar),
                      ("pool", nc.gpsimd), ("sp", nc.sync)):
            sem = es.enter_context(nc.semaphore("s_" + nm))
            self.engs[nm] = Eng(e, sem, nm, is_pe=(nm == "pe"))
        self.bufs = []
        self.nsem = 0

    def sb(self, name, shape, dt, es=None):
        t = (es or self.es).enter_context(self.nc.sbuf_tensor("sb_" + name, shape, dt))
        b = Buf(t, name); self.bufs.append(b); return b

    def ps(self, name, shape, dt=F32, es=None):
        t = (es or self.es).enter_context(self.nc.psum_tensor("ps_" + name, shape, dt))
        b = Buf(t, name); self.bufs.append(b); return b

    def dram(self, name, shape, dt, kind="Internal"):
        t = self.nc.dram_tensor(name, shape, dt, kind=kind).ap()
        b = Buf(t, name); self.bufs.append(b); return b

    def view(self, buf, name):
        b = Buf(buf.t, name); self.bufs.append(b); return b

    def _wait(self, eng, deps):
        best = {}
        for (sem, val) in deps:
            k = id(sem)
            if k not in best or best[k][1] < val:
                best[k] = (sem, val)
        for k, (sem, val) in best.items():
            if eng.is_pe and sem is eng.sem:
                continue
            if eng.seen.get(k, 0) >= val:
                continue
            eng.e.wait_ge(sem, val)
            eng.seen[k] = val

    def _deps(self, reads, writes):
        deps = []
        for b in reads:
            if b.w: deps.append(b.w)
        for b in writes:
            if b.w: deps.append(b.w)
            deps.extend(b.r)
        return deps

    def op(self, en, fn, reads=(), writes=()):
        eng = self.engs[en]
        self._wait(eng, self._deps(reads, writes))
        ins = fn(eng.e)
        eng.cnt += 1
        ins.then_inc(eng.sem, 1)
        tok = (eng.sem, eng.cnt)
        for b in writes:
            b.w = tok; b.r = []
        for b in reads:
            if b not in writes:
                b.r.append(tok)

    def dma(self, en, fn, dst, srcs=(), waw=True):
        eng = self.engs[en]
        self._wait(eng, self._deps(srcs, [dst] if waw else []))
        if dst.dsem is None:
            dst.dsem = self.es.enter_context(self.nc.semaphore("d%d" % self.nsem))
            self.nsem += 1
        ins = fn(eng.e)
        dst.dcnt += 1
        ins.then_inc(dst.dsem, 16)
        tok = (dst.dsem, 16 * dst.dcnt)
        dst.w = tok; dst.r = []
        for b in srcs:
            b.r.append(tok)

    def barrier(self):
        toks = []
        for b in self.bufs:
            if b.w: toks.append(b.w)
            toks.extend(b.r)
        for e in self.engs.values():
            if e.cnt: toks.append((e.sem, e.cnt))
        for e in self.engs.values():
            self._wait(e, toks)
        for b in self.bufs:
            b.w = None; b.r = []

    def finish(self, bufs):
        eng = self.engs["sp"]
        self._wait(eng, [b.w for b in bufs if b.w])


def build(NPRE, NOWN, stage=2):
    nc = bass.Bass("TRN2", target_bir_lowering=False)
    es = ExitStack()
    with es:
        K = Ctx(nc, es)

        def din(name, shape, dt=F32):
            return K.dram(name, shape, dt, kind="ExternalInput")
        NP1 = max(NPRE, 1)
        xo = din("xo", [NOWN * 128, D]); xp = din("xp", [NP1 * 128, D])
        valid_d = din("valid", [128, NP1])
        mask0_d = din("mask0", [128, 256]); maskN_d = din("maskN", [128, 256])
        cos_d = din("cosd", [(NOWN + 1) * 128, 32]); sin_d = din("sind", [(NOWN + 1) * 128, 32])
        cT_d = din("cT", [128, 8])
        w_ada_d = din("w_ada", [D, 6 * D]); b_ada_d = din("b_ada", [6 * D])
        n1w_d = din("norm1_w", [D]); n2w_d = din("norm2_w", [D]); nfw_d = din("norm_f_w", [D])
        w_in_d = din("w_in", [D, NPROJ])
        cw_d = din("conv_w", [128, 8, 4])
        sink_d = din("att_sinks", [8]); ib_d = din("i_bias", [4, 1]); fb_d = din("f_bias", [4, 1])
        mnw_d = din("mlstm_norm_w", [512])
        wa_d = din("w_att", [512, D]); wb_d = din("w_ml", [512, D]); wo_d = din("w_out", [D, D])
        wq_d = din("wq", [D, 2048]); sk_d = din("subkT", [128, 2, 128])
        pu_d = din("pu", [16384, D]); pv_d = din("pv", [16384, D])
        ident_d = din("ident", [128, 128]); tri_d = din("tri", [128, 128])
        sel_d = din("sel", [4, 4, 128]); i4_d = din("i4", [4, 4]); iota_d = din("iota16", [128, 16])
        y = K.dram("y", [NOWN * 128, D], F32, kind="ExternalOutput")
        modbc_d = K.dram("modbc", [128, 6 * D], F32)
        hbuf_d = K.dram("hbuf", [NOWN * 128, D], F32)
        pub_d = K.dram("pub", [16384, D], BF16)
        pvb_d = K.dram("pvb", [16384, D], BF16)

        ident = K.sb("ident", [128, 128], F32); identb = K.sb("identb", [128, 128], BF16)
        K.dma("sp", lambda e: e.dma_start(out=ident[:], in_=ident_d[:]), ident, [ident_d])
        K.op("dve", lambda e: e.tensor_copy(out=identb[:], in_=ident[:]), [ident], [identb])
        ones = K.sb("ones", [128, 128], F32)
        K.op("dve", lambda e: e.memset(ones[:], 1.0), [], [ones])
        P = [K.ps("P%d" % i, [128, 512], F32) for i in range(8)]

        def bcast_load(dst, src_ap, src_buf):
            K.dma("sp", lambda e: e.dma_start(out=dst[:], in_=src_ap.partition_broadcast(128)), dst, [src_buf])

        with ExitStack() as e0:
            cT = K.sb("cT", [128, 8], F32, e0); cact = K.sb("cact", [128, 8], F32, e0)
            crep = K.sb("crep", [128, 8, 128], BF16, e0)
            K.dma("sp", lambda e: e.dma_start(out=cT[:], in_=cT_d[:]), cT, [cT_d])
            K.op("act", lambda e: e.activation(out=cact[:], in_=cT[:], func=AF.Silu), [cT], [cact])
            K.op("dve", lambda e: e.tensor_copy(out=crep[:], in_=cact[:].unsqueeze(2).to_broadcast([128, 8, 128])),
                 [cact], [crep])
            wst = [K.sb("wst%d" % i, [128, 8, 512], F32, e0) for i in range(2)]
            wsb = [K.sb("wsb%d" % i, [128, 8, 512], BF16, e0) for i in range(2)]
            badab = K.sb("badab", [128, 6 * D], F32, e0)
            bcast_load(badab, b_ada_d[:], b_ada_d)
            modsb = K.sb("modsb", [128, 6 * D], F32, e0)
            wv = w_ada_d.t.rearrange("(kc p) n -> p kc n", p=128)
            for j in range(12):
                ws = wst[j % 2]
                K.dma("sp", lambda e: e.dma_start(out=ws[:], in_=wv[:, :, j * 512:(j + 1) * 512]), ws, [w_ada_d])
                pj = P[j % 2]
                wb_ = wsb[j % 2]
                K.op("act" if j % 2 else "dve", (lambda e: e.activation(out=wb_[:], in_=ws[:], func=AF.Copy)) if j % 2 else
                     (lambda e: e.tensor_copy(out=wb_[:], in_=ws[:])), [ws], [wb_])
                for kc in range(8):
                    K.op("pe", lambda e: e.matmul(pj[:, :], lhsT=crep[:, kc, :], rhs=wb_[:, kc, :],
                                                  start=(kc == 0), stop=(kc == 7)), [crep, wb_], [pj])
                K.op("dve", lambda e: e.tensor_tensor(out=modsb[:, j * 512:(j + 1) * 512], in0=pj[:, :],
                                                      in1=badab[:, j * 512:(j + 1) * 512], op=ALU.add),
                     [pj, badab], [modsb])
            nwb = K.sb("nwb", [128, D], F32, e0)
            for (wd, slot) in ((n1w_d, 1), (n2w_d, 4)):
                bcast_load(nwb, wd[:], wd)
                K.op("dve", lambda e: e.scalar_tensor_tensor(out=modsb[:, slot * D:(slot + 1) * D],
                                                             in0=modsb[:, slot * D:(slot + 1) * D], scalar=1.0,
                                                             in1=nwb[:], op0=ALU.add, op1=ALU.mult),
                     [modsb, nwb], [modsb])
            K.dma("sp", lambda e: e.dma_start(out=modbc_d[:], in_=modsb[:]), modbc_d, [modsb])
        K.barrier()

        def load_bf16(dst, nrow_chunks, ncols, src_view, src_buf, stage):
            i = 0
            for kc in range(nrow_chunks):
                for c0 in range(0, ncols, 2048):
                    c1 = min(ncols, c0 + 2048)
                    st = stage[i % 2]; i += 1
                    pp = dst.t.shape[0]
                    K.dma("sp", lambda e: e.dma_start(out=st[0:pp, 0:c1 - c0], in_=src_view[:, kc, c0:c1]), st, [src_buf])
                    eng = "act" if (i % 2) else "dve"
                    if eng == "act":
                        K.op("act", lambda e: e.activation(out=dst[:, kc, c0:c1], in_=st[0:pp, 0:c1 - c0], func=AF.Copy), [st], [dst])
                    else:
                        K.op("dve", lambda e: e.tensor_copy(out=dst[:, kc, c0:c1], in_=st[0:pp, 0:c1 - c0]), [st], [dst])

        def rmsnorm_mod(xt, ut, A, B, sm):
            junk, ssq, rstd = sm
            K.op("act", lambda e: e.activation(out=junk[:].bitcast(BF16), in_=xt[:], func=AF.Square, accum_out=ssq[:, 0:1]), [xt], [junk, ssq])
            K.op("dve", lambda e: e.tensor_scalar(out=ssq[:, 1:2], in0=ssq[:, 0:1], scalar1=1.0 / D, scalar2=EPS,
                                                  op0=ALU.mult, op1=ALU.add), [ssq], [ssq])
            K.op("act", lambda e: e.activation(out=ssq[:, 2:3], in_=ssq[:, 1:2], func=AF.Ln), [ssq], [ssq])
            K.op("act", lambda e: e.activation(out=rstd[:], in_=ssq[:, 2:3], func=AF.Exp, scale=-0.5), [ssq], [rstd])
            K.op("dve", lambda e: e.scalar_tensor_tensor(out=ut[:], in0=xt[:], scalar=rstd[:, 0:1], in1=A[:],
                                                         op0=ALU.mult, op1=ALU.mult), [xt, rstd, A], [ut])
            if B is not None:
                K.op("pool", lambda e: e.tensor_tensor(out=ut[:], in0=ut[:], in1=B[:], op=ALU.add), [ut, B], [ut])

        def transpose_to(src, dstT, nk, pa, pb, dt32=True, srcbuf=None):
            srcbuf = srcbuf or src
            idn = ident if dt32 else identb
            for half in range((nk + 3) // 4):
                pb_ = (pa, pb)[half % 2]
                n = min(4, nk - half * 4)
                if dt32:
                    pv = pb_[:, :].rearrange("p (a b) -> p a b", b=128)
                else:
                    pv = pb_[:, :].bitcast(BF16).rearrange("p (a b) -> p a b", b=128)
                for q in range(n):
                    kc = half * 4 + q
                    K.op("pe", lambda e: e.transpose(out=pv[:, q, :], in_=src[:, kc * 128:(kc + 1) * 128], identity=idn[:]),
                         [srcbuf, idn], [pb_])
                K.op("act", lambda e: e.activation(out=dstT[:, half * 4:half * 4 + n, :], in_=pv[:, 0:n, :], func=AF.Copy),
                     [pb_], [dstT])

        with ExitStack() as e1:
            w_in = K.sb("w_in", [128, 8, NPROJ], BF16, e1)
            wa = K.sb("wa", [64, 8, D], BF16, e1)
            wb = K.sb("wb", [128, 4, D], BF16, e1)
            wo = K.sb("wo", [128, 8, D], BF16, e1)
            with ExitStack() as est:
                stage = [K.sb("stg%d" % i, [128, 2048], F32, est) for i in range(2)]
                load_bf16(w_in, 8, NPROJ, w_in_d.t.rearrange("(kc p) n -> p kc n", p=128), w_in_d, stage)
                load_bf16(wa, 8, D, wa_d.t.rearrange("(h p) n -> p h n", p=64), wa_d, stage)
                load_bf16(wb, 4, D, wb_d.t.rearrange("(kc p) n -> p kc n", p=128), wb_d, stage)
                load_bf16(wo, 8, D, wo_d.t.rearrange("(kc p) n -> p kc n", p=128), wo_d, stage)
                K.barrier()
            A1 = K.sb("A1", [128, D], F32, e1); B1 = K.sb("B1", [128, D], F32, e1); G1 = K.sb("G1", [128, D], F32, e1)
            K.dma("sp", lambda e: e.dma_start(out=A1[:], in_=modbc_d[:, 1 * D:2 * D]), A1, [modbc_d])
            K.dma("sp", lambda e: e.dma_start(out=B1[:], in_=modbc_d[:, 0:D]), B1, [modbc_d])
            K.dma("sp", lambda e: e.dma_start(out=G1[:], in_=modbc_d[:, 2 * D:3 * D]), G1, [modbc_d])
            cw = K.sb("cw", [128, 8, 4], F32, e1)
            K.dma("sp", lambda e: e.dma_start(out=cw[:], in_=cw_d[:]), cw, [cw_d])
            sinkb = K.sb("sinkb", [128, 8], F32, e1); bcast_load(sinkb, sink_d[:], sink_d)
            mnwb = K.sb("mnwb", [128, 512], F32, e1); bcast_load(mnwb, mnw_d[:], mnw_d)
            ibc = K.sb("ibc", [4, 1], F32, e1); fbc = K.sb("fbc", [4, 1], F32, e1)
            K.dma("sp", lambda e: e.dma_start(out=ibc[:], in_=ib_d[:]), ibc, [ib_d])
            K.dma("sp", lambda e: e.dma_start(out=fbc[:], in_=fb_d[:]), fbc, [fb_d])
            nfb = K.sb("nfb", [4, 1], F32, e1)
            K.op("dve", lambda e: e.tensor_scalar(out=nfb[:], in0=fbc[:], scalar1=-1.0, scalar2=None, op0=ALU.mult), [fbc], [nfb])
            valid = K.sb("valid", [128, NP1], F32, e1); pen = K.sb("pen", [128, NP1], F32, e1)
            K.dma("sp", lambda e: e.dma_start(out=valid[:], in_=valid_d[:]), valid, [valid_d])
            K.op("dve", lambda e: e.tensor_scalar(out=pen[:], in0=valid[:], scalar1=-1.0, scalar2=1e30, op0=ALU.add, op1=ALU.mult),
                 [valid], [pen])
            masks = [K.sb("mask0", [128, 256], F32, e1), K.sb("maskN", [128, 256], F32, e1)]
            K.dma("sp", lambda e: e.dma_start(out=masks[0][:], in_=mask0_d[:]), masks[0], [mask0_d])
            K.dma("sp", lambda e: e.dma_start(out=masks[1][:], in_=maskN_d[:]), masks[1], [maskN_d])
            tri = K.sb("tri", [128, 128], F32, e1)
            K.dma("sp", lambda e: e.dma_start(out=tri[:], in_=tri_d[:]), tri, [tri_d])
            sel = K.sb("sel", [4, 4, 128], F32, e1); i4 = K.sb("i4", [4, 4], F32, e1)
            K.dma("sp", lambda e: e.dma_start(out=sel[:], in_=sel_d[:]), sel, [sel_d])
            K.dma("sp", lambda e: e.dma_start(out=i4[:], in_=i4_d[:]), i4, [i4_d])

            xt = [K.sb("xt0", [128, D], F32, e1)] * 2
            ut = K.sb("ut", [128, D], F32, e1)
            junkF = K.sb("junk", [128, 512], F32, e1)
            ssq = K.sb("ssq", [128, 4], F32, e1); rstd = K.sb("rstd", [128, 1], F32, e1)
            uT = K.sb("uT", [128, 8, 128], BF16, e1)
            qk = K.sb("qk", [128, 640], F32, e1)
            vsb = [K.sb("vsb%d" % i, [128, 2, 64], BF16, e1) for i in range(2)]
            mv = K.sb("mv", [128, 4, 129], BF16, e1)
            K.op("dve", lambda e: e.memset(mv[:], 1.0), [], [mv])
            so = K.sb("so", [128, 512], F32, e1)
            sg = K.sb("sg", [128, 2048], BF16, e1)
            cin = K.sb("cin", [128, 8, 131], F32, e1)
            K.op("dve", lambda e: e.memset(cin[:], 0.0), [], [cin])
            qkT = K.sb("qkT", [128, 8, 128], BF16, e1)
            cs = K.sb("cs", [128, 32], F32, e1); sn = K.sb("sn", [128, 32], F32, e1)
            qkr = K.sb("qkr", [128, 10, 64], BF16, e1)
            qT = K.sb("qT", [64, 8, 128], BF16, e1)
            kT = [K.sb("kT%d" % i, [64, 2, 128], BF16, e1) for i in range(2)]
            s_sb = K.sb("s_sb", [128, 8, 256], F32, e1)
            p_bf = K.sb("p_bf", [128, 8, 256], BF16, e1)
            s_flat = s_sb[:].rearrange("p h k -> p (h k)")
            class _V:
                def __init__(s_, ap): s_.ap = ap
                def __getitem__(s_, k): return s_.ap[k]
            rtv = [_V(s_flat[:, i * 320:(i + 1) * 320].rearrange("p (h d) -> p h d", d=32)) for i in range(4)]
            m1 = _V(s_flat[:, 0:D]); m2 = _V(s_flat[:, D:2 * D])
            cy = _V(s_flat[:, 0:D].rearrange("p (j t) -> p j t", t=128))
            mg = _V(p_bf[:].rearrange("p h k -> p (h k)")[:, 0:D])
            sm8 = K.sb("sm8", [128, 40], F32, e1)
            pT = K.sb("pT", [128, 16, 128], BF16, e1)
            attT = K.sb("attT", [64, 8, 128], BF16, e1)
            g = {n: K.sb("g_" + n, [4, 128], F32, e1) for n in
                 ("i", "e", "lf", "b", "a", "ew", "imb", "r", "mm", "nmm", "wi", "mo", "em", "one")}
            K.op("dve", lambda e: e.memset(g["one"][:], 1.0), [], [g["one"]])
            gs = K.sb("gs", [4, 16], F32, e1)
            mst = K.sb("mst", [4, 1], F32, e1)
            K.op("dve", lambda e: e.memset(mst[:], 0.0), [], [mst])
            dg = K.sb("dg", [4, 4], F32, e1)
            tms = K.sb("tms", [128, 20], F32, e1)
            Cst = K.sb("Cst", [128, 4, 129], F32, e1)
            K.op("dve", lambda e: e.memset(Cst[:], 0.0), [], [Cst])
            Cbf = K.sb("Cbf", [128, 4, 129], BF16, e1)
            wk = K.sb("wk", [128, 4, 128], BF16, e1)
            Eb = K.sb("Eb", [128, 128], F32, e1)
            WT = K.sb("WT", [128, 128], BF16, e1)
            p1 = K.sb("p1", [128, 129], F32, e1); nd = K.sb("nd", [128, 129], F32, e1)
            hid = K.sb("hid", [128, 512], F32, e1); hsq = junkF
            hs = K.sb("hs", [128, 12], F32, e1)
            mls = K.sb("mls", [128, 512], BF16, e1)
            mlsT = K.sb("mlsT", [128, 4, 128], BF16, e1)
            mT = pT
            hh = ut

            def chunk(ci, prefix, kv, full):
                x = xt[ci % 2]
                srcd = xp if prefix else xo
                K.dma("sp", lambda e: e.dma_start(out=x[:], in_=srcd[ci * 128:(ci + 1) * 128, :]), x, [srcd])
                rmsnorm_mod(x, ut, A1, B1, (junkF, ssq, rstd))
                def dump(name, buf, ap, w):
                    if full and DBG == name:
                        K.dma("pool", lambda e: e.dma_start(out=y[ci * 128:(ci + 1) * 128, 0:w], in_=ap), y, [buf])
                dump("u", ut, ut[:], D)
                if DBG == "AB":
                    DBGs = "AB"
                    K.dma("pool", lambda e: e.dma_start(out=y[ci * 128:(ci + 1) * 128, :], in_=(A1 if ci == 0 else B1)[:]), y, [A1, B1]) if full else None
                transpose_to(ut, uT, 8, P[0], P[1])
                par = (ci % 2) if full else 1
                if kv and not full:
                    par = 1
                def tm_group(pb, c0, c1):
                    for kc in range(8):
                        K.op("pe", lambda e: e.matmul(pb[:, 0:c1 - c0], lhsT=uT[:, kc, :], rhs=w_in[:, kc, c0:c1],
                                                      start=(kc == 0), stop=(kc == 7)), [uT, w_in], [pb])
                if kv:
                    tm_group(P[2], 0, 512); tm_group(P[3], 512, 768)
                    K.op("act", lambda e: e.activation(out=qk[:, 0:512], in_=P[2][:, :], func=AF.Copy), [P[2]], [qk])
                    K.op("act", lambda e: e.activation(out=qk[:, 512:640], in_=P[3][:, 0:128], func=AF.Copy), [P[3]], [qk])
                    vdst = vsb[par]
                    K.op("dve", lambda e: e.tensor_copy(out=vdst[:].rearrange("p a b -> p (a b)"), in_=P[3][:, 128:256]),
                         [P[3]], [vdst])
                tm_group(P[4], 1792, 2304)
                K.op("act", lambda e: e.activation(out=mv[:, :, 0:128], in_=P[4][:, :].rearrange("p (a b) -> p a b", b=128),
                                                   func=AF.Copy), [P[4]], [mv])
                if full:
                    tm_group(P[5], 2304, 2816)
                    K.op("act", lambda e: e.activation(out=so[:], in_=P[5][:, :], func=AF.Sigmoid), [P[5]], [so])
                    for j in range(4):
                        pb = P[2 + (j % 2)]
                        tm_group(pb, 2824 + j * 512, 2824 + (j + 1) * 512)
                        K.op("act", lambda e: e.activation(out=sg[:, j * 512:(j + 1) * 512], in_=pb[:, :], func=AF.Sigmoid),
                             [pb], [sg])
                vcol = valid[:, ci:ci + 1] if prefix else ones[:, 0:1]
                vb = valid if prefix else ones
                j0 = 0 if (full or kv) else 4
                for j in range(j0, 8):
                    pb = P[6 + (j // 4) % 2]
                    q = j % 4
                    for kc in range(8):
                        K.op("pe", lambda e: e.matmul(pb[:, q * 128:(q + 1) * 128], lhsT=w_in[:, kc, 768 + j * 128:768 + (j + 1) * 128],
                                                      rhs=uT[:, kc, :], start=(kc == 0), stop=(kc == 7)), [uT, w_in], [pb])
                    K.op("dve", lambda e: e.tensor_scalar(out=cin[:, j, 3:131], in0=pb[:, q * 128:(q + 1) * 128],
                                                          scalar1=vcol, scalar2=None, op0=ALU.mult), [pb, vb], [cin])
                pgi = P[0]; pgf = P[1]
                for kc in range(8):
                    K.op("pe", lambda e: e.matmul(pgi[0:4, 0:128], lhsT=w_in[:, kc, 2816:2820], rhs=uT[:, kc, :],
                                                  start=(kc == 0), stop=(kc == 7)), [uT, w_in], [pgi])
                for kc in range(8):
                    K.op("pe", lambda e: e.matmul(pgf[0:4, 0:128], lhsT=w_in[:, kc, 2820:2824], rhs=uT[:, kc, :],
                                                  start=(kc == 0), stop=(kc == 7)), [uT, w_in], [pgf])
                for j in range(j0, 8):
                    K.op("dve", lambda e: e.tensor_scalar(out=cy[:, j, :], in0=cin[:, j, 0:128], scalar1=cw[:, j, 0:1],
                                                          scalar2=None, op0=ALU.mult), [cin, cw], [s_sb])
                    for t in range(1, 4):
                        K.op("dve", lambda e: e.scalar_tensor_tensor(out=cy[:, j, :], in0=cin[:, j, t:t + 128],
                                                                     scalar=cw[:, j, t:t + 1], in1=cy[:, j, :],
                                                                     op0=ALU.mult, op1=ALU.add), [cin, cw, s_sb], [s_sb])
                K.op("pool", lambda e: e.tensor_copy(out=cin[:, j0:8, 0:3], in_=cin[:, j0:8, 128:131]), [cin], [cin])
                K.op("act", lambda e: e.activation(out=qkT[:, j0:8, :], in_=cy[:, j0:8, :], func=AF.Silu), [s_sb], [qkT])
                K.op("pool", lambda e: e.tensor_scalar(out=qkT[:, 4:8, :], in0=qkT[:, 4:8, :], scalar1=128.0 ** -0.5,
                                                       scalar2=None, op0=ALU.mult), [qkT], [qkT])
                gi, ge, lf, gb, ga_, ew, imb, gr, mm, nmm, wi, gmo, em, gone = (g[n] for n in
                    ("i", "e", "lf", "b", "a", "ew", "imb", "r", "mm", "nmm", "wi", "mo", "em", "one"))
                if prefix:
                    K.op("dve", lambda e: e.tensor_scalar(out=gi[:], in0=pgi[0:4, 0:128], scalar1=ibc[:, 0:1], scalar2=valid[0:4, ci:ci + 1],
                                                          op0=ALU.add, op1=ALU.mult), [pgi, ibc, valid], [gi])
                    K.op("dve", lambda e: e.tensor_scalar(out=gi[:], in0=gi[:], scalar1=pen[0:4, ci:ci + 1], scalar2=None,
                                                          op0=ALU.add), [gi, pen], [gi])
                else:
                    K.op("dve", lambda e: e.tensor_scalar(out=gi[:], in0=pgi[0:4, 0:128], scalar1=ibc[:, 0:1], scalar2=None,
                                                          op0=ALU.add), [pgi, ibc], [gi])
                K.op("act", lambda e: e.activation(out=ge[:], in_=pgf[0:4, 0:128], func=AF.Exp, bias=nfb[:, 0:1], scale=-1.0),
                     [pgf, nfb], [ge])
                K.op("act", lambda e: e.activation(out=ge[:], in_=ge[:], func=AF.Ln, bias=1.0), [ge], [ge])
                v4 = valid[0:4, ci:ci + 1] if prefix else ones[0:4, 0:1]
                K.op("dve", lambda e: e.tensor_scalar(out=lf[:], in0=ge[:], scalar1=v4, scalar2=-1.0, op0=ALU.mult, op1=ALU.mult),
                     [ge, vb], [lf])
                K.op("dve", lambda e: e.tensor_tensor_scan(out=gb[:], data0=gone[:], data1=lf[:], initial=0.0,
                                                           op0=ALU.mult, op1=ALU.add), [gone, lf], [gb])
                K.op("dve", lambda e: e.tensor_tensor(out=imb[:], in0=gi[:], in1=gb[:], op=ALU.subtract), [gi, gb], [imb])
                K.op("dve", lambda e: e.tensor_scalar(out=ga_[:], in0=imb[:], scalar1=gb[:, 127:128], scalar2=None, op0=ALU.add),
                     [imb, gb], [ga_])
                K.op("dve", lambda e: e.tensor_reduce(out=gs[:, 0:1], in_=ga_[:], axis=AX.X, op=ALU.max), [ga_], [gs])
                K.op("dve", lambda e: e.tensor_tensor(out=gs[:, 1:2], in0=gb[:, 127:128], in1=mst[:], op=ALU.add), [gb, mst], [gs])
                K.op("dve", lambda e: e.tensor_tensor(out=gs[:, 2:3], in0=gs[:, 1:2], in1=gs[:, 0:1], op=ALU.max), [gs], [gs])
                K.op("dve", lambda e: e.tensor_scalar(out=gs[:, 3:4], in0=gs[:, 2:3], scalar1=-1.0, scalar2=None, op0=ALU.mult), [gs], [gs])
                K.op("dve", lambda e: e.tensor_tensor(out=gs[:, 4:5], in0=gs[:, 1:2], in1=gs[:, 2:3], op=ALU.subtract), [gs], [gs])
                K.op("act", lambda e: e.activation(out=gs[:, 5:6], in_=gs[:, 4:5], func=AF.Exp), [gs], [gs])
                K.op("act", lambda e: e.activation(out=ew[:], in_=ga_[:], func=AF.Exp, bias=gs[:, 3:4]), [ga_, gs], [ew])
                K.op("dve", lambda e: e.tensor_scalar(out=dg[:], in0=i4[:], scalar1=gs[:, 5:6], scalar2=None, op0=ALU.mult), [i4, gs], [dg])
                ptm = P[0]
                rows = [ew]
                if full:
                    K.op("dve", lambda e: e.tensor_tensor_scan(out=gr[:], data0=gone[:], data1=imb[:], initial=-1e30,
                                                               op0=ALU.mult, op1=ALU.max), [gone, imb], [gr])
                    K.op("dve", lambda e: e.tensor_scalar(out=mm[:], in0=gr[:], scalar1=mst[:, 0:1], scalar2=None, op0=ALU.max), [gr, mst], [mm])
                    K.op("dve", lambda e: e.tensor_scalar(out=nmm[:], in0=mm[:], scalar1=-1.0, scalar2=None, op0=ALU.mult), [mm], [nmm])
                    K.op("act", lambda e: e.activation(out=wi[:], in_=mm[:], func=AF.Exp, bias=mst[:, 0:1], scale=-1.0), [mm, mst], [wi])
                    K.op("dve", lambda e: e.tensor_tensor(out=gmo[:], in0=gb[:], in1=mm[:], op=ALU.add), [gb, mm], [gmo])
                    K.op("act", lambda e: e.activation(out=em[:], in_=gmo[:], func=AF.Exp, scale=-1.0), [gmo], [em])
                    rows = [ew, imb, wi, em]
                for r_i, rr in enumerate(rows):
                    K.op("pe", lambda e: e.transpose(out=ptm[:, r_i * 4:(r_i + 1) * 4], in_=rr[:, :], identity=ident[0:4, 0:4]),
                         [rr, ident], [ptm])
                K.op("pe", lambda e: e.matmul(ptm[:, 16:20], lhsT=ones[0:4, :], rhs=dg[:, :], start=True, stop=True), [ones, dg], [ptm])
                K.op("act", lambda e: e.activation(out=tms[:], in_=ptm[:, 0:20], func=AF.Copy), [ptm], [tms])

                if kv:
                    K.dma("sp", lambda e: e.dma_start(out=cs[:], in_=cos_d[(ci + 1 if full else 0) * 128:(ci + 2 if full else 1) * 128, :]), cs, [cos_d])
                    K.dma("sp", lambda e: e.dma_start(out=sn[:], in_=sin_d[(ci + 1 if full else 0) * 128:(ci + 2 if full else 1) * 128, :]), sn, [sin_d])
                    dump("qk", qk, qk[:], 640)
                    q4 = qk[:].rearrange("p (h two d) -> p h two d", two=2, d=32)
                    x1 = q4[:, :, 0, :]; x2 = q4[:, :, 1, :]
                    cb = cs[:].unsqueeze(1).to_broadcast([128, 10, 32]); sbb = sn[:].unsqueeze(1).to_broadcast([128, 10, 32])
                    qr4 = qkr[:].rearrange("p h (two d) -> p h two d", two=2)
                    K.op("dve", lambda e: e.tensor_tensor(out=rtv[0][:], in0=x1, in1=cb, op=ALU.mult), [qk, cs], [s_sb])
                    K.op("pool", lambda e: e.tensor_tensor(out=rtv[1][:], in0=x2, in1=sbb, op=ALU.mult), [qk, sn], [s_sb])
                    K.op("dve", lambda e: e.tensor_tensor(out=qr4[:, :, 0, :], in0=rtv[0][:], in1=rtv[1][:], op=ALU.subtract), [s_sb], [qkr])
                    K.op("pool", lambda e: e.tensor_tensor(out=rtv[2][:], in0=x2, in1=cb, op=ALU.mult), [qk, cs], [s_sb])
                    K.op("dve", lambda e: e.tensor_tensor(out=rtv[3][:], in0=x1, in1=sbb, op=ALU.mult), [qk, sn], [s_sb])
                    K.op("dve", lambda e: e.tensor_tensor(out=qr4[:, :, 1, :], in0=rtv[2][:], in1=rtv[3][:], op=ALU.add), [s_sb], [qkr])
                    pq = P[2]; pk = P[3]
                    pqv = pq[0:64, :].bitcast(BF16).rearrange("p (a b) -> p a b", b=128)
                    pkv = pk[0:64, :].bitcast(BF16).rearrange("p (a b) -> p a b", b=128)
                    h0 = 0 if full else 8
                    for h in range(h0, 10):
                        dst = pqv[:, h, :] if h < 8 else pkv[:, h - 8, :]
                        pbuf = pq if h < 8 else pk
                        K.op("pe", lambda e: e.transpose(out=dst, in_=qkr[:, h, :], identity=identb[:]), [qkr, identb], [pbuf])
                    if full:
                        K.op("act", lambda e: e.activation(out=qT[:], in_=pqv[:, 0:8, :], func=AF.Copy), [pq], [qT])
                    kdst = kT[par]
                    K.op("act", lambda e: e.activation(out=kdst[:], in_=pkv[:, 0:2, :], func=AF.Copy), [pk], [kdst])

                if full:
                    kprev, kcur = kT[1 - par], kT[par]
                    vprev, vcur = vsb[1 - par], vsb[par]
                    mk_ = masks[0] if ci == 0 else masks[1]
                    for h in range(8):
                        pb = P[2 + h // 2]
                        o = (h % 2) * 256
                        K.op("pe", lambda e: e.matmul(pb[:, o:o + 128], lhsT=qT[:, h, :], rhs=kprev[:, h // 4, :], start=True, stop=True),
                             [qT, kprev], [pb])
                        K.op("pe", lambda e: e.matmul(pb[:, o + 128:o + 256], lhsT=qT[:, h, :], rhs=kcur[:, h // 4, :], start=True, stop=True),
                             [qT, kcur], [pb])
                    for q in range(4):
                        pb = P[2 + q]
                        K.op("dve", lambda e: e.scalar_tensor_tensor(out=s_sb[:, 2 * q:2 * q + 2, :],
                                                                     in0=pb[:, :].rearrange("p (a b) -> p a b", b=256), scalar=0.125,
                                                                     in1=mk_[:].unsqueeze(1).to_broadcast([128, 2, 256]),
                                                                     op0=ALU.mult, op1=ALU.add), [pb, mk_], [s_sb])
                    K.op("dve", lambda e: e.tensor_reduce(out=sm8[:, 0:8], in_=s_sb[:], axis=AX.X, op=ALU.max), [s_sb], [sm8])
                    K.op("dve", lambda e: e.tensor_tensor(out=sm8[:, 0:8], in0=sm8[:, 0:8], in1=sinkb[:], op=ALU.max), [sm8, sinkb], [sm8])
                    K.op("dve", lambda e: e.tensor_tensor(out=s_sb[:], in0=s_sb[:], in1=sm8[:, 0:8].unsqueeze(2).to_broadcast([128, 8, 256]),
                                                          op=ALU.subtract), [s_sb, sm8], [s_sb])
                    K.op("act", lambda e: e.activation(out=s_sb[:], in_=s_sb[:], func=AF.Exp), [s_sb], [s_sb])
                    K.op("dve", lambda e: e.tensor_reduce(out=sm8[:, 16:24], in_=s_sb[:], axis=AX.X, op=ALU.add), [s_sb], [sm8])
                    K.op("dve", lambda e: e.tensor_tensor(out=sm8[:, 8:16], in0=sinkb[:], in1=sm8[:, 0:8], op=ALU.subtract), [sm8, sinkb], [sm8])
                    K.op("act", lambda e: e.activation(out=sm8[:, 24:32], in_=sm8[:, 8:16], func=AF.Exp), [sm8], [sm8])
                    K.op("dve", lambda e: e.tensor_tensor(out=sm8[:, 16:24], in0=sm8[:, 16:24], in1=sm8[:, 24:32], op=ALU.add), [sm8], [sm8])
                    K.op("dve", lambda e: e.reciprocal(out=sm8[:, 32:40], in_=sm8[:, 16:24]), [sm8], [sm8])
                    K.op("dve", lambda e: e.tensor_tensor(out=p_bf[:], in0=s_sb[:], in1=sm8[:, 32:40].unsqueeze(2).to_broadcast([128, 8, 256]),
                                                          op=ALU.mult), [s_sb, sm8], [p_bf])
                    for hh_ in range(2):
                        pb = P[2 + hh_]
                        pvw = pb[:, :].bitcast(BF16).rearrange("p (a b) -> p a b", b=128)
                        for h4 in range(4):
                            h = hh_ * 4 + h4
                            for half in range(2):
                                K.op("pe", lambda e: e.transpose(out=pvw[:, h4 * 2 + half, :], in_=p_bf[:, h, half * 128:(half + 1) * 128],
                                                                 identity=identb[:]), [p_bf, identb], [pb])
                        K.op("act", lambda e: e.activation(out=pT[:, hh_ * 8:(hh_ + 1) * 8, :], in_=pvw[:, 0:8, :], func=AF.Copy), [pb], [pT])
                    for hh_ in range(2):
                        pb = P[4 + hh_]
                        for h4 in range(4):
                            h = hh_ * 4 + h4
                            K.op("pe", lambda e: e.matmul(pb[0:64, h4 * 128:(h4 + 1) * 128], lhsT=vprev[:, h // 4, :], rhs=pT[:, 2 * h, :],
                                                          start=True, stop=False), [vprev, pT], [pb])
                            K.op("pe", lambda e: e.matmul(pb[0:64, h4 * 128:(h4 + 1) * 128], lhsT=vcur[:, h // 4, :], rhs=pT[:, 2 * h + 1, :],
                                                          start=False, stop=True), [vcur, pT], [pb])
                        K.op("act", lambda e: e.activation(out=attT[:, hh_ * 4:(hh_ + 1) * 4, :],
                                                           in_=pb[0:64, :].rearrange("p (a b) -> p a b", b=128), func=AF.Copy), [pb], [attT])

                    K.op("act", lambda e: e.activation(out=Cbf[:], in_=Cst[:], func=AF.Copy), [Cst], [Cbf])
                    for h in range(4):
                        pbc = P[6]; pst = P[7]
                        K.op("pe", lambda e: e.matmul(pbc[:, 0:128], lhsT=sel[:, h, :], rhs=nmm[:, :], start=True, stop=True), [sel, nmm], [pbc])
                        K.op("act", lambda e: e.activation(out=Eb[:], in_=pbc[:, 0:128], func=AF.Exp, bias=tms[:, 4 + h:5 + h]), [pbc, tms], [Eb])
                        K.op("pool", lambda e: e.tensor_tensor(out=Eb[:], in0=Eb[:], in1=tri[:], op=ALU.mult), [Eb, tri], [Eb])
                        K.op("pe", lambda e: e.matmul(pst[:, 0:128], lhsT=qkT[:, 4 + h, :], rhs=qkT[:, h, :], start=True, stop=True), [qkT], [pst])
                        K.op("dve", lambda e: e.tensor_tensor(out=WT[:], in0=pst[:, 0:128], in1=Eb[:], op=ALU.mult), [pst, Eb], [WT])
                        K.op("pe", lambda e: e.matmul(pbc[:, 128:257], lhsT=WT[:], rhs=mv[:, h, :], start=True, stop=True), [WT, mv], [pbc])
                        K.op("pe", lambda e: e.matmul(pst[:, 128:257], lhsT=qkT[:, h, :], rhs=Cbf[:, h, :], start=True, stop=True), [qkT, Cbf], [pst])
                        K.op("act", lambda e: e.activation(out=p1[:], in_=pbc[:, 128:257], func=AF.Copy), [pbc], [p1])
                        K.op("dve", lambda e: e.scalar_tensor_tensor(out=nd[:], in0=pst[:, 128:257], scalar=tms[:, 8 + h:9 + h], in1=p1[:],
                                                                     op0=ALU.mult, op1=ALU.add), [pst, tms, p1], [nd])
                        K.op("dve", lambda e: e.scalar_tensor_tensor(out=hs[:, 10:11], in0=nd[:, 128:129], scalar=-1.0, in1=nd[:, 128:129],
                                                                     op0=ALU.mult, op1=ALU.max), [nd], [hs])
                        K.op("dve", lambda e: e.tensor_scalar(out=hs[:, 8:9], in0=hs[:, 10:11], scalar1=tms[:, 12 + h:13 + h], scalar2=None,
                                                              op0=ALU.max), [hs, tms], [hs])
                        K.op("dve", lambda e: e.reciprocal(out=hs[:, 9:10], in_=hs[:, 8:9]), [hs], [hs])
                        K.op("dve", lambda e: e.scalar_tensor_tensor(out=hid[:, h * 128:(h + 1) * 128], in0=nd[:, 0:128], scalar=hs[:, 9:10],
                                                                     in1=so[:, h * 128:(h + 1) * 128], op0=ALU.mult, op1=ALU.mult), [nd, hs, so], [hid])
                    dump("hid", hid, hid[:], 512)
                    K.op("pool", lambda e: e.tensor_tensor(out=hsq[:], in0=hid[:], in1=hid[:], op=ALU.mult), [hid], [hsq])
                    K.op("dve", lambda e: e.tensor_reduce(out=hs[:, 0:4], in_=hsq[:].rearrange("p (a b) -> p a b", b=128), axis=AX.X, op=ALU.add),
                         [hsq], [hs])
                    K.op("dve", lambda e: e.tensor_scalar(out=hs[:, 0:4], in0=hs[:, 0:4], scalar1=1.0 / 128, scalar2=EPS, op0=ALU.mult, op1=ALU.add), [hs], [hs])
                    K.op("act", lambda e: e.activation(out=hs[:, 0:4], in_=hs[:, 0:4], func=AF.Sqrt), [hs], [hs])
                    K.op("dve", lambda e: e.reciprocal(out=hs[:, 4:8], in_=hs[:, 0:4]), [hs], [hs])
                    for h in range(4):
                        K.op("dve", lambda e: e.scalar_tensor_tensor(out=mls[:, h * 128:(h + 1) * 128], in0=hid[:, h * 128:(h + 1) * 128],
                                                                     scalar=hs[:, 4 + h:5 + h], in1=mnwb[:, h * 128:(h + 1) * 128],
                                                                     op0=ALU.mult, op1=ALU.mult), [hid, hs, mnwb], [mls])
                    transpose_to(mls, mlsT, 4, P[6], P[7], dt32=False)

                pkt = P[6]
                pktv = pkt[:, :].bitcast(BF16).rearrange("p (a b) -> p a b", b=128)
                for h in range(4):
                    K.op("pe", lambda e: e.transpose(out=pktv[:, h, :], in_=qkT[:, 4 + h, :], identity=identb[:]), [qkT, identb], [pkt])
                for h in range(4):
                    K.op("dve", lambda e: e.tensor_scalar(out=wk[:, h, :], in0=pktv[:, h, :], scalar1=tms[:, h:h + 1], scalar2=None, op0=ALU.mult),
                         [pkt, tms], [wk])
                pu_ = P[7]
                for h in range(4):
                    K.op("pe", lambda e: e.matmul(pu_[:, h * 129:(h + 1) * 129] if h < 3 else P[6][:, 0:129], lhsT=wk[:, h, :], rhs=mv[:, h, :],
                                                  start=True, stop=True), [wk, mv], [pu_ if h < 3 else P[6]])
                for h in range(4):
                    src = pu_[:, h * 129:(h + 1) * 129] if h < 3 else P[6][:, 0:129]
                    sb_ = pu_ if h < 3 else P[6]
                    K.op("dve", lambda e: e.scalar_tensor_tensor(out=Cst[:, h, :], in0=Cst[:, h, :], scalar=tms[:, 16 + h:17 + h], in1=src,
                                                                 op0=ALU.mult, op1=ALU.add), [Cst, tms, sb_], [Cst])
                K.op("dve", lambda e: e.tensor_copy(out=mst[:], in_=gs[:, 2:3]), [gs], [mst])

                if full:
                    for nh in range(2):
                        pa_ = P[2 + nh]; pb_ = P[4 + nh]
                        for h in range(8):
                            K.op("pe", lambda e: e.matmul(pa_[:, :], lhsT=attT[:, h, :], rhs=wa[:, h, nh * 512:(nh + 1) * 512],
                                                          start=(h == 0), stop=(h == 7)), [attT, wa], [pa_])
                        for kc in range(4):
                            K.op("pe", lambda e: e.matmul(pb_[:, :], lhsT=mlsT[:, kc, :], rhs=wb[:, kc, nh * 512:(nh + 1) * 512],
                                                          start=(kc == 0), stop=(kc == 3)), [mlsT, wb], [pb_])
                        K.op("dve", lambda e: e.tensor_tensor(out=m1[:, nh * 512:(nh + 1) * 512], in0=pa_[:, :], in1=sg[:, nh * 512:(nh + 1) * 512],
                                                              op=ALU.mult), [pa_, sg], [s_sb])
                        K.op("dve", lambda e: e.tensor_tensor(out=m2[:, nh * 512:(nh + 1) * 512], in0=pb_[:, :], in1=sg[:, D + nh * 512:D + (nh + 1) * 512],
                                                              op=ALU.mult), [pb_, sg], [s_sb])
                    K.op("pool", lambda e: e.tensor_tensor(out=mg[:], in0=(m2 if ONLY == "b" else m1)[:], in1=(m1 if ONLY == "a" else m2)[:], op=ALU.add), [s_sb], [p_bf])
                    transpose_to(mg, mT, 8, P[2], P[3], dt32=False, srcbuf=p_bf)
                    for nh in range(2):
                        po = P[4 + nh]
                        for kc in range(8):
                            K.op("pe", lambda e: e.matmul(po[:, :], lhsT=mT[:, kc, :], rhs=wo[:, kc, nh * 512:(nh + 1) * 512],
                                                          start=(kc == 0), stop=(kc == 7)), [mT, wo], [po])
                        K.op("dve", lambda e: e.tensor_tensor(out=m1[:, nh * 512:(nh + 1) * 512], in0=po[:, :], in1=G1[:, nh * 512:(nh + 1) * 512],
                                                              op=ALU.mult), [po, G1], [s_sb])
                    K.op("pool", lambda e: e.tensor_tensor(out=hh[:], in0=m1[:], in1=x[:], op=ALU.add), [s_sb, x], [hh])
                    K.dma("pool", lambda e: e.dma_start(out=hbuf_d[ci * 128:(ci + 1) * 128, :], in_=hh[:]), hbuf_d, [hh])

            class _V2:
                def __init__(s_, ap): s_.ap = ap
                def __getitem__(s_, k): return s_.ap[k]
            sgf = sg[:].bitcast(F32)
            pbf32 = p_bf[:].rearrange("p h k -> p (h k)").bitcast(F32)
            hidb = hid[:].bitcast(BF16)
            mlsf = mls[:].bitcast(F32)
            PX = [(xt[0], _V2(xt[0][:])), (s_sb, _V2(s_flat[:, D:2 * D]))]
            PU = [(ut, _V2(ut[:])), (p_bf, _V2(pbf32))]
            PUT = [(uT, _V2(uT[:])), (pT, _V2(pT[:, 0:8, :]))]
            PCIN = [(cin, _V2(cin[:, 4:8, :])), (sg, _V2(sgf[:, 0:524].rearrange("p (j t) -> p j t", t=131)))]
            PCY = [(s_sb, _V2(cy[:, 4:8, :])), (s_sb, _V2(cy[:, 0:4, :]))]
            PKT = [(qkT, _V2(qkT[:, 4:8, :])), (qkT, _V2(qkT[:, 0:4, :]))]
            PMV = [(mv, _V2(mv[:])), (hid, _V2(hidb[:, 0:516].rearrange("p (h e) -> p h e", e=129)))]
            PWK = [(wk, _V2(wk[:])), (qkr, _V2(qkr[:].rearrange("p h d -> p (h d)")[:, 0:512].rearrange("p (h d) -> p h d", d=128)))]
            g1n = ("i", "e", "lf", "b", "a", "ew", "imb")
            PG = [{n: (g[n], _V2(g[n][:])) for n in g1n},
                  {n: ((so, _V2(so[0:4, k * 128:(k + 1) * 128])) if k < 4 else (mls, _V2(mlsf[0:4, (k - 4) * 128:(k - 3) * 128])))
                   for k, n in enumerate(("i", "e", "lf", "b", "a", "ew"))}]
            PG[1]["imb"] = (qk, _V2(qk[0:4, 128:256]))
            PGS = [(gs, _V2(gs[:])), (qk, _V2(qk[0:4, 0:16]))]
            PDG = [(dg, _V2(dg[:])), (qk, _V2(qk[0:4, 16:20]))]
            PTMS = [(tms, _V2(tms[:])), (qk, _V2(qk[:, 32:52]))]
            PSQ = [((ssq, _V2(ssq[:])), (rstd, _V2(rstd[:]))), ((qk, _V2(qk[:, 60:64])), (qk, _V2(qk[:, 64:65])))]
            tEb = K.view(pT, "tEb")
            if NPRE > 1:
                K.op("dve", lambda e: e.memset(PMV[1][1][:], 1.0), [], [hid])
                K.op("dve", lambda e: e.memset(PCIN[1][1][:], 0.0), [], [sg])

            def pfA(ci, p):
                xb, xv = PX[p]; ub, uv = PU[p]; utb, utv = PUT[p]; cb_, cv = PCIN[p]; mb, mvv = PMV[p]
                (sqb, sqv), (rsb, rsv) = PSQ[p]
                Q0, Q1, Q2, Q3 = P[4 * p:4 * p + 4]
                K.dma("sp", lambda e: e.dma_start(out=xv[:], in_=xp[ci * 128:(ci + 1) * 128, :]), xb, [xp])
                K.op("act", lambda e: e.activation(out=junkF[:].bitcast(BF16), in_=xv[:], func=AF.Square, accum_out=sqv[:, 0:1]), [xb], [junkF, sqb]); yield
                K.op("dve", lambda e: e.tensor_scalar(out=sqv[:, 1:2], in0=sqv[:, 0:1], scalar1=1.0 / D, scalar2=EPS, op0=ALU.mult, op1=ALU.add), [sqb], [sqb]); yield
                K.op("act", lambda e: e.activation(out=sqv[:, 2:3], in_=sqv[:, 1:2], func=AF.Ln), [sqb], [sqb]); yield
                K.op("act", lambda e: e.activation(out=rsv[:], in_=sqv[:, 2:3], func=AF.Exp, scale=-0.5), [sqb], [rsb]); yield
                K.op("dve", lambda e: e.scalar_tensor_tensor(out=uv[:], in0=xv[:], scalar=rsv[:, 0:1], in1=A1[:], op0=ALU.mult, op1=ALU.mult), [xb, rsb, A1], [ub]); yield
                K.op("pool", lambda e: e.tensor_tensor(out=uv[:], in0=uv[:], in1=B1[:], op=ALU.add), [ub, B1], [ub]); yield
                for half in range(2):
                    pb_ = (Q0, Q1)[half]
                    pv = pb_[:, :].rearrange("p (a b) -> p a b", b=128)
                    for q in range(4):
                        kc = half * 4 + q
                        K.op("pe", lambda e: e.transpose(out=pv[:, q, :], in_=uv[:, kc * 128:(kc + 1) * 128], identity=ident[:]), [ub, ident], [pb_])
                    K.op("act", lambda e: e.activation(out=utv[:, half * 4:half * 4 + 4, :], in_=pv[:, 0:4, :], func=AF.Copy), [pb_], [utb]); yield
                for kc in range(8):
                    K.op("pe", lambda e: e.matmul(Q2[:, 0:512], lhsT=utv[:, kc, :], rhs=w_in[:, kc, 1792:2304], start=(kc == 0), stop=(kc == 7)), [utb, w_in], [Q2])
                K.op("act", lambda e: e.activation(out=mvv[:, :, 0:128], in_=Q2[:, :].rearrange("p (a b) -> p a b", b=128), func=AF.Copy), [Q2], [mb]); yield
                for j in range(4):
                    for kc in range(8):
                        K.op("pe", lambda e: e.matmul(Q3[:, j * 128:(j + 1) * 128], lhsT=w_in[:, kc, 1280 + j * 128:1280 + (j + 1) * 128], rhs=utv[:, kc, :],
                                                      start=(kc == 0), stop=(kc == 7)), [utb, w_in], [Q3])
                K.op("dve", lambda e: e.tensor_scalar(out=cv[:, :, 3:131], in0=Q3[:, :].rearrange("p (a b) -> p a b", b=128), scalar1=valid[:, ci:ci + 1],
                                                      scalar2=None, op0=ALU.mult), [Q3, valid], [cb_]); yield
                ob, ov = PCIN[1 - p]
                K.op("pool", lambda e: e.tensor_copy(out=ov[:, :, 0:3], in_=cv[:, :, 128:131]), [cb_], [ob]); yield
                for kc in range(8):
                    K.op("pe", lambda e: e.matmul(Q0[0:4, 0:128], lhsT=w_in[:, kc, 2816:2820], rhs=utv[:, kc, :], start=(kc == 0), stop=(kc == 7)), [utb, w_in], [Q0])
                for kc in range(8):
                    K.op("pe", lambda e: e.matmul(Q0[0:4, 128:256], lhsT=w_in[:, kc, 2820:2824], rhs=utv[:, kc, :], start=(kc == 0), stop=(kc == 7)), [utb, w_in], [Q0])
                yield

            def pfB(ci, p):
                cb_, cv = PCIN[p]; cyb, cyv = PCY[p]; kb, kv_ = PKT[p]; mb, mvv = PMV[p]; wkb, wkv = PWK[p]
                G = PG[p]; gsb, gsv = PGS[p]; dgb, dgv = PDG[p]; tmb, tmv = PTMS[p]
                Q0, Q1, Q2, Q3 = P[4 * p:4 * p + 4]
                for j in range(4):
                    K.op("dve", lambda e: e.tensor_scalar(out=cyv[:, j, :], in0=cv[:, j, 0:128], scalar1=cw[:, 4 + j, 0:1], scalar2=None, op0=ALU.mult), [cb_, cw], [cyb]); yield
                    for t in range(1, 4):
                        K.op("dve", lambda e: e.scalar_tensor_tensor(out=cyv[:, j, :], in0=cv[:, j, t:t + 128], scalar=cw[:, 4 + j, t:t + 1], in1=cyv[:, j, :],
                                                                     op0=ALU.mult, op1=ALU.add), [cb_, cw, cyb], [cyb]); yield
                tE = pT[:, 8:16, :].rearrange("p a b -> p (a b)").bitcast(F32).rearrange("p (j t) -> p j t", t=128)
                K.op("act", lambda e: e.activation(out=tE, in_=cyv[:], func=AF.Exp, scale=-1.0), [cyb], [tEb]); yield
                K.op("dve", lambda e: e.tensor_scalar(out=tE, in0=tE, scalar1=1.0, scalar2=None, op0=ALU.add), [tEb], [tEb]); yield
                K.op("dve", lambda e: e.reciprocal(out=tE, in_=tE), [tEb], [tEb]); yield
                K.op("dve", lambda e: e.scalar_tensor_tensor(out=kv_[:], in0=cyv[:], scalar=128.0 ** -0.5, in1=tE, op0=ALU.mult, op1=ALU.mult), [cyb, tEb], [kb]); yield
                (gib, giv), (geb, gev), (lfb, lfv), (gbb, gbv), (gab, gav), (ewb, ewv), (imbb, imbv) = (G[n] for n in g1n)
                K.op("dve", lambda e: e.tensor_scalar(out=giv[:], in0=Q0[0:4, 0:128], scalar1=ibc[:, 0:1], scalar2=valid[0:4, ci:ci + 1], op0=ALU.add, op1=ALU.mult), [Q0, ibc, valid], [gib]); yield
                K.op("dve", lambda e: e.tensor_scalar(out=giv[:], in0=giv[:], scalar1=pen[0:4, ci:ci + 1], scalar2=None, op0=ALU.add), [gib, pen], [gib]); yield
                K.op("act", lambda e: e.activation(out=gev[:], in_=Q0[0:4, 128:256], func=AF.Exp, bias=nfb[:, 0:1], scale=-1.0), [Q0, nfb], [geb]); yield
                K.op("act", lambda e: e.activation(out=gev[:], in_=gev[:], func=AF.Ln, bias=1.0), [geb], [geb]); yield
                K.op("dve", lambda e: e.tensor_scalar(out=lfv[:], in0=gev[:], scalar1=valid[0:4, ci:ci + 1], scalar2=-1.0, op0=ALU.mult, op1=ALU.mult), [geb, valid], [lfb]); yield
                K.op("dve", lambda e: e.tensor_tensor_scan(out=gbv[:], data0=g["one"][:], data1=lfv[:], initial=0.0, op0=ALU.mult, op1=ALU.add), [g["one"], lfb], [gbb]); yield
                K.op("dve", lambda e: e.tensor_tensor(out=imbv[:], in0=giv[:], in1=gbv[:], op=ALU.subtract), [gib, gbb], [imbb]); yield
                K.op("dve", lambda e: e.tensor_scalar(out=gav[:], in0=imbv[:], scalar1=gbv[:, 127:128], scalar2=None, op0=ALU.add), [imbb, gbb], [gab]); yield
                K.op("dve", lambda e: e.tensor_reduce(out=gsv[:, 0:1], in_=gav[:], axis=AX.X, op=ALU.max), [gab], [gsb]); yield
                K.op("dve", lambda e: e.tensor_tensor(out=gsv[:, 1:2], in0=gbv[:, 127:128], in1=mst[:], op=ALU.add), [gbb, mst], [gsb]); yield
                K.op("dve", lambda e: e.tensor_tensor(out=gsv[:, 2:3], in0=gsv[:, 1:2], in1=gsv[:, 0:1], op=ALU.max), [gsb], [gsb]); yield
                K.op("dve", lambda e: e.tensor_copy(out=mst[:], in_=gsv[:, 2:3]), [gsb], [mst]); yield
                K.op("dve", lambda e: e.tensor_scalar(out=gsv[:, 3:4], in0=gsv[:, 2:3], scalar1=-1.0, scalar2=None, op0=ALU.mult), [gsb], [gsb]); yield
                K.op("dve", lambda e: e.tensor_tensor(out=gsv[:, 4:5], in0=gsv[:, 1:2], in1=gsv[:, 2:3], op=ALU.subtract), [gsb], [gsb]); yield
                K.op("act", lambda e: e.activation(out=gsv[:, 5:6], in_=gsv[:, 4:5], func=AF.Exp), [gsb], [gsb]); yield
                K.op("act", lambda e: e.activation(out=ewv[:], in_=gav[:], func=AF.Exp, bias=gsv[:, 3:4]), [gab, gsb], [ewb]); yield
                K.op("dve", lambda e: e.tensor_scalar(out=dgv[:], in0=i4[:], scalar1=gsv[:, 5:6], scalar2=None, op0=ALU.mult), [i4, gsb], [dgb]); yield
                K.op("pe", lambda e: e.transpose(out=Q0[:, 256:260], in_=ewv[:, :], identity=ident[0:4, 0:4]), [ewb, ident], [Q0])
                K.op("pe", lambda e: e.matmul(Q0[:, 272:276], lhsT=ones[0:4, :], rhs=dgv[:, :], start=True, stop=True), [ones, dgb], [Q0])
                K.op("act", lambda e: e.activation(out=tmv[:, 0:20], in_=Q0[:, 256:276], func=AF.Copy), [Q0], [tmb]); yield
                pktv = Q1[:, :].bitcast(BF16).rearrange("p (a b) -> p a b", b=128)
                for h in range(4):
                    K.op("pe", lambda e: e.transpose(out=pktv[:, h, :], in_=kv_[:, h, :], identity=identb[:]), [kb, identb], [Q1])
                for h in range(4):
                    K.op("dve", lambda e: e.tensor_scalar(out=wkv[:, h, :], in0=pktv[:, h, :], scalar1=tmv[:, h:h + 1], scalar2=None, op0=ALU.mult), [Q1, tmb], [wkb]); yield
                for h in range(4):
                    dst = Q2[:, h * 129:(h + 1) * 129] if h < 3 else Q3[:, 0:129]
                    K.op("pe", lambda e: e.matmul(dst, lhsT=wkv[:, h, :], rhs=mvv[:, h, :], start=True, stop=True), [wkb, mb], [Q2 if h < 3 else Q3])
                for h in range(4):
                    src = Q2[:, h * 129:(h + 1) * 129] if h < 3 else Q3[:, 0:129]
                    K.op("dve", lambda e: e.scalar_tensor_tensor(out=Cst[:, h, :], in0=Cst[:, h, :], scalar=tmv[:, 16 + h:17 + h], in1=src,
                                                                 op0=ALU.mult, op1=ALU.add), [Cst, tmb, Q2 if h < 3 else Q3], [Cst]); yield

            def step(gn):
                if gn is None:
                    return None
                try:
                    next(gn); return gn
                except StopIteration:
                    return None

            NPP = max(NPRE - 1, 0)
            if NPP > 0:
                ga_gen = pfA(0, 0)
                while ga_gen is not None:
                    ga_gen = step(ga_gen)
                for ci in range(NPP):
                    gb_gen = pfB(ci, ci % 2)
                    ga_gen = pfA(ci + 1, (ci + 1) % 2) if ci + 1 < NPP else None
                    while gb_gen is not None or ga_gen is not None:
                        gb_gen = step(gb_gen)
                        ga_gen = step(ga_gen)
                lb, lv = PCIN[(NPP - 1) % 2]
                if (NPP - 1) % 2 == 1:
                    K.op("pool", lambda e: e.tensor_copy(out=cin[:, 4:8, 0:3], in_=lv[:, :, 128:131]), [lb], [cin])
                K.barrier()
                K.op("dve", lambda e: e.memset(mv[:], 1.0), [], [mv])
            if NPRE > 0:
                chunk(NPRE - 1, True, True, False)
            if NPRE == 0:
                K.op("dve", lambda e: e.memset(kT[1][:], 0.0), [], [kT[1]])
                K.op("dve", lambda e: e.memset(vsb[1][:], 0.0), [], [vsb[1]])
            for c in range(NOWN):
                chunk(c, False, True, True)
        K.barrier()
        if stage == 1:
            with ExitStack() as ed:
                tt = K.sb("dbgt", [128, D], F32, ed)
                for c in range(NOWN if DBG is None else 0):
                    K.dma("sp", lambda e: e.dma_start(out=tt[:], in_=hbuf_d[c * 128:(c + 1) * 128, :]), tt, [hbuf_d])
                    K.dma("sp", lambda e: e.dma_start(out=y[c * 128:(c + 1) * 128, :], in_=tt[:]), y, [tt])
                K.finish([y])
            return nc

        with ExitStack() as e2:
            stage = [K.sb("stg2_%d" % i, [128, 2048], F32, e2) for i in range(2)]
            wq = K.sb("wq", [128, 8, 2048], BF16, e2)
            load_bf16(wq, 8, 2048, wq_d.t.rearrange("(kc p) n -> p kc n", p=128), wq_d, stage)
            skT = K.sb("skT", [128, 2, 128], F32, e2)
            K.dma("sp", lambda e: e.dma_start(out=skT[:], in_=sk_d[:]), skT, [sk_d])
            A2 = K.sb("A2", [128, D], F32, e2); B2 = K.sb("B2", [128, D], F32, e2); G2 = K.sb("G2", [128, D], F32, e2)
            K.dma("sp", lambda e: e.dma_start(out=A2[:], in_=modbc_d[:, 4 * D:5 * D]), A2, [modbc_d])
            K.dma("sp", lambda e: e.dma_start(out=B2[:], in_=modbc_d[:, 3 * D:4 * D]), B2, [modbc_d])
            K.dma("sp", lambda e: e.dma_start(out=G2[:], in_=modbc_d[:, 5 * D:6 * D]), G2, [modbc_d])
            NF = K.sb("NF", [128, D], F32, e2); bcast_load(NF, nfw_d[:], nfw_d)
            iota16 = K.sb("iota16", [128, 16], F32, e2)
            K.dma("sp", lambda e: e.dma_start(out=iota16[:], in_=iota_d[:]), iota16, [iota_d])
            ht = [K.sb("ht%d" % i, [128, D], F32, e2) for i in range(2)]
            u2 = K.sb("u2", [128, D], F32, e2)
            junk = K.sb("junk2", [128, 512], F32, e2); junkf = K.sb("junkf", [128, D], F32, e2)
            ssq = K.sb("ssq2", [128, 4], F32, e2); rstd = K.sb("rstd2", [128, 1], F32, e2)
            u2T = K.sb("u2T", [128, 8, 128], BF16, e2)
            qyT = K.sb("qyT", [128, 16, 128], F32, e2)
            sc = K.sb("sc", [128, 16, 128], F32, e2); sc2 = K.sb("sc2", [128, 16, 128], F32, e2)
            tv = K.sb("tv", [128, 16, 16], F32, e2); ti = K.sb("ti", [128, 16, 16], U32, e2)
            tif = K.sb("tif", [128, 16, 16], F32, e2)
            cand = K.sb("cand", [128, 8, 256], F32, e2); cand2 = K.sb("cand2", [128, 8, 256], F32, e2)
            bs = K.sb("bs", [128, 8, 16], F32, e2); bp = K.sb("bp", [128, 8, 16], U32, e2)
            au = K.sb("au", [128, 8, 16], U32, e2); bu = K.sb("bu", [128, 8, 16], U32, e2)
            af = K.sb("af", [128, 8, 16], F32, e2); bfl = K.sb("bfl", [128, 8, 16], F32, e2)
            eq = K.sb("eq", [128, 8, 16, 16], F32, e2)
            isl = K.sb("isl", [128, 8, 16], F32, e2); jsl = K.sb("jsl", [128, 8, 16], F32, e2)
            idxf = K.sb("idxf", [128, 128], F32, e2); idx = K.sb("idx", [128, 128], I32, e2)
            gsm = K.sb("gsm", [128, 24], F32, e2)
            gate = K.sb("gate", [128, 8, 16], F32, e2)
            dots = K.sb("dots", [128, 128], F32, e2); wsl = K.sb("wsl", [128, 128], F32, e2)
            acc = K.sb("acc", [128, D], F32, e2)
            R = 10
            ring = [K.sb("ring%d" % i, [128, D], F32, e2) for i in range(R)]
            yt = K.sb("yt", [128, D], F32, e2)
            tmpr = [K.sb("tmpr%d" % i, [128, D], BF16, e2) for i in range(3)]
            class _RB:
                def __init__(s_, b): s_.b = b
                def __getitem__(s_, k): return s_.b[:].bitcast(BF16)[:, 0:D][k]
            rcount = [0]
            blk = 0
            for (src_t, dst_t) in ((pu_d, pub_d), (pv_d, pvb_d)):
                sv_ = src_t.t.rearrange("(p q) d -> p q d", q=128)
                dv_ = dst_t.t.rearrange("(p q) d -> p q d", q=128)
                for q in range(128):
                    rbf = ring[blk % R]; tbf = tmpr[blk % 3]
                    K.dma("sp", lambda e: e.dma_start(out=rbf[:], in_=sv_[:, q, :]), rbf, [src_t])
                    engs = ("dve", "act")
                    en = engs[blk % 2]
                    if en == "act":
                        K.op("act", lambda e: e.activation(out=tbf[:], in_=rbf[:], func=AF.Copy), [rbf], [tbf])
                    else:
                        K.op(en, lambda e: e.tensor_copy(out=tbf[:], in_=rbf[:]), [rbf], [tbf])
                    K.dma("pool", lambda e: e.dma_start(out=dv_[:, q, :], in_=tbf[:]), dst_t, [tbf], waw=False)
                    blk += 1

            def gather(src_d, col, idx):
                rb = ring[rcount[0] % R]; rcount[0] += 1
                K.dma("pool", lambda e: e.indirect_dma_start(out=rb[:].bitcast(BF16)[:, 0:D], out_offset=None, in_=src_d[:, :],
                                                             in_offset=bass.IndirectOffsetOnAxis(ap=idx[:, col:col + 1], axis=0)),
                      rb, [src_d, idx])
                return rb

            idxs = [idx, K.sb("idx_b", [128, 128], I32, e2)]

            def front(c):
                h_ = ht[c % 2]
                idx = idxs[c % 2]
                K.dma("sp", lambda e: e.dma_start(out=h_[:], in_=hbuf_d[c * 128:(c + 1) * 128, :]), h_, [hbuf_d])
                rmsnorm_mod(h_, u2, A2, B2, (junk, ssq, rstd))
                transpose_to(u2, u2T, 8, P[0], P[1])
                for hp in range(16):
                    pb = P[2 + hp // 4]
                    q = hp % 4
                    for kc in range(8):
                        K.op("pe", lambda e: e.matmul(pb[:, q * 128:(q + 1) * 128], lhsT=wq[:, kc, hp * 128:(hp + 1) * 128], rhs=u2T[:, kc, :],
                                                      start=(kc == 0), stop=(kc == 7)), [wq, u2T], [pb])
                    if q == 3:
                        K.op("act", lambda e: e.activation(out=qyT[:, hp - 3:hp + 1, :], in_=pb[:, :].rearrange("p (a b) -> p a b", b=128),
                                                           func=AF.Copy), [pb], [qyT])
                        yield
                for hp in range(16):
                    pb = P[2 + hp // 4]
                    q = hp % 4
                    K.op("pe", lambda e: e.matmul(pb[:, q * 128:(q + 1) * 128], lhsT=qyT[:, hp, :], rhs=skT[:, hp % 2, :], start=True, stop=True),
                         [qyT, skT], [pb])
                    if q == 3:
                        K.op("act", lambda e: e.activation(out=sc[:, hp - 3:hp + 1, :], in_=pb[:, :].rearrange("p (a b) -> p a b", b=128),
                                                           func=AF.Copy), [pb], [sc])
                        yield
                for hp in range(16):
                    K.op("dve", lambda e: e.max(out=tv[:, hp, 0:8], in_=sc[:, hp, :]), [sc], [tv])
                    yield
                    K.op("dve", lambda e: e.max_index(out=ti[:, hp, 0:8], in_max=tv[:, hp, 0:8], in_values=sc[:, hp, :]), [sc, tv], [ti])
                    yield
                    K.op("dve", lambda e: e.match_replace(out=sc2[:, hp, :], in_to_replace=tv[:, hp, 0:8], in_values=sc[:, hp, :], imm_value=-1e30),
                         [sc, tv], [sc2])
                    yield
                    K.op("dve", lambda e: e.max(out=tv[:, hp, 8:16], in_=sc2[:, hp, :]), [sc2], [tv])
                    yield
                    K.op("dve", lambda e: e.max_index(out=ti[:, hp, 8:16], in_max=tv[:, hp, 8:16], in_values=sc2[:, hp, :]), [sc2, tv], [ti])
                    yield
                tv4 = tv[:].rearrange("p (h s) k -> p h s k", s=2)
                K.op("dve", lambda e: e.tensor_tensor(out=cand[:].rearrange("p h (a b) -> p h a b", b=16),
                                                      in0=tv4[:, :, 0, :].unsqueeze(3).to_broadcast([128, 8, 16, 16]),
                                                      in1=tv4[:, :, 1, :].unsqueeze(2).to_broadcast([128, 8, 16, 16]), op=ALU.add), [tv], [cand])
                yield
                for h in range(8):
                    K.op("dve", lambda e: e.max(out=bs[:, h, 0:8], in_=cand[:, h, :]), [cand], [bs])
                    yield
                    K.op("dve", lambda e: e.max_index(out=bp[:, h, 0:8], in_max=bs[:, h, 0:8], in_values=cand[:, h, :]), [cand, bs], [bp])
                    yield
                    K.op("dve", lambda e: e.match_replace(out=cand2[:, h, :], in_to_replace=bs[:, h, 0:8], in_values=cand[:, h, :], imm_value=-1e30),
                         [cand, bs], [cand2])
                    yield
                    K.op("dve", lambda e: e.max(out=bs[:, h, 8:16], in_=cand2[:, h, :]), [cand2], [bs])
                    yield
                    K.op("dve", lambda e: e.max_index(out=bp[:, h, 8:16], in_max=bs[:, h, 8:16], in_values=cand2[:, h, :]), [cand2, bs], [bp])
                    yield
                K.op("dve", lambda e: e.tensor_scalar(out=au[:], in0=bp[:], scalar1=4, scalar2=None, op0=ALU.logical_shift_right), [bp], [au])
                yield
                K.op("dve", lambda e: e.tensor_scalar(out=bu[:], in0=bp[:], scalar1=15, scalar2=None, op0=ALU.bitwise_and), [bp], [bu])
                yield
                K.op("dve", lambda e: e.tensor_copy(out=af[:], in_=au[:]), [au], [af])
                yield
                K.op("dve", lambda e: e.tensor_copy(out=bfl[:], in_=bu[:]), [bu], [bfl])
                yield
                K.op("dve", lambda e: e.tensor_copy(out=tif[:], in_=ti[:]), [ti], [tif])
                yield
                tif4 = tif[:].rearrange("p (h s) k -> p h s k", s=2)
                for (srcf, side, dst) in ((af, 0, isl), (bfl, 1, jsl)):
                    K.op("dve", lambda e: e.tensor_tensor(out=eq[:], in0=iota16[:].unsqueeze(1).unsqueeze(1).to_broadcast([128, 8, 16, 16]),
                                                          in1=srcf[:].unsqueeze(3).to_broadcast([128, 8, 16, 16]), op=ALU.is_equal), [iota16, srcf], [eq])
                    yield
                    K.op("dve", lambda e: e.tensor_tensor(out=eq[:], in0=eq[:], in1=tif4[:, :, side, :].unsqueeze(2).to_broadcast([128, 8, 16, 16]),
                                                          op=ALU.mult), [eq, tif], [eq])
                    yield
                    K.op("dve", lambda e: e.tensor_reduce(out=dst[:], in_=eq[:], axis=AX.X, op=ALU.add), [eq], [dst])
                    yield
                K.op("dve", lambda e: e.scalar_tensor_tensor(out=idxf[:], in0=isl[:].rearrange("p h k -> p (h k)"), scalar=128.0,
                                                             in1=jsl[:].rearrange("p h k -> p (h k)"), op0=ALU.mult, op1=ALU.add), [isl, jsl], [idxf])
                yield
                K.op("dve", lambda e: e.tensor_scalar(out=idxf[:], in0=idxf[:], scalar1=0.0, scalar2=16383.0, op0=ALU.max, op1=ALU.min), [idxf], [idxf])
                yield
                K.op("dve", lambda e: e.tensor_copy(out=idx[:], in_=idxf[:]), [idxf], [idx])
                yield
                K.op("dve", lambda e: e.tensor_tensor(out=gate[:], in0=bs[:], in1=bs[:, :, 0:1].to_broadcast([128, 8, 16]), op=ALU.subtract), [bs], [gate])
                yield
                K.op("act", lambda e: e.activation(out=gate[:], in_=gate[:], func=AF.Exp), [gate], [gate])
                yield
                K.op("dve", lambda e: e.tensor_reduce(out=gsm[:, 0:8], in_=gate[:], axis=AX.X, op=ALU.add), [gate], [gsm])
                yield
                K.op("dve", lambda e: e.reciprocal(out=gsm[:, 8:16], in_=gsm[:, 0:8]), [gsm], [gsm])
                yield
                K.op("dve", lambda e: e.tensor_tensor(out=gate[:], in0=gate[:], in1=gsm[:, 8:16].unsqueeze(2).to_broadcast([128, 8, 16]), op=ALU.mult),
                     [gate, gsm], [gate])
                yield
                yield

            def drain(g, n=None):
                k = 0
                while g is not None and (n is None or k < n):
                    try:
                        next(g)
                    except StopIteration:
                        return None
                    k += 1
                return g

            gen = drain(front(0))
            for c in range(NOWN):
                h_ = ht[c % 2]
                idx = idxs[c % 2]
                for s in range(128):
                    rb = gather(pub_d, s, idx); rbv = _RB(rb)
                    K.op("dve", lambda e: e.scalar_tensor_tensor(out=junkf[:], in0=u2[:], scalar=1.0, in1=rbv[:], op0=ALU.mult, op1=ALU.mult,
                                                                 accum_out=dots[:, s:s + 1]), [u2, rb], [junkf, dots])
                K.op("act", lambda e: e.activation(out=wsl[:], in_=dots[:], func=AF.Gelu), [dots], [wsl])
                K.op("dve", lambda e: e.tensor_tensor(out=wsl[:], in0=wsl[:], in1=gate[:].rearrange("p h k -> p (h k)"), op=ALU.mult), [wsl, gate], [wsl])
                gen = front(c + 1) if c + 1 < NOWN else None
                for s in range(128):
                    rb = gather(pvb_d, s, idx); rbv = _RB(rb)
                    tb = tmpr[s % 3]
                    K.op("act", lambda e: e.activation(out=tb[:], in_=rbv[:], func=AF.Copy, scale=wsl[:, s:s + 1]), [rb, wsl], [tb])
                    for nh in range(2):
                        K.op("pe", lambda e: e.matmul(P[6 + nh][:, :], lhsT=identb[:], rhs=tb[:, nh * 512:(nh + 1) * 512],
                                                      start=(s == 0), stop=(s == 127)), [identb, tb], [P[6 + nh]])
                    gen = drain(gen, 2)
                gen = drain(gen)
                for nh in range(2):
                    K.op("dve", lambda e: e.tensor_tensor(out=acc[:, nh * 512:(nh + 1) * 512], in0=P[6 + nh][:, :], in1=G2[:, nh * 512:(nh + 1) * 512],
                                                          op=ALU.mult), [P[6 + nh], G2], [acc])
                K.op("pool", lambda e: e.tensor_tensor(out=acc[:], in0=acc[:], in1=h_[:], op=ALU.add), [acc, h_], [acc])
                rmsnorm_mod(acc, yt, NF, None, (junk, ssq, rstd))
                K.dma("sp", lambda e: e.dma_start(out=y[c * 128:(c + 1) * 128, :], in_=yt[:]), y, [yt])
            K.finish([y])
    return nc


SEQ_FULL = 16384
ONLY = None
DBG = None
_cache = {}


def _consts():
    ident = np.eye(128, dtype=np.float32)
    tri = np.triu(np.ones((128, 128), np.float32))
    sel = np.zeros((4, 4, 128), np.float32)
    for h in range(4):
        sel[h, h, :] = 1.0
    i4 = np.eye(4, dtype=np.float32)
    iota16 = np.tile(np.arange(16, dtype=np.float32)[None, :], (128, 1))
    qi = np.arange(128)[:, None]; ki = np.arange(256)[None, :]
    rel = qi + 128 - ki
    ok = (rel >= 0) & (rel < 128)
    maskN = np.where(ok, 0.0, NEG).astype(np.float32)
    mask0 = np.where(ok & (ki >= 128), 0.0, NEG).astype(np.float32)
    return dict(ident=ident, tri=tri, sel=sel, i4=i4, iota16=iota16, maskN=maskN, mask0=mask0)


def run(inputs, S, ncore_per_seq, NOWN, stage=2):
    x = np.asarray(inputs["x"], np.float32)
    B = x.shape[0]
    NPRE = (ncore_per_seq - 1) * NOWN
    key = (NPRE, NOWN, stage)
    if key not in _cache:
        _cache[key] = build(NPRE, NOWN, stage)
    nc = _cache[key]
    f = lambda k: np.ascontiguousarray(np.asarray(inputs[k], np.float32))
    cst = _consts()
    half = 32
    inv_freq = (np.float32(10000.0) ** (-np.arange(half, dtype=np.float32) * np.float32(2.0) / np.float32(64))).astype(np.float32)
    shared = dict(
        w_ada=f("w_ada")[0], b_ada=f("b_ada")[0], norm1_w=f("norm1_w")[0], norm2_w=f("norm2_w")[0], norm_f_w=f("norm_f_w"),
        w_in=f("w_in")[0],
        conv_w=np.ascontiguousarray(f("conv_w")[0].reshape(4, 8, 128).transpose(2, 1, 0)),
        att_sinks=f("att_sinks")[0], i_bias=f("i_bias")[0].reshape(4, 1), f_bias=f("f_bias")[0].reshape(4, 1),
        mlstm_norm_w=f("mlstm_norm_w")[0], w_att=f("w_att_branch")[0], w_ml=f("w_mlstm_branch")[0], w_out=f("w_out")[0],
        wq=f("peer_w_query")[0], subkT=np.ascontiguousarray(f("peer_sub_keys")[0].transpose(2, 0, 1)),
        pu=f("peer_u")[0], pv=f("peer_v")[0],
        ident=cst["ident"], tri=cst["tri"], sel=cst["sel"], i4=cst["i4"], iota16=cst["iota16"], maskN=cst["maskN"],
    )
    c = f("c")
    in_maps = []
    T = NOWN * 128
    for b in range(B):
        for j in range(ncore_per_seq):
            start = j * T
            xo = np.ascontiguousarray(x[b, start:start + T])
            NP1 = max(NPRE, 1)
            xp = np.zeros((NP1 * 128, D), np.float32)
            valid = np.zeros((128, NP1), np.float32)
            if NPRE > 0 and start > 0:
                xp[NPRE * 128 - start:] = x[b, :start]
                valid[:, NPRE - start // 128:] = 1.0
            pos = (np.arange(start - 128, start + T, dtype=np.float32))
            ang = (pos[:, None] * inv_freq[None, :]).astype(np.float32)
            m = dict(shared)
            m.update(xo=xo, xp=xp, valid=valid, mask0=(cst["mask0"] if j == 0 else cst["maskN"]),
                     cosd=np.cos(ang).astype(np.float32), sind=np.sin(ang).astype(np.float32),
                     cT=np.ascontiguousarray(c[b].reshape(8, 128).T))
            in_maps.append(m)
    n = len(in_maps)
    res = run_bass_kernel_spmd(nc, in_maps, core_ids=list(range(n)))
    out = np.zeros((B, S, D), np.float32)
    k = 0
    for b in range(B):
        for j in range(ncore_per_seq):
            out[b, j * T:(j + 1) * T] = res.results[k]["y"]
            k += 1
    return out


def kernel(**inputs):
    return run(inputs, SEQ_FULL, 4, 32)
```

```python
import numpy as np
from contextlib import ExitStack
import concourse.bass as bass
import concourse.mybir as mybir
from concourse.bass_utils import run_bass_kernel_spmd

F32 = mybir.dt.float32
BF16 = mybir.dt.bfloat16
I32 = mybir.dt.int32
U32 = mybir.dt.uint32
AF = mybir.ActivationFunctionType
ALU = mybir.AluOpType
AX = mybir.AxisListType

D = 1024
NPROJ = 4872
EPS = 1e-6
NEG = -30000.0


class Buf:
    def __init__(self, t, name):
        self.t = t
        self.name = name
        self.w = None
        self.r = []
        self.dsem = None
        self.dcnt = 0

    def __getitem__(self, k):
        return self.t[k]


class Eng:
    def __init__(self, e, sem, name, is_pe=False):
        self.e, self.sem, self.name, self.cnt, self.is_pe = e, sem, name, 0, is_pe
        self.seen = {}


class Ctx:
    def __init__(self, nc, es):
        self.nc, self.es = nc, es
        self.engs = {}
        for nm, e in (("pe", nc.tensor), ("dve", nc.vector), ("act", nc.scalar),
                      ("pool", nc.gpsimd), ("sp", nc.sync)):
            sem = es.enter_context(nc.semaphore("s_" + nm))
            self.engs[nm] = Eng(e, sem, nm, is_pe=(nm == "pe"))
        self.bufs = []
        self.nsem = 0

    def sb(self, name, shape, dt, es=None):
        t = (es or self.es).enter_context(self.nc.sbuf_tensor("sb_" + name, shape, dt))
        b = Buf(t, name); self.bufs.append(b); return b

    def ps(self, name, shape, dt=F32, es=None):
        t = (es or self.es).enter_context(self.nc.psum_tensor("ps_" + name, shape, dt))
        b = Buf(t, name); self.bufs.append(b); return b

    def dram(self, name, shape, dt, kind="Internal"):
        t = self.nc.dram_tensor(name, shape, dt, kind=kind).ap()
        b = Buf(t, name); self.bufs.append(b); return b

    def view(self, buf, name):
        b = Buf(buf.t, name); self.bufs.append(b); return b

    def _wait(self, eng, deps):
        best = {}
        for (sem, val) in deps:
            k = id(sem)
            if k not in best or best[k][1] < val:
                best[k] = (sem, val)
        for k, (sem, val) in best.items():
            if eng.is_pe and sem is eng.sem:
                continue
            if eng.seen.get(k, 0) >= val:
                continue
            eng.e.wait_ge(sem, val)
            eng.seen[k] = val

    def _deps(self, reads, writes):
        deps = []
        for b in reads:
            if b.w: deps.append(b.w)
        for b in writes:
            if b.w: deps.append(b.w)
            deps.extend(b.r)
        return deps

    def op(self, en, fn, reads=(), writes=()):
        eng = self.engs[en]
        self._wait(eng, self._deps(reads, writes))
        ins = fn(eng.e)
        eng.cnt += 1
        ins.then_inc(eng.sem, 1)
        tok = (eng.sem, eng.cnt)
        for b in writes:
            b.w = tok; b.r = []
        for b in reads:
            if b not in writes:
                b.r.append(tok)

    def dma(self, en, fn, dst, srcs=(), waw=True):
        eng = self.engs[en]
        self._wait(eng, self._deps(srcs, [dst] if waw else []))
        if dst.dsem is None:
            dst.dsem = self.es.enter_context(self.nc.semaphore("d%d" % self.nsem))
            self.nsem += 1
        ins = fn(eng.e)
        dst.dcnt += 1
        ins.then_inc(dst.dsem, 16)
        tok = (dst.dsem, 16 * dst.dcnt)
        dst.w = tok; dst.r = []
        for b in srcs:
            b.r.append(tok)

    def barrier(self):
        toks = []
        for b in self.bufs:
            if b.w: toks.append(b.w)
            toks.extend(b.r)
        for e in self.engs.values():
            if e.cnt: toks.append((e.sem, e.cnt))
        for e in self.engs.values():
            self._wait(e, toks)
        for b in self.bufs:
            b.w = None; b.r = []

    def finish(self, bufs):
        eng = self.engs["sp"]
        self._wait(eng, [b.w for b in bufs if b.w])


def build(NPRE, NOWN, stage=2):
    nc = bass.Bass("TRN2", target_bir_lowering=False)
    es = ExitStack()
    with es:
        K = Ctx(nc, es)

        def din(name, shape, dt=F32):
            return K.dram(name, shape, dt, kind="ExternalInput")
        NP1 = max(NPRE, 1)
        xo = din("xo", [NOWN * 128, D]); xp = din("xp", [NP1 * 128, D])
        valid_d = din("valid", [128, NP1])
        mask0_d = din("mask0", [128, 256]); maskN_d = din("maskN", [128, 256])
        cos_d = din("cosd", [(NOWN + 1) * 128, 32]); sin_d = din("sind", [(NOWN + 1) * 128, 32])
        cT_d = din("cT", [128, 8])
        w_ada_d = din("w_ada", [D, 6 * D]); b_ada_d = din("b_ada", [6 * D])
        n1w_d = din("norm1_w", [D]); n2w_d = din("norm2_w", [D]); nfw_d = din("norm_f_w", [D])
        w_in_d = din("w_in", [D, NPROJ])
        cw_d = din("conv_w", [128, 8, 4])
        sink_d = din("att_sinks", [8]); ib_d = din("i_bias", [4, 1]); fb_d = din("f_bias", [4, 1])
        mnw_d = din("mlstm_norm_w", [512])
        wa_d = din("w_att", [512, D]); wb_d = din("w_ml", [512, D]); wo_d = din("w_out", [D, D])
        wq_d = din("wq", [D, 2048]); sk_d = din("subkT", [128, 2, 128])
        pu_d = din("pu", [16384, D]); pv_d = din("pv", [16384, D])
        ident_d = din("ident", [128, 128]); tri_d = din("tri", [128, 128])
        sel_d = din("sel", [4, 4, 128]); i4_d = din("i4", [4, 4]); iota_d = din("iota16", [128, 16])
        y = K.dram("y", [NOWN * 128, D], F32, kind="ExternalOutput")
        modbc_d = K.dram("modbc", [128, 6 * D], F32)
        hbuf_d = K.dram("hbuf", [NOWN * 128, D], F32)
        pub_d = K.dram("pub", [16384, D], BF16)
        pvb_d = K.dram("pvb", [16384, D], BF16)

        ident = K.sb("ident", [128, 128], F32); identb = K.sb("identb", [128, 128], BF16)
        K.dma("sp", lambda e: e.dma_start(out=ident[:], in_=ident_d[:]), ident, [ident_d])
        K.op("dve", lambda e: e.tensor_copy(out=identb[:], in_=ident[:]), [ident], [identb])
        ones = K.sb("ones", [128, 128], F32)
        K.op("dve", lambda e: e.memset(ones[:], 1.0), [], [ones])
        P = [K.ps("P%d" % i, [128, 512], F32) for i in range(8)]

        def bcast_load(dst, src_ap, src_buf):
            K.dma("sp", lambda e: e.dma_start(out=dst[:], in_=src_ap.partition_broadcast(128)), dst, [src_buf])

        with ExitStack() as e0:
            cT = K.sb("cT", [128, 8], F32, e0); cact = K.sb("cact", [128, 8], F32, e0)
            crep = K.sb("crep", [128, 8, 128], BF16, e0)
            K.dma("sp", lambda e: e.dma_start(out=cT[:], in_=cT_d[:]), cT, [cT_d])
            K.op("act", lambda e: e.activation(out=cact[:], in_=cT[:], func=AF.Silu), [cT], [cact])
            K.op("dve", lambda e: e.tensor_copy(out=crep[:], in_=cact[:].unsqueeze(2).to_broadcast([128, 8, 128])),
                 [cact], [crep])
            wst = [K.sb("wst%d" % i, [128, 8, 512], F32, e0) for i in range(2)]
            wsb = [K.sb("wsb%d" % i, [128, 8, 512], BF16, e0) for i in range(2)]
            badab = K.sb("badab", [128, 6 * D], F32, e0)
            bcast_load(badab, b_ada_d[:], b_ada_d)
            modsb = K.sb("modsb", [128, 6 * D], F32, e0)
            wv = w_ada_d.t.rearrange("(kc p) n -> p kc n", p=128)
            for j in range(12):
                ws = wst[j % 2]
                K.dma("sp", lambda e: e.dma_start(out=ws[:], in_=wv[:, :, j * 512:(j + 1) * 512]), ws, [w_ada_d])
                pj = P[j % 2]
                wb_ = wsb[j % 2]
                K.op("act" if j % 2 else "dve", (lambda e: e.activation(out=wb_[:], in_=ws[:], func=AF.Copy)) if j % 2 else
                     (lambda e: e.tensor_copy(out=wb_[:], in_=ws[:])), [ws], [wb_])
                for kc in range(8):
                    K.op("pe", lambda e: e.matmul(pj[:, :], lhsT=crep[:, kc, :], rhs=wb_[:, kc, :],
                                                  start=(kc == 0), stop=(kc == 7)), [crep, wb_], [pj])
                K.op("dve", lambda e: e.tensor_tensor(out=modsb[:, j * 512:(j + 1) * 512], in0=pj[:, :],
                                                      in1=badab[:, j * 512:(j + 1) * 512], op=ALU.add),
                     [pj, badab], [modsb])
            nwb = K.sb("nwb", [128, D], F32, e0)
            for (wd, slot) in ((n1w_d, 1), (n2w_d, 4)):
                bcast_load(nwb, wd[:], wd)
                K.op("dve", lambda e: e.scalar_tensor_tensor(out=modsb[:, slot * D:(slot + 1) * D],
                                                             in0=modsb[:, slot * D:(slot + 1) * D], scalar=1.0,
                                                             in1=nwb[:], op0=ALU.add, op1=ALU.mult),
                     [modsb, nwb], [modsb])
            K.dma("sp", lambda e: e.dma_start(out=modbc_d[:], in_=modsb[:]), modbc_d, [modsb])
        K.barrier()

        def load_bf16(dst, nrow_chunks, ncols, src_view, src_buf, stage):
            i = 0
            for kc in range(nrow_chunks):
                for c0 in range(0, ncols, 2048):
                    c1 = min(ncols, c0 + 2048)
                    st = stage[i % 2]; i += 1
                    pp = dst.t.shape[0]
                    K.dma("sp", lambda e: e.dma_start(out=st[0:pp, 0:c1 - c0], in_=src_view[:, kc, c0:c1]), st, [src_buf])
                    eng = "act" if (i % 2) else "dve"
                    if eng == "act":
                        K.op("act", lambda e: e.activation(out=dst[:, kc, c0:c1], in_=st[0:pp, 0:c1 - c0], func=AF.Copy), [st], [dst])
                    else:
                        K.op("dve", lambda e: e.tensor_copy(out=dst[:, kc, c0:c1], in_=st[0:pp, 0:c1 - c0]), [st], [dst])

        def rmsnorm_mod(xt, ut, A, B, sm):
            junk, ssq, rstd = sm
            K.op("act", lambda e: e.activation(out=junk[:].bitcast(BF16), in_=xt[:], func=AF.Square, accum_out=ssq[:, 0:1]), [xt], [junk, ssq])
            K.op("dve", lambda e: e.tensor_scalar(out=ssq[:, 1:2], in0=ssq[:, 0:1], scalar1=1.0 / D, scalar2=EPS,
                                                  op0=ALU.mult, op1=ALU.add), [ssq], [ssq])
            K.op("act", lambda e: e.activation(out=ssq[:, 2:3], in_=ssq[:, 1:2], func=AF.Ln), [ssq], [ssq])
            K.op("act", lambda e: e.activation(out=rstd[:], in_=ssq[:, 2:3], func=AF.Exp, scale=-0.5), [ssq], [rstd])
            K.op("dve", lambda e: e.scalar_tensor_tensor(out=ut[:], in0=xt[:], scalar=rstd[:, 0:1], in1=A[:],
                                                         op0=ALU.mult, op1=ALU.mult), [xt, rstd, A], [ut])
            if B is not None:
                K.op("pool", lambda e: e.tensor_tensor(out=ut[:], in0=ut[:], in1=B[:], op=ALU.add), [ut, B], [ut])

        def transpose_to(src, dstT, nk, pa, pb, dt32=True, srcbuf=None):
            srcbuf = srcbuf or src
            idn = ident if dt32 else identb
            for half in range((nk + 3) // 4):
                pb_ = (pa, pb)[half % 2]
                n = min(4, nk - half * 4)
                if dt32:
                    pv = pb_[:, :].rearrange("p (a b) -> p a b", b=128)
                else:
                    pv = pb_[:, :].bitcast(BF16).rearrange("p (a b) -> p a b", b=128)
                for q in range(n):
                    kc = half * 4 + q
                    K.op("pe", lambda e: e.transpose(out=pv[:, q, :], in_=src[:, kc * 128:(kc + 1) * 128], identity=idn[:]),
                         [srcbuf, idn], [pb_])
                K.op("act", lambda e: e.activation(out=dstT[:, half * 4:half * 4 + n, :], in_=pv[:, 0:n, :], func=AF.Copy),
                     [pb_], [dstT])

        with ExitStack() as e1:
            w_in = K.sb("w_in", [128, 8, NPROJ], BF16, e1)
            wa = K.sb("wa", [64, 8, D], BF16, e1)
            wb = K.sb("wb", [128, 4, D], BF16, e1)
            wo = K.sb("wo", [128, 8, D], BF16, e1)
            with ExitStack() as est:
                stage = [K.sb("stg%d" % i, [128, 2048], F32, est) for i in range(2)]
                load_bf16(w_in, 8, NPROJ, w_in_d.t.rearrange("(kc p) n -> p kc n", p=128), w_in_d, stage)
                load_bf16(wa, 8, D, wa_d.t.rearrange("(h p) n -> p h n", p=64), wa_d, stage)
                load_bf16(wb, 4, D, wb_d.t.rearrange("(kc p) n -> p kc n", p=128), wb_d, stage)
                load_bf16(wo, 8, D, wo_d.t.rearrange("(kc p) n -> p kc n", p=128), wo_d, stage)
                K.barrier()
            A1 = K.sb("A1", [128, D], F32, e1); B1 = K.sb("B1", [128, D], F32, e1); G1 = K.sb("G1", [128, D], F32, e1)
            K.dma("sp", lambda e: e.dma_start(out=A1[:], in_=modbc_d[:, 1 * D:2 * D]), A1, [modbc_d])
            K.dma("sp", lambda e: e.dma_start(out=B1[:], in_=modbc_d[:, 0:D]), B1, [modbc_d])
            K.dma("sp", lambda e: e.dma_start(out=G1[:], in_=modbc_d[:, 2 * D:3 * D]), G1, [modbc_d])
            cw = K.sb("cw", [128, 8, 4], F32, e1)
            K.dma("sp", lambda e: e.dma_start(out=cw[:], in_=cw_d[:]), cw, [cw_d])
            sinkb = K.sb("sinkb", [128, 8], F32, e1); bcast_load(sinkb, sink_d[:], sink_d)
            mnwb = K.sb("mnwb", [128, 512], F32, e1); bcast_load(mnwb, mnw_d[:], mnw_d)
            ibc = K.sb("ibc", [4, 1], F32, e1); fbc = K.sb("fbc", [4, 1], F32, e1)
            K.dma("sp", lambda e: e.dma_start(out=ibc[:], in_=ib_d[:]), ibc, [ib_d])
            K.dma("sp", lambda e: e.dma_start(out=fbc[:], in_=fb_d[:]), fbc, [fb_d])
            nfb = K.sb("nfb", [4, 1], F32, e1)
            K.op("dve", lambda e: e.tensor_scalar(out=nfb[:], in0=fbc[:], scalar1=-1.0, scalar2=None, op0=ALU.mult), [fbc], [nfb])
            valid = K.sb("valid", [128, NP1], F32, e1); pen = K.sb("pen", [128, NP1], F32, e1)
            K.dma("sp", lambda e: e.dma_start(out=valid[:], in_=valid_d[:]), valid, [valid_d])
            K.op("dve", lambda e: e.tensor_scalar(out=pen[:], in0=valid[:], scalar1=-1.0, scalar2=1e30, op0=ALU.add, op1=ALU.mult),
                 [valid], [pen])
            masks = [K.sb("mask0", [128, 256], F32, e1), K.sb("maskN", [128, 256], F32, e1)]
            K.dma("sp", lambda e: e.dma_start(out=masks[0][:], in_=mask0_d[:]), masks[0], [mask0_d])
            K.dma("sp", lambda e: e.dma_start(out=masks[1][:], in_=maskN_d[:]), masks[1], [maskN_d])
            tri = K.sb("tri", [128, 128], F32, e1)
            K.dma("sp", lambda e: e.dma_start(out=tri[:], in_=tri_d[:]), tri, [tri_d])
            sel = K.sb("sel", [4, 4, 128], F32, e1); i4 = K.sb("i4", [4, 4], F32, e1)
            K.dma("sp", lambda e: e.dma_start(out=sel[:], in_=sel_d[:]), sel, [sel_d])
            K.dma("sp", lambda e: e.dma_start(out=i4[:], in_=i4_d[:]), i4, [i4_d])

            xt = [K.sb("xt0", [128, D], F32, e1)] * 2
            ut = K.sb("ut", [128, D], F32, e1)
            junkF = K.sb("junk", [128, 512], F32, e1)
            ssq = K.sb("ssq", [128, 4], F32, e1); rstd = K.sb("rstd", [128, 1], F32, e1)
            uT = K.sb("uT", [128, 8, 128], BF16, e1)
            qk = K.sb("qk", [128, 640], F32, e1)
            vsb = [K.sb("vsb%d" % i, [128, 2, 64], BF16, e1) for i in range(2)]
            mv = K.sb("mv", [128, 4, 129], BF16, e1)
            K.op("dve", lambda e: e.memset(mv[:], 1.0), [], [mv])
            so = K.sb("so", [128, 512], F32, e1)
            sg = K.sb("sg", [128, 2048], BF16, e1)
            cin = K.sb("cin", [128, 8, 131], F32, e1)
            K.op("dve", lambda e: e.memset(cin[:], 0.0), [], [cin])
            qkT = K.sb("qkT", [128, 8, 128], BF16, e1)
            cs = K.sb("cs", [128, 32], F32, e1); sn = K.sb("sn", [128, 32], F32, e1)
            qkr = K.sb("qkr", [128, 10, 64], BF16, e1)
            qT = K.sb("qT", [64, 8, 128], BF16, e1)
            kT = [K.sb("kT%d" % i, [64, 2, 128], BF16, e1) for i in range(2)]
            s_sb = K.sb("s_sb", [128, 8, 256], F32, e1)
            p_bf = K.sb("p_bf", [128, 8, 256], BF16, e1)
            s_flat = s_sb[:].rearrange("p h k -> p (h k)")
            class _V:
                def __init__(s_, ap): s_.ap = ap
                def __getitem__(s_, k): return s_.ap[k]
            rtv = [_V(s_flat[:, i * 320:(i + 1) * 320].rearrange("p (h d) -> p h d", d=32)) for i in range(4)]
            m1 = _V(s_flat[:, 0:D]); m2 = _V(s_flat[:, D:2 * D])
            cy = _V(s_flat[:, 0:D].rearrange("p (j t) -> p j t", t=128))
            mg = _V(p_bf[:].rearrange("p h k -> p (h k)")[:, 0:D])
            sm8 = K.sb("sm8", [128, 40], F32, e1)
            pT = K.sb("pT", [128, 16, 128], BF16, e1)
            attT = K.sb("attT", [64, 8, 128], BF16, e1)
            g = {n: K.sb("g_" + n, [4, 128], F32, e1) for n in
                 ("i", "e", "lf", "b", "a", "ew", "imb", "r", "mm", "nmm", "wi", "mo", "em", "one")}
            K.op("dve", lambda e: e.memset(g["one"][:], 1.0), [], [g["one"]])
            gs = K.sb("gs", [4, 16], F32, e1)
            mst = K.sb("mst", [4, 1], F32, e1)
            K.op("dve", lambda e: e.memset(mst[:], 0.0), [], [mst])
            dg = K.sb("dg", [4, 4], F32, e1)
            tms = K.sb("tms", [128, 20], F32, e1)
            Cst = K.sb("Cst", [128, 4, 129], F32, e1)
            K.op("dve", lambda e: e.memset(Cst[:], 0.0), [], [Cst])
            Cbf = K.sb("Cbf", [128, 4, 129], BF16, e1)
            wk = K.sb("wk", [128, 4, 128], BF16, e1)
            Eb = K.sb("Eb", [128, 128], F32, e1)
            WT = K.sb("WT", [128, 128], BF16, e1)
            p1 = K.sb("p1", [128, 129], F32, e1); nd = K.sb("nd", [128, 129], F32, e1)
            hid = K.sb("hid", [128, 512], F32, e1); hsq = junkF
            hs = K.sb("hs", [128, 12], F32, e1)
            mls = K.sb("mls", [128, 512], BF16, e1)
            mlsT = K.sb("mlsT", [128, 4, 128], BF16, e1)
            mT = pT
            hh = ut

            def chunk(ci, prefix, kv, full):
                x = xt[ci % 2]
                srcd = xp if prefix else xo
                K.dma("sp", lambda e: e.dma_start(out=x[:], in_=srcd[ci * 128:(ci + 1) * 128, :]), x, [srcd])
                rmsnorm_mod(x, ut, A1, B1, (junkF, ssq, rstd))
                def dump(name, buf, ap, w):
                    if full and DBG == name:
                        K.dma("pool", lambda e: e.dma_start(out=y[ci * 128:(ci + 1) * 128, 0:w], in_=ap), y, [buf])
                dump("u", ut, ut[:], D)
                if DBG == "AB":
                    DBGs = "AB"
                    K.dma("pool", lambda e: e.dma_start(out=y[ci * 128:(ci + 1) * 128, :], in_=(A1 if ci == 0 else B1)[:]), y, [A1, B1]) if full else None
                transpose_to(ut, uT, 8, P[0], P[1])
                par = (ci % 2) if full else 1
                if kv and not full:
                    par = 1
                def tm_group(pb, c0, c1):
                    for kc in range(8):
                        K.op("pe", lambda e: e.matmul(pb[:, 0:c1 - c0], lhsT=uT[:, kc, :], rhs=w_in[:, kc, c0:c1],
                                                      start=(kc == 0), stop=(kc == 7)), [uT, w_in], [pb])
                if kv:
                    tm_group(P[2], 0, 512); tm_group(P[3], 512, 768)
                    K.op("act", lambda e: e.activation(out=qk[:, 0:512], in_=P[2][:, :], func=AF.Copy), [P[2]], [qk])
                    K.op("act", lambda e: e.activation(out=qk[:, 512:640], in_=P[3][:, 0:128], func=AF.Copy), [P[3]], [qk])
                    vdst = vsb[par]
                    K.op("dve", lambda e: e.tensor_copy(out=vdst[:].rearrange("p a b -> p (a b)"), in_=P[3][:, 128:256]),
                         [P[3]], [vdst])
                tm_group(P[4], 1792, 2304)
                K.op("act", lambda e: e.activation(out=mv[:, :, 0:128], in_=P[4][:, :].rearrange("p (a b) -> p a b", b=128),
                                                   func=AF.Copy), [P[4]], [mv])
                if full:
                    tm_group(P[5], 2304, 2816)
                    K.op("act", lambda e: e.activation(out=so[:], in_=P[5][:, :], func=AF.Sigmoid), [P[5]], [so])
                    for j in range(4):
                        pb = P[2 + (j % 2)]
                        tm_group(pb, 2824 + j * 512, 2824 + (j + 1) * 512)
                        K.op("act", lambda e: e.activation(out=sg[:, j * 512:(j + 1) * 512], in_=pb[:, :], func=AF.Sigmoid),
                             [pb], [sg])
                vcol = valid[:, ci:ci + 1] if prefix else ones[:, 0:1]
                vb = valid if prefix else ones
                j0 = 0 if (full or kv) else 4
                for j in range(j0, 8):
                    pb = P[6 + (j // 4) % 2]
                    q = j % 4
                    for kc in range(8):
                        K.op("pe", lambda e: e.matmul(pb[:, q * 128:(q + 1) * 128], lhsT=w_in[:, kc, 768 + j * 128:768 + (j + 1) * 128],
                                                      rhs=uT[:, kc, :], start=(kc == 0), stop=(kc == 7)), [uT, w_in], [pb])
                    K.op("dve", lambda e: e.tensor_scalar(out=cin[:, j, 3:131], in0=pb[:, q * 128:(q + 1) * 128],
                                                          scalar1=vcol, scalar2=None, op0=ALU.mult), [pb, vb], [cin])
                pgi = P[0]; pgf = P[1]
                for kc in range(8):
                    K.op("pe", lambda e: e.matmul(pgi[0:4, 0:128], lhsT=w_in[:, kc, 2816:2820], rhs=uT[:, kc, :],
                                                  start=(kc == 0), stop=(kc == 7)), [uT, w_in], [pgi])
                for kc in range(8):
                    K.op("pe", lambda e: e.matmul(pgf[0:4, 0:128], lhsT=w_in[:, kc, 2820:2824], rhs=uT[:, kc, :],
                                                  start=(kc == 0), stop=(kc == 7)), [uT, w_in], [pgf])
                for j in range(j0, 8):
                    K.op("dve", lambda e: e.tensor_scalar(out=cy[:, j, :], in0=cin[:, j, 0:128], scalar1=cw[:, j, 0:1],
                                                          scalar2=None, op0=ALU.mult), [cin, cw], [s_sb])
                    for t in range(1, 4):
                        K.op("dve", lambda e: e.scalar_tensor_tensor(out=cy[:, j, :], in0=cin[:, j, t:t + 128],
                                                                     scalar=cw[:, j, t:t + 1], in1=cy[:, j, :],
                                                                     op0=ALU.mult, op1=ALU.add), [cin, cw, s_sb], [s_sb])
                K.op("pool", lambda e: e.tensor_copy(out=cin[:, j0:8, 0:3], in_=cin[:, j0:8, 128:131]), [cin], [cin])
                K.op("act", lambda e: e.activation(out=qkT[:, j0:8, :], in_=cy[:, j0:8, :], func=AF.Silu), [s_sb], [qkT])
                K.op("pool", lambda e: e.tensor_scalar(out=qkT[:, 4:8, :], in0=qkT[:, 4:8, :], scalar1=128.0 ** -0.5,
                                                       scalar2=None, op0=ALU.mult), [qkT], [qkT])
                gi, ge, lf, gb, ga_, ew, imb, gr, mm, nmm, wi, gmo, em, gone = (g[n] for n in
                    ("i", "e", "lf", "b", "a", "ew", "imb", "r", "mm", "nmm", "wi", "mo", "em", "one"))
                if prefix:
                    K.op("dve", lambda e: e.tensor_scalar(out=gi[:], in0=pgi[0:4, 0:128], scalar1=ibc[:, 0:1], scalar2=valid[0:4, ci:ci + 1],
                                                          op0=ALU.add, op1=ALU.mult), [pgi, ibc, valid], [gi])
                    K.op("dve", lambda e: e.tensor_scalar(out=gi[:], in0=gi[:], scalar1=pen[0:4, ci:ci + 1], scalar2=None,
                                                          op0=ALU.add), [gi, pen], [gi])
                else:
                    K.op("dve", lambda e: e.tensor_scalar(out=gi[:], in0=pgi[0:4, 0:128], scalar1=ibc[:, 0:1], scalar2=None,
                                                          op0=ALU.add), [pgi, ibc], [gi])
                K.op("act", lambda e: e.activation(out=ge[:], in_=pgf[0:4, 0:128], func=AF.Exp, bias=nfb[:, 0:1], scale=-1.0),
                     [pgf, nfb], [ge])
                K.op("act", lambda e: e.activation(out=ge[:], in_=ge[:], func=AF.Ln, bias=1.0), [ge], [ge])
                v4 = valid[0:4, ci:ci + 1] if prefix else ones[0:4, 0:1]
                K.op("dve", lambda e: e.tensor_scalar(out=lf[:], in0=ge[:], scalar1=v4, scalar2=-1.0, op0=ALU.mult, op1=ALU.mult),
                     [ge, vb], [lf])
                K.op("dve", lambda e: e.tensor_tensor_scan(out=gb[:], data0=gone[:], data1=lf[:], initial=0.0,
                                                           op0=ALU.mult, op1=ALU.add), [gone, lf], [gb])
                K.op("dve", lambda e: e.tensor_tensor(out=imb[:], in0=gi[:], in1=gb[:], op=ALU.subtract), [gi, gb], [imb])
                K.op("dve", lambda e: e.tensor_scalar(out=ga_[:], in0=imb[:], scalar1=gb[:, 127:128], scalar2=None, op0=ALU.add),
                     [imb, gb], [ga_])
                K.op("dve", lambda e: e.tensor_reduce(out=gs[:, 0:1], in_=ga_[:], axis=AX.X, op=ALU.max), [ga_], [gs])
                K.op("dve", lambda e: e.tensor_tensor(out=gs[:, 1:2], in0=gb[:, 127:128], in1=mst[:], op=ALU.add), [gb, mst], [gs])
                K.op("dve", lambda e: e.tensor_tensor(out=gs[:, 2:3], in0=gs[:, 1:2], in1=gs[:, 0:1], op=ALU.max), [gs], [gs])
                K.op("dve", lambda e: e.tensor_scalar(out=gs[:, 3:4], in0=gs[:, 2:3], scalar1=-1.0, scalar2=None, op0=ALU.mult), [gs], [gs])
                K.op("dve", lambda e: e.tensor_tensor(out=gs[:, 4:5], in0=gs[:, 1:2], in1=gs[:, 2:3], op=ALU.subtract), [gs], [gs])
                K.op("act", lambda e: e.activation(out=gs[:, 5:6], in_=gs[:, 4:5], func=AF.Exp), [gs], [gs])
                K.op("act", lambda e: e.activation(out=ew[:], in_=ga_[:], func=AF.Exp, bias=gs[:, 3:4]), [ga_, gs], [ew])
                K.op("dve", lambda e: e.tensor_scalar(out=dg[:], in0=i4[:], scalar1=gs[:, 5:6], scalar2=None, op0=ALU.mult), [i4, gs], [dg])
                ptm = P[0]
                rows = [ew]
                if full:
                    K.op("dve", lambda e: e.tensor_tensor_scan(out=gr[:], data0=gone[:], data1=imb[:], initial=-1e30,
                                                               op0=ALU.mult, op1=ALU.max), [gone, imb], [gr])
                    K.op("dve", lambda e: e.tensor_scalar(out=mm[:], in0=gr[:], scalar1=mst[:, 0:1], scalar2=None, op0=ALU.max), [gr, mst], [mm])
                    K.op("dve", lambda e: e.tensor_scalar(out=nmm[:], in0=mm[:], scalar1=-1.0, scalar2=None, op0=ALU.mult), [mm], [nmm])
                    K.op("act", lambda e: e.activation(out=wi[:], in_=mm[:], func=AF.Exp, bias=mst[:, 0:1], scale=-1.0), [mm, mst], [wi])
                    K.op("dve", lambda e: e.tensor_tensor(out=gmo[:], in0=gb[:], in1=mm[:], op=ALU.add), [gb, mm], [gmo])
                    K.op("act", lambda e: e.activation(out=em[:], in_=gmo[:], func=AF.Exp, scale=-1.0), [gmo], [em])
                    rows = [ew, imb, wi, em]
                for r_i, rr in enumerate(rows):
                    K.op("pe", lambda e: e.transpose(out=ptm[:, r_i * 4:(r_i + 1) * 4], in_=rr[:, :], identity=ident[0:4, 0:4]),
                         [rr, ident], [ptm])
                K.op("pe", lambda e: e.matmul(ptm[:, 16:20], lhsT=ones[0:4, :], rhs=dg[:, :], start=True, stop=True), [ones, dg], [ptm])
                K.op("act", lambda e: e.activation(out=tms[:], in_=ptm[:, 0:20], func=AF.Copy), [ptm], [tms])

                if kv:
                    K.dma("sp", lambda e: e.dma_start(out=cs[:], in_=cos_d[(ci + 1 if full else 0) * 128:(ci + 2 if full else 1) * 128, :]), cs, [cos_d])
                    K.dma("sp", lambda e: e.dma_start(out=sn[:], in_=sin_d[(ci + 1 if full else 0) * 128:(ci + 2 if full else 1) * 128, :]), sn, [sin_d])
                    dump("qk", qk, qk[:], 640)
                    q4 = qk[:].rearrange("p (h two d) -> p h two d", two=2, d=32)
                    x1 = q4[:, :, 0, :]; x2 = q4[:, :, 1, :]
                    cb = cs[:].unsqueeze(1).to_broadcast([128, 10, 32]); sbb = sn[:].unsqueeze(1).to_broadcast([128, 10, 32])
                    qr4 = qkr[:].rearrange("p h (two d) -> p h two d", two=2)
                    K.op("dve", lambda e: e.tensor_tensor(out=rtv[0][:], in0=x1, in1=cb, op=ALU.mult), [qk, cs], [s_sb])
                    K.op("pool", lambda e: e.tensor_tensor(out=rtv[1][:], in0=x2, in1=sbb, op=ALU.mult), [qk, sn], [s_sb])
                    K.op("dve", lambda e: e.tensor_tensor(out=qr4[:, :, 0, :], in0=rtv[0][:], in1=rtv[1][:], op=ALU.subtract), [s_sb], [qkr])
                    K.op("pool", lambda e: e.tensor_tensor(out=rtv[2][:], in0=x2, in1=cb, op=ALU.mult), [qk, cs], [s_sb])
                    K.op("dve", lambda e: e.tensor_tensor(out=rtv[3][:], in0=x1, in1=sbb, op=ALU.mult), [qk, sn], [s_sb])
                    K.op("dve", lambda e: e.tensor_tensor(out=qr4[:, :, 1, :], in0=rtv[2][:], in1=rtv[3][:], op=ALU.add), [s_sb], [qkr])
                    pq = P[2]; pk = P[3]
                    pqv = pq[0:64, :].bitcast(BF16).rearrange("p (a b) -> p a b", b=128)
                    pkv = pk[0:64, :].bitcast(BF16).rearrange("p (a b) -> p a b", b=128)
                    h0 = 0 if full else 8
                    for h in range(h0, 10):
                        dst = pqv[:, h, :] if h < 8 else pkv[:, h - 8, :]
                        pbuf = pq if h < 8 else pk
                        K.op("pe", lambda e: e.transpose(out=dst, in_=qkr[:, h, :], identity=identb[:]), [qkr, identb], [pbuf])
                    if full:
                        K.op("act", lambda e: e.activation(out=qT[:], in_=pqv[:, 0:8, :], func=AF.Copy), [pq], [qT])
                    kdst = kT[par]
                    K.op("act", lambda e: e.activation(out=kdst[:], in_=pkv[:, 0:2, :], func=AF.Copy), [pk], [kdst])

                if full:
                    kprev, kcur = kT[1 - par], kT[par]
                    vprev, vcur = vsb[1 - par], vsb[par]
                    mk_ = masks[0] if ci == 0 else masks[1]
                    for h in range(8):
                        pb = P[2 + h // 2]
                        o = (h % 2) * 256
                        K.op("pe", lambda e: e.matmul(pb[:, o:o + 128], lhsT=qT[:, h, :], rhs=kprev[:, h // 4, :], start=True, stop=True),
                             [qT, kprev], [pb])
                        K.op("pe", lambda e: e.matmul(pb[:, o + 128:o + 256], lhsT=qT[:, h, :], rhs=kcur[:, h // 4, :], start=True, stop=True),
                             [qT, kcur], [pb])
                    for q in range(4):
                        pb = P[2 + q]
                        K.op("dve", lambda e: e.scalar_tensor_tensor(out=s_sb[:, 2 * q:2 * q + 2, :],
                                                                     in0=pb[:, :].rearrange("p (a b) -> p a b", b=256), scalar=0.125,
                                                                     in1=mk_[:].unsqueeze(1).to_broadcast([128, 2, 256]),
                                                                     op0=ALU.mult, op1=ALU.add), [pb, mk_], [s_sb])
                    K.op("dve", lambda e: e.tensor_reduce(out=sm8[:, 0:8], in_=s_sb[:], axis=AX.X, op=ALU.max), [s_sb], [sm8])
                    K.op("dve", lambda e: e.tensor_tensor(out=sm8[:, 0:8], in0=sm8[:, 0:8], in1=sinkb[:], op=ALU.max), [sm8, sinkb], [sm8])
                    K.op("dve", lambda e: e.tensor_tensor(out=s_sb[:], in0=s_sb[:], in1=sm8[:, 0:8].unsqueeze(2).to_broadcast([128, 8, 256]),
                                                          op=ALU.subtract), [s_sb, sm8], [s_sb])
                    K.op("act", lambda e: e.activation(out=s_sb[:], in_=s_sb[:], func=AF.Exp), [s_sb], [s_sb])
                    K.op("dve", lambda e: e.tensor_reduce(out=sm8[:, 16:24], in_=s_sb[:], axis=AX.X, op=ALU.add), [s_sb], [sm8])
                    K.op("dve", lambda e: e.tensor_tensor(out=sm8[:, 8:16], in0=sinkb[:], in1=sm8[:, 0:8], op=ALU.subtract), [sm8, sinkb], [sm8])
                    K.op("act", lambda e: e.activation(out=sm8[:, 24:32], in_=sm8[:, 8:16], func=AF.Exp), [sm8], [sm8])
                    K.op("dve", lambda e: e.tensor_tensor(out=sm8[:, 16:24], in0=sm8[:, 16:24], in1=sm8[:, 24:32], op=ALU.add), [sm8], [sm8])
                    K.op("dve", lambda e: e.reciprocal(out=sm8[:, 32:40], in_=sm8[:, 16:24]), [sm8], [sm8])
                    K.op("dve", lambda e: e.tensor_tensor(out=p_bf[:], in0=s_sb[:], in1=sm8[:, 32:40].unsqueeze(2).to_broadcast([128, 8, 256]),
                                                          op=ALU.mult), [s_sb, sm8], [p_bf])
                    for hh_ in range(2):
                        pb = P[2 + hh_]
                        pvw = pb[:, :].bitcast(BF16).rearrange("p (a b) -> p a b", b=128)
                        for h4 in range(4):
                            h = hh_ * 4 + h4
                            for half in range(2):
                                K.op("pe", lambda e: e.transpose(out=pvw[:, h4 * 2 + half, :], in_=p_bf[:, h, half * 128:(half + 1) * 128],
                                                                 identity=identb[:]), [p_bf, identb], [pb])
                        K.op("act", lambda e: e.activation(out=pT[:, hh_ * 8:(hh_ + 1) * 8, :], in_=pvw[:, 0:8, :], func=AF.Copy), [pb], [pT])
                    for hh_ in range(2):
                        pb = P[4 + hh_]
                        for h4 in range(4):
                            h = hh_ * 4 + h4
                            K.op("pe", lambda e: e.matmul(pb[0:64, h4 * 128:(h4 + 1) * 128], lhsT=vprev[:, h // 4, :], rhs=pT[:, 2 * h, :],
                                                          start=True, stop=False), [vprev, pT], [pb])
                            K.op("pe", lambda e: e.matmul(pb[0:64, h4 * 128:(h4 + 1) * 128], lhsT=vcur[:, h // 4, :], rhs=pT[:, 2 * h + 1, :],
                                                          start=False, stop=True), [vcur, pT], [pb])
                        K.op("act", lambda e: e.activation(out=attT[:, hh_ * 4:(hh_ + 1) * 4, :],
                                                           in_=pb[0:64, :].rearrange("p (a b) -> p a b", b=128), func=AF.Copy), [pb], [attT])

                    K.op("act", lambda e: e.activation(out=Cbf[:], in_=Cst[:], func=AF.Copy), [Cst], [Cbf])
                    for h in range(4):
                        pbc = P[6]; pst = P[7]
                        K.op("pe", lambda e: e.matmul(pbc[:, 0:128], lhsT=sel[:, h, :], rhs=nmm[:, :], start=True, stop=True), [sel, nmm], [pbc])
                        K.op("act", lambda e: e.activation(out=Eb[:], in_=pbc[:, 0:128], func=AF.Exp, bias=tms[:, 4 + h:5 + h]), [pbc, tms], [Eb])
                        K.op("pool", lambda e: e.tensor_tensor(out=Eb[:], in0=Eb[:], in1=tri[:], op=ALU.mult), [Eb, tri], [Eb])
                        K.op("pe", lambda e: e.matmul(pst[:, 0:128], lhsT=qkT[:, 4 + h, :], rhs=qkT[:, h, :], start=True, stop=True), [qkT], [pst])
                        K.op("dve", lambda e: e.tensor_tensor(out=WT[:], in0=pst[:, 0:128], in1=Eb[:], op=ALU.mult), [pst, Eb], [WT])
                        K.op("pe", lambda e: e.matmul(pbc[:, 128:257], lhsT=WT[:], rhs=mv[:, h, :], start=True, stop=True), [WT, mv], [pbc])
                        K.op("pe", lambda e: e.matmul(pst[:, 128:257], lhsT=qkT[:, h, :], rhs=Cbf[:, h, :], start=True, stop=True), [qkT, Cbf], [pst])
                        K.op("act", lambda e: e.activation(out=p1[:], in_=pbc[:, 128:257], func=AF.Copy), [pbc], [p1])
                        K.op("dve", lambda e: e.scalar_tensor_tensor(out=nd[:], in0=pst[:, 128:257], scalar=tms[:, 8 + h:9 + h], in1=p1[:],
                                                                     op0=ALU.mult, op1=ALU.add), [pst, tms, p1], [nd])
                        K.op("dve", lambda e: e.scalar_tensor_tensor(out=hs[:, 10:11], in0=nd[:, 128:129], scalar=-1.0, in1=nd[:, 128:129],
                                                                     op0=ALU.mult, op1=ALU.max), [nd], [hs])
                        K.op("dve", lambda e: e.tensor_scalar(out=hs[:, 8:9], in0=hs[:, 10:11], scalar1=tms[:, 12 + h:13 + h], scalar2=None,
                                                              op0=ALU.max), [hs, tms], [hs])
                        K.op("dve", lambda e: e.reciprocal(out=hs[:, 9:10], in_=hs[:, 8:9]), [hs], [hs])
                        K.op("dve", lambda e: e.scalar_tensor_tensor(out=hid[:, h * 128:(h + 1) * 128], in0=nd[:, 0:128], scalar=hs[:, 9:10],
                                                                     in1=so[:, h * 128:(h + 1) * 128], op0=ALU.mult, op1=ALU.mult), [nd, hs, so], [hid])
                    dump("hid", hid, hid[:], 512)
                    K.op("pool", lambda e: e.tensor_tensor(out=hsq[:], in0=hid[:], in1=hid[:], op=ALU.mult), [hid], [hsq])
                    K.op("dve", lambda e: e.tensor_reduce(out=hs[:, 0:4], in_=hsq[:].rearrange("p (a b) -> p a b", b=128), axis=AX.X, op=ALU.add),
                         [hsq], [hs])
                    K.op("dve", lambda e: e.tensor_scalar(out=hs[:, 0:4], in0=hs[:, 0:4], scalar1=1.0 / 128, scalar2=EPS, op0=ALU.mult, op1=ALU.add), [hs], [hs])
                    K.op("act", lambda e: e.activation(out=hs[:, 0:4], in_=hs[:, 0:4], func=AF.Sqrt), [hs], [hs])
                    K.op("dve", lambda e: e.reciprocal(out=hs[:, 4:8], in_=hs[:, 0:4]), [hs], [hs])
                    for h in range(4):
                        K.op("dve", lambda e: e.scalar_tensor_tensor(out=mls[:, h * 128:(h + 1) * 128], in0=hid[:, h * 128:(h + 1) * 128],
                                                                     scalar=hs[:, 4 + h:5 + h], in1=mnwb[:, h * 128:(h + 1) * 128],
                                                                     op0=ALU.mult, op1=ALU.mult), [hid, hs, mnwb], [mls])
                    transpose_to(mls, mlsT, 4, P[6], P[7], dt32=False)

                pkt = P[6]
                pktv = pkt[:, :].bitcast(BF16).rearrange("p (a b) -> p a b", b=128)
                for h in range(4):
                    K.op("pe", lambda e: e.transpose(out=pktv[:, h, :], in_=qkT[:, 4 + h, :], identity=identb[:]), [qkT, identb], [pkt])
                for h in range(4):
                    K.op("dve", lambda e: e.tensor_scalar(out=wk[:, h, :], in0=pktv[:, h, :], scalar1=tms[:, h:h + 1], scalar2=None, op0=ALU.mult),
                         [pkt, tms], [wk])
                pu_ = P[7]
                for h in range(4):
                    K.op("pe", lambda e: e.matmul(pu_[:, h * 129:(h + 1) * 129] if h < 3 else P[6][:, 0:129], lhsT=wk[:, h, :], rhs=mv[:, h, :],
                                                  start=True, stop=True), [wk, mv], [pu_ if h < 3 else P[6]])
                for h in range(4):
                    src = pu_[:, h * 129:(h + 1) * 129] if h < 3 else P[6][:, 0:129]
                    sb_ = pu_ if h < 3 else P[6]
                    K.op("dve", lambda e: e.scalar_tensor_tensor(out=Cst[:, h, :], in0=Cst[:, h, :], scalar=tms[:, 16 + h:17 + h], in1=src,
                                                                 op0=ALU.mult, op1=ALU.add), [Cst, tms, sb_], [Cst])
                K.op("dve", lambda e: e.tensor_copy(out=mst[:], in_=gs[:, 2:3]), [gs], [mst])

                if full:
                    for nh in range(2):
                        pa_ = P[2 + nh]; pb_ = P[4 + nh]
                        for h in range(8):
                            K.op("pe", lambda e: e.matmul(pa_[:, :], lhsT=attT[:, h, :], rhs=wa[:, h, nh * 512:(nh + 1) * 512],
                                                          start=(h == 0), stop=(h == 7)), [attT, wa], [pa_])
                        for kc in range(4):
                            K.op("pe", lambda e: e.matmul(pb_[:, :], lhsT=mlsT[:, kc, :], rhs=wb[:, kc, nh * 512:(nh + 1) * 512],
                                                          start=(kc == 0), stop=(kc == 3)), [mlsT, wb], [pb_])
                        K.op("dve", lambda e: e.tensor_tensor(out=m1[:, nh * 512:(nh + 1) * 512], in0=pa_[:, :], in1=sg[:, nh * 512:(nh + 1) * 512],
                                                              op=ALU.mult), [pa_, sg], [s_sb])
                        K.op("dve", lambda e: e.tensor_tensor(out=m2[:, nh * 512:(nh + 1) * 512], in0=pb_[:, :], in1=sg[:, D + nh * 512:D + (nh + 1) * 512],
                                                              op=ALU.mult), [pb_, sg], [s_sb])
                    K.op("pool", lambda e: e.tensor_tensor(out=mg[:], in0=(m2 if ONLY == "b" else m1)[:], in1=(m1 if ONLY == "a" else m2)[:], op=ALU.add), [s_sb], [p_bf])
                    transpose_to(mg, mT, 8, P[2], P[3], dt32=False, srcbuf=p_bf)
                    for nh in range(2):
                        po = P[4 + nh]
                        for kc in range(8):
                            K.op("pe", lambda e: e.matmul(po[:, :], lhsT=mT[:, kc, :], rhs=wo[:, kc, nh * 512:(nh + 1) * 512],
                                                          start=(kc == 0), stop=(kc == 7)), [mT, wo], [po])
                        K.op("dve", lambda e: e.tensor_tensor(out=m1[:, nh * 512:(nh + 1) * 512], in0=po[:, :], in1=G1[:, nh * 512:(nh + 1) * 512],
                                                              op=ALU.mult), [po, G1], [s_sb])
                    K.op("pool", lambda e: e.tensor_tensor(out=hh[:], in0=m1[:], in1=x[:], op=ALU.add), [s_sb, x], [hh])
                    K.dma("pool", lambda e: e.dma_start(out=hbuf_d[ci * 128:(ci + 1) * 128, :], in_=hh[:]), hbuf_d, [hh])

            class _V2:
                def __init__(s_, ap): s_.ap = ap
                def __getitem__(s_, k): return s_.ap[k]
            sgf = sg[:].bitcast(F32)
            pbf32 = p_bf[:].rearrange("p h k -> p (h k)").bitcast(F32)
            hidb = hid[:].bitcast(BF16)
            mlsf = mls[:].bitcast(F32)
            PX = [(xt[0], _V2(xt[0][:])), (s_sb, _V2(s_flat[:, D:2 * D]))]
            PU = [(ut, _V2(ut[:])), (p_bf, _V2(pbf32))]
            PUT = [(uT, _V2(uT[:])), (pT, _V2(pT[:, 0:8, :]))]
            PCIN = [(cin, _V2(cin[:, 4:8, :])), (sg, _V2(sgf[:, 0:524].rearrange("p (j t) -> p j t", t=131)))]
            PCY = [(s_sb, _V2(cy[:, 4:8, :])), (s_sb, _V2(cy[:, 0:4, :]))]
            PKT = [(qkT, _V2(qkT[:, 4:8, :])), (qkT, _V2(qkT[:, 0:4, :]))]
            PMV = [(mv, _V2(mv[:])), (hid, _V2(hidb[:, 0:516].rearrange("p (h e) -> p h e", e=129)))]
            PWK = [(wk, _V2(wk[:])), (qkr, _V2(qkr[:].rearrange("p h d -> p (h d)")[:, 0:512].rearrange("p (h d) -> p h d", d=128)))]
            g1n = ("i", "e", "lf", "b", "a", "ew", "imb")
            PG = [{n: (g[n], _V2(g[n][:])) for n in g1n},
                  {n: ((so, _V2(so[0:4, k * 128:(k + 1) * 128])) if k < 4 else (mls, _V2(mlsf[0:4, (k - 4) * 128:(k - 3) * 128])))
                   for k, n in enumerate(("i", "e", "lf", "b", "a", "ew"))}]
            PG[1]["imb"] = (qk, _V2(qk[0:4, 128:256]))
            PGS = [(gs, _V2(gs[:])), (qk, _V2(qk[0:4, 0:16]))]
            PDG = [(dg, _V2(dg[:])), (qk, _V2(qk[0:4, 16:20]))]
            PTMS = [(tms, _V2(tms[:])), (qk, _V2(qk[:, 32:52]))]
            PSQ = [((ssq, _V2(ssq[:])), (rstd, _V2(rstd[:]))), ((qk, _V2(qk[:, 60:64])), (qk, _V2(qk[:, 64:65])))]
            tEb = K.view(pT, "tEb")
            if NPRE > 1:
                K.op("dve", lambda e: e.memset(PMV[1][1][:], 1.0), [], [hid])
                K.op("dve", lambda e: e.memset(PCIN[1][1][:], 0.0), [], [sg])

            def pfA(ci, p):
                xb, xv = PX[p]; ub, uv = PU[p]; utb, utv = PUT[p]; cb_, cv = PCIN[p]; mb, mvv = PMV[p]
                (sqb, sqv), (rsb, rsv) = PSQ[p]
                Q0, Q1, Q2, Q3 = P[4 * p:4 * p + 4]
                K.dma("sp", lambda e: e.dma_start(out=xv[:], in_=xp[ci * 128:(ci + 1) * 128, :]), xb, [xp])
                K.op("act", lambda e: e.activation(out=junkF[:].bitcast(BF16), in_=xv[:], func=AF.Square, accum_out=sqv[:, 0:1]), [xb], [junkF, sqb]); yield
                K.op("dve", lambda e: e.tensor_scalar(out=sqv[:, 1:2], in0=sqv[:, 0:1], scalar1=1.0 / D, scalar2=EPS, op0=ALU.mult, op1=ALU.add), [sqb], [sqb]); yield
                K.op("act", lambda e: e.activation(out=sqv[:, 2:3], in_=sqv[:, 1:2], func=AF.Ln), [sqb], [sqb]); yield
                K.op("act", lambda e: e.activation(out=rsv[:], in_=sqv[:, 2:3], func=AF.Exp, scale=-0.5), [sqb], [rsb]); yield
                K.op("dve", lambda e: e.scalar_tensor_tensor(out=uv[:], in0=xv[:], scalar=rsv[:, 0:1], in1=A1[:], op0=ALU.mult, op1=ALU.mult), [xb, rsb, A1], [ub]); yield
                K.op("pool", lambda e: e.tensor_tensor(out=uv[:], in0=uv[:], in1=B1[:], op=ALU.add), [ub, B1], [ub]); yield
                for half in range(2):
                    pb_ = (Q0, Q1)[half]
                    pv = pb_[:, :].rearrange("p (a b) -> p a b", b=128)
                    for q in range(4):
                        kc = half * 4 + q
                        K.op("pe", lambda e: e.transpose(out=pv[:, q, :], in_=uv[:, kc * 128:(kc + 1) * 128], identity=ident[:]), [ub, ident], [pb_])
                    K.op("act", lambda e: e.activation(out=utv[:, half * 4:half * 4 + 4, :], in_=pv[:, 0:4, :], func=AF.Copy), [pb_], [utb]); yield
                for kc in range(8):
                    K.op("pe", lambda e: e.matmul(Q2[:, 0:512], lhsT=utv[:, kc, :], rhs=w_in[:, kc, 1792:2304], start=(kc == 0), stop=(kc == 7)), [utb, w_in], [Q2])
                K.op("act", lambda e: e.activation(out=mvv[:, :, 0:128], in_=Q2[:, :].rearrange("p (a b) -> p a b", b=128), func=AF.Copy), [Q2], [mb]); yield
                for j in range(4):
                    for kc in range(8):
                        K.op("pe", lambda e: e.matmul(Q3[:, j * 128:(j + 1) * 128], lhsT=w_in[:, kc, 1280 + j * 128:1280 + (j + 1) * 128], rhs=utv[:, kc, :],
                                                      start=(kc == 0), stop=(kc == 7)), [utb, w_in], [Q3])
                K.op("dve", lambda e: e.tensor_scalar(out=cv[:, :, 3:131], in0=Q3[:, :].rearrange("p (a b) -> p a b", b=128), scalar1=valid[:, ci:ci + 1],
                                                      scalar2=None, op0=ALU.mult), [Q3, valid], [cb_]); yield
                ob, ov = PCIN[1 - p]
                K.op("pool", lambda e: e.tensor_copy(out=ov[:, :, 0:3], in_=cv[:, :, 128:131]), [cb_], [ob]); yield
                for kc in range(8):
                    K.op("pe", lambda e: e.matmul(Q0[0:4, 0:128], lhsT=w_in[:, kc, 2816:2820], rhs=utv[:, kc, :], start=(kc == 0), stop=(kc == 7)), [utb, w_in], [Q0])
                for kc in range(8):
                    K.op("pe", lambda e: e.matmul(Q0[0:4, 128:256], lhsT=w_in[:, kc, 2820:2824], rhs=utv[:, kc, :], start=(kc == 0), stop=(kc == 7)), [utb, w_in], [Q0])
                yield

            def pfB(ci, p):
                cb_, cv = PCIN[p]; cyb, cyv = PCY[p]; kb, kv_ = PKT[p]; mb, mvv = PMV[p]; wkb, wkv = PWK[p]
                G = PG[p]; gsb, gsv = PGS[p]; dgb, dgv = PDG[p]; tmb, tmv = PTMS[p]
                Q0, Q1, Q2, Q3 = P[4 * p:4 * p + 4]
                for j in range(4):
                    K.op("dve", lambda e: e.tensor_scalar(out=cyv[:, j, :], in0=cv[:, j, 0:128], scalar1=cw[:, 4 + j, 0:1], scalar2=None, op0=ALU.mult), [cb_, cw], [cyb]); yield
                    for t in range(1, 4):
                        K.op("dve", lambda e: e.scalar_tensor_tensor(out=cyv[:, j, :], in0=cv[:, j, t:t + 128], scalar=cw[:, 4 + j, t:t + 1], in1=cyv[:, j, :],
                                                                     op0=ALU.mult, op1=ALU.add), [cb_, cw, cyb], [cyb]); yield
                tE = pT[:, 8:16, :].rearrange("p a b -> p (a b)").bitcast(F32).rearrange("p (j t) -> p j t", t=128)
                K.op("act", lambda e: e.activation(out=tE, in_=cyv[:], func=AF.Exp, scale=-1.0), [cyb], [tEb]); yield
                K.op("dve", lambda e: e.tensor_scalar(out=tE, in0=tE, scalar1=1.0, scalar2=None, op0=ALU.add), [tEb], [tEb]); yield
                K.op("dve", lambda e: e.reciprocal(out=tE, in_=tE), [tEb], [tEb]); yield
                K.op("dve", lambda e: e.scalar_tensor_tensor(out=kv_[:], in0=cyv[:], scalar=128.0 ** -0.5, in1=tE, op0=ALU.mult, op1=ALU.mult), [cyb, tEb], [kb]); yield
                (gib, giv), (geb, gev), (lfb, lfv), (gbb, gbv), (gab, gav), (ewb, ewv), (imbb, imbv) = (G[n] for n in g1n)
                K.op("dve", lambda e: e.tensor_scalar(out=giv[:], in0=Q0[0:4, 0:128], scalar1=ibc[:, 0:1], scalar2=valid[0:4, ci:ci + 1], op0=ALU.add, op1=ALU.mult), [Q0, ibc, valid], [gib]); yield
                K.op("dve", lambda e: e.tensor_scalar(out=giv[:], in0=giv[:], scalar1=pen[0:4, ci:ci + 1], scalar2=None, op0=ALU.add), [gib, pen], [gib]); yield
                K.op("act", lambda e: e.activation(out=gev[:], in_=Q0[0:4, 128:256], func=AF.Exp, bias=nfb[:, 0:1], scale=-1.0), [Q0, nfb], [geb]); yield
                K.op("act", lambda e: e.activation(out=gev[:], in_=gev[:], func=AF.Ln, bias=1.0), [geb], [geb]); yield
                K.op("dve", lambda e: e.tensor_scalar(out=lfv[:], in0=gev[:], scalar1=valid[0:4, ci:ci + 1], scalar2=-1.0, op0=ALU.mult, op1=ALU.mult), [geb, valid], [lfb]); yield
                K.op("dve", lambda e: e.tensor_tensor_scan(out=gbv[:], data0=g["one"][:], data1=lfv[:], initial=0.0, op0=ALU.mult, op1=ALU.add), [g["one"], lfb], [gbb]); yield
                K.op("dve", lambda e: e.tensor_tensor(out=imbv[:], in0=giv[:], in1=gbv[:], op=ALU.subtract), [gib, gbb], [imbb]); yield
                K.op("dve", lambda e: e.tensor_scalar(out=gav[:], in0=imbv[:], scalar1=gbv[:, 127:128], scalar2=None, op0=ALU.add), [imbb, gbb], [gab]); yield
                K.op("dve", lambda e: e.tensor_reduce(out=gsv[:, 0:1], in_=gav[:], axis=AX.X, op=ALU.max), [gab], [gsb]); yield
                K.op("dve", lambda e: e.tensor_tensor(out=gsv[:, 1:2], in0=gbv[:, 127:128], in1=mst[:], op=ALU.add), [gbb, mst], [gsb]); yield
                K.op("dve", lambda e: e.tensor_tensor(out=gsv[:, 2:3], in0=gsv[:, 1:2], in1=gsv[:, 0:1], op=ALU.max), [gsb], [gsb]); yield
                K.op("dve", lambda e: e.tensor_copy(out=mst[:], in_=gsv[:, 2:3]), [gsb], [mst]); yield
                K.op("dve", lambda e: e.tensor_scalar(out=gsv[:, 3:4], in0=gsv[:, 2:3], scalar1=-1.0, scalar2=None, op0=ALU.mult), [gsb], [gsb]); yield
                K.op("dve", lambda e: e.tensor_tensor(out=gsv[:, 4:5], in0=gsv[:, 1:2], in1=gsv[:, 2:3], op=ALU.subtract), [gsb], [gsb]); yield
                K.op("act", lambda e: e.activation(out=gsv[:, 5:6], in_=gsv[:, 4:5], func=AF.Exp), [gsb], [gsb]); yield
                K.op("act", lambda e: e.activation(out=ewv[:], in_=gav[:], func=AF.Exp, bias=gsv[:, 3:4]), [gab, gsb], [ewb]); yield
                K.op("dve", lambda e: e.tensor_scalar(out=dgv[:], in0=i4[:], scalar1=gsv[:, 5:6], scalar2=None, op0=ALU.mult), [i4, gsb], [dgb]); yield
                K.op("pe", lambda e: e.transpose(out=Q0[:, 256:260], in_=ewv[:, :], identity=ident[0:4, 0:4]), [ewb, ident], [Q0])
                K.op("pe", lambda e: e.matmul(Q0[:, 272:276], lhsT=ones[0:4, :], rhs=dgv[:, :], start=True, stop=True), [ones, dgb], [Q0])
                K.op("act", lambda e: e.activation(out=tmv[:, 0:20], in_=Q0[:, 256:276], func=AF.Copy), [Q0], [tmb]); yield
                pktv = Q1[:, :].bitcast(BF16).rearrange("p (a b) -> p a b", b=128)
                for h in range(4):
                    K.op("pe", lambda e: e.transpose(out=pktv[:, h, :], in_=kv_[:, h, :], identity=identb[:]), [kb, identb], [Q1])
                for h in range(4):
                    K.op("dve", lambda e: e.tensor_scalar(out=wkv[:, h, :], in0=pktv[:, h, :], scalar1=tmv[:, h:h + 1], scalar2=None, op0=ALU.mult), [Q1, tmb], [wkb]); yield
                for h in range(4):
                    dst = Q2[:, h * 129:(h + 1) * 129] if h < 3 else Q3[:, 0:129]
                    K.op("pe", lambda e: e.matmul(dst, lhsT=wkv[:, h, :], rhs=mvv[:, h, :], start=True, stop=True), [wkb, mb], [Q2 if h < 3 else Q3])
                for h in range(4):
                    src = Q2[:, h * 129:(h + 1) * 129] if h < 3 else Q3[:, 0:129]
                    K.op("dve", lambda e: e.scalar_tensor_tensor(out=Cst[:, h, :], in0=Cst[:, h, :], scalar=tmv[:, 16 + h:17 + h], in1=src,
                                                                 op0=ALU.mult, op1=ALU.add), [Cst, tmb, Q2 if h < 3 else Q3], [Cst]); yield

            def step(gn):
                if gn is None:
                    return None
                try:
                    next(gn); return gn
                except StopIteration:
                    return None

            NPP = max(NPRE - 1, 0)
            if NPP > 0:
                ga_gen = pfA(0, 0)
                while ga_gen is not None:
                    ga_gen = step(ga_gen)
                for ci in range(NPP):
                    gb_gen = pfB(ci, ci % 2)
                    ga_gen = pfA(ci + 1, (ci + 1) % 2) if ci + 1 < NPP else None
                    while gb_gen is not None or ga_gen is not None:
                        gb_gen = step(gb_gen)
                        ga_gen = step(ga_gen)
                lb, lv = PCIN[(NPP - 1) % 2]
                if (NPP - 1) % 2 == 1:
                    K.op("pool", lambda e: e.tensor_copy(out=cin[:, 4:8, 0:3], in_=lv[:, :, 128:131]), [lb], [cin])
                K.barrier()
                K.op("dve", lambda e: e.memset(mv[:], 1.0), [], [mv])
            if NPRE > 0:
                chunk(NPRE - 1, True, True, False)
            if NPRE == 0:
                K.op("dve", lambda e: e.memset(kT[1][:], 0.0), [], [kT[1]])
                K.op("dve", lambda e: e.memset(vsb[1][:], 0.0), [], [vsb[1]])
            for c in range(NOWN):
                chunk(c, False, True, True)
        K.barrier()
        if stage == 1:
            with ExitStack() as ed:
                tt = K.sb("dbgt", [128, D], F32, ed)
                for c in range(NOWN if DBG is None else 0):
                    K.dma("sp", lambda e: e.dma_start(out=tt[:], in_=hbuf_d[c * 128:(c + 1) * 128, :]), tt, [hbuf_d])
                    K.dma("sp", lambda e: e.dma_start(out=y[c * 128:(c + 1) * 128, :], in_=tt[:]), y, [tt])
                K.finish([y])
            return nc

        with ExitStack() as e2:
            stage = [K.sb("stg2_%d" % i, [128, 2048], F32, e2) for i in range(2)]
            wq = K.sb("wq", [128, 8, 2048], BF16, e2)
            load_bf16(wq, 8, 2048, wq_d.t.rearrange("(kc p) n -> p kc n", p=128), wq_d, stage)
            skT = K.sb("skT", [128, 2, 128], F32, e2)
            K.dma("sp", lambda e: e.dma_start(out=skT[:], in_=sk_d[:]), skT, [sk_d])
            A2 = K.sb("A2", [128, D], F32, e2); B2 = K.sb("B2", [128, D], F32, e2); G2 = K.sb("G2", [128, D], F32, e2)
            K.dma("sp", lambda e: e.dma_start(out=A2[:], in_=modbc_d[:, 4 * D:5 * D]), A2, [modbc_d])
            K.dma("sp", lambda e: e.dma_start(out=B2[:], in_=modbc_d[:, 3 * D:4 * D]), B2, [modbc_d])
            K.dma("sp", lambda e: e.dma_start(out=G2[:], in_=modbc_d[:, 5 * D:6 * D]), G2, [modbc_d])
            NF = K.sb("NF", [128, D], F32, e2); bcast_load(NF, nfw_d[:], nfw_d)
            iota16 = K.sb("iota16", [128, 16], F32, e2)
            K.dma("sp", lambda e: e.dma_start(out=iota16[:], in_=iota_d[:]), iota16, [iota_d])
            ht = [K.sb("ht%d" % i, [128, D], F32, e2) for i in range(2)]
            u2 = K.sb("u2", [128, D], F32, e2)
            junk = K.sb("junk2", [128, 512], F32, e2); junkf = K.sb("junkf", [128, D], F32, e2)
            ssq = K.sb("ssq2", [128, 4], F32, e2); rstd = K.sb("rstd2", [128, 1], F32, e2)
            u2T = K.sb("u2T", [128, 8, 128], BF16, e2)
            qyT = K.sb("qyT", [128, 16, 128], F32, e2)
            sc = K.sb("sc", [128, 16, 128], F32, e2); sc2 = K.sb("sc2", [128, 16, 128], F32, e2)
            tv = K.sb("tv", [128, 16, 16], F32, e2); ti = K.sb("ti", [128, 16, 16], U32, e2)
            tif = K.sb("tif", [128, 16, 16], F32, e2)
            cand = K.sb("cand", [128, 8, 256], F32, e2); cand2 = K.sb("cand2", [128, 8, 256], F32, e2)
            bs = K.sb("bs", [128, 8, 16], F32, e2); bp = K.sb("bp", [128, 8, 16], U32, e2)
            au = K.sb("au", [128, 8, 16], U32, e2); bu = K.sb("bu", [128, 8, 16], U32, e2)
            af = K.sb("af", [128, 8, 16], F32, e2); bfl = K.sb("bfl", [128, 8, 16], F32, e2)
            eq = K.sb("eq", [128, 8, 16, 16], F32, e2)
            isl = K.sb("isl", [128, 8, 16], F32, e2); jsl = K.sb("jsl", [128, 8, 16], F32, e2)
            idxf = K.sb("idxf", [128, 128], F32, e2); idx = K.sb("idx", [128, 128], I32, e2)
            gsm = K.sb("gsm", [128, 24], F32, e2)
            gate = K.sb("gate", [128, 8, 16], F32, e2)
            dots = K.sb("dots", [128, 128], F32, e2); wsl = K.sb("wsl", [128, 128], F32, e2)
            acc = K.sb("acc", [128, D], F32, e2)
            R = 12
            ring = [K.sb("ring%d" % i, [128, D], F32, e2) for i in range(R)]
            yt = K.sb("yt", [128, D], F32, e2)
            tmpr = [K.sb("tmpr%d" % i, [128, D], BF16, e2) for i in range(3)]
            class _RB:
                def __init__(s_, b): s_.b = b
                def __getitem__(s_, k): return s_.b[:].bitcast(BF16)[:, 0:D][k]
            rcount = [0]
            blk = 0
            for (src_t, dst_t) in ((pu_d, pub_d), (pv_d, pvb_d)):
                sv_ = src_t.t.rearrange("(p q) d -> p q d", q=128)
                dv_ = dst_t.t.rearrange("(p q) d -> p q d", q=128)
                for q in range(128):
                    rbf = ring[blk % R]; tbf = tmpr[blk % 3]
                    K.dma("sp", lambda e: e.dma_start(out=rbf[:], in_=sv_[:, q, :]), rbf, [src_t])
                    engs = ("dve", "act")
                    en = engs[blk % 2]
                    if en == "act":
                        K.op("act", lambda e: e.activation(out=tbf[:], in_=rbf[:], func=AF.Copy), [rbf], [tbf])
                    else:
                        K.op(en, lambda e: e.tensor_copy(out=tbf[:], in_=rbf[:]), [rbf], [tbf])
                    K.dma("pool", lambda e: e.dma_start(out=dv_[:, q, :], in_=tbf[:]), dst_t, [tbf], waw=False)
                    blk += 1

            def gather(src_d, col, idx):
                rb = ring[rcount[0] % R]; rcount[0] += 1
                K.dma("pool", lambda e: e.indirect_dma_start(out=rb[:].bitcast(BF16)[:, 0:D], out_offset=None, in_=src_d[:, :],
                                                             in_offset=bass.IndirectOffsetOnAxis(ap=idx[:, col:col + 1], axis=0)),
                      rb, [src_d, idx])
                return rb

            idxs = [idx, K.sb("idx_b", [128, 128], I32, e2)]

            def front(c):
                h_ = ht[c % 2]
                idx = idxs[c % 2]
                K.dma("sp", lambda e: e.dma_start(out=h_[:], in_=hbuf_d[c * 128:(c + 1) * 128, :]), h_, [hbuf_d])
                rmsnorm_mod(h_, u2, A2, B2, (junk, ssq, rstd))
                transpose_to(u2, u2T, 8, P[0], P[1])
                for hp in range(16):
                    pb = P[2 + hp // 4]
                    q = hp % 4
                    for kc in range(8):
                        K.op("pe", lambda e: e.matmul(pb[:, q * 128:(q + 1) * 128], lhsT=wq[:, kc, hp * 128:(hp + 1) * 128], rhs=u2T[:, kc, :],
                                                      start=(kc == 0), stop=(kc == 7)), [wq, u2T], [pb])
                    if q == 3:
                        K.op("act", lambda e: e.activation(out=qyT[:, hp - 3:hp + 1, :], in_=pb[:, :].rearrange("p (a b) -> p a b", b=128),
                                                           func=AF.Copy), [pb], [qyT])
                        yield
                for hp in range(16):
                    pb = P[2 + hp // 4]
                    q = hp % 4
                    K.op("pe", lambda e: e.matmul(pb[:, q * 128:(q + 1) * 128], lhsT=qyT[:, hp, :], rhs=skT[:, hp % 2, :], start=True, stop=True),
                         [qyT, skT], [pb])
                    if q == 3:
                        K.op("act", lambda e: e.activation(out=sc[:, hp - 3:hp + 1, :], in_=pb[:, :].rearrange("p (a b) -> p a b", b=128),
                                                           func=AF.Copy), [pb], [sc])
                        yield
                for hp in range(16):
                    K.op("dve", lambda e: e.max(out=tv[:, hp, 0:8], in_=sc[:, hp, :]), [sc], [tv])
                    yield
                    K.op("dve", lambda e: e.max_index(out=ti[:, hp, 0:8], in_max=tv[:, hp, 0:8], in_values=sc[:, hp, :]), [sc, tv], [ti])
                    yield
                    K.op("dve", lambda e: e.match_replace(out=sc2[:, hp, :], in_to_replace=tv[:, hp, 0:8], in_values=sc[:, hp, :], imm_value=-1e30),
                         [sc, tv], [sc2])
                    yield
                    K.op("dve", lambda e: e.max(out=tv[:, hp, 8:16], in_=sc2[:, hp, :]), [sc2], [tv])
                    yield
                    K.op("dve", lambda e: e.max_index(out=ti[:, hp, 8:16], in_max=tv[:, hp, 8:16], in_values=sc2[:, hp, :]), [sc2, tv], [ti])
                    yield
                tv4 = tv[:].rearrange("p (h s) k -> p h s k", s=2)
                K.op("dve", lambda e: e.tensor_tensor(out=cand[:].rearrange("p h (a b) -> p h a b", b=16),
                                                      in0=tv4[:, :, 0, :].unsqueeze(3).to_broadcast([128, 8, 16, 16]),
                                                      in1=tv4[:, :, 1, :].unsqueeze(2).to_broadcast([128, 8, 16, 16]), op=ALU.add), [tv], [cand])
                yield
                for h in range(8):
                    K.op("dve", lambda e: e.max(out=bs[:, h, 0:8], in_=cand[:, h, :]), [cand], [bs])
                    yield
                    K.op("dve", lambda e: e.max_index(out=bp[:, h, 0:8], in_max=bs[:, h, 0:8], in_values=cand[:, h, :]), [cand, bs], [bp])
                    yield
                    K.op("dve", lambda e: e.match_replace(out=cand2[:, h, :], in_to_replace=bs[:, h, 0:8], in_values=cand[:, h, :], imm_value=-1e30),
                         [cand, bs], [cand2])
                    yield
                    K.op("dve", lambda e: e.max(out=bs[:, h, 8:16], in_=cand2[:, h, :]), [cand2], [bs])
                    yield
                    K.op("dve", lambda e: e.max_index(out=bp[:, h, 8:16], in_max=bs[:, h, 8:16], in_values=cand2[:, h, :]), [cand2, bs], [bp])
                    yield
                K.op("dve", lambda e: e.tensor_scalar(out=au[:], in0=bp[:], scalar1=4, scalar2=None, op0=ALU.logical_shift_right), [bp], [au])
                yield
                K.op("dve", lambda e: e.tensor_scalar(out=bu[:], in0=bp[:], scalar1=15, scalar2=None, op0=ALU.bitwise_and), [bp], [bu])
                yield
                K.op("dve", lambda e: e.tensor_copy(out=af[:], in_=au[:]), [au], [af])
                yield
                K.op("dve", lambda e: e.tensor_copy(out=bfl[:], in_=bu[:]), [bu], [bfl])
                yield
                K.op("dve", lambda e: e.tensor_copy(out=tif[:], in_=ti[:]), [ti], [tif])
                yield
                tif4 = tif[:].rearrange("p (h s) k -> p h s k", s=2)
                for (srcf, side, dst) in ((af, 0, isl), (bfl, 1, jsl)):
                    K.op("dve", lambda e: e.tensor_tensor(out=eq[:], in0=iota16[:].unsqueeze(1).unsqueeze(1).to_broadcast([128, 8, 16, 16]),
                                                          in1=srcf[:].unsqueeze(3).to_broadcast([128, 8, 16, 16]), op=ALU.is_equal), [iota16, srcf], [eq])
                    yield
                    K.op("dve", lambda e: e.tensor_tensor(out=eq[:], in0=eq[:], in1=tif4[:, :, side, :].unsqueeze(2).to_broadcast([128, 8, 16, 16]),
                                                          op=ALU.mult), [eq, tif], [eq])
                    yield
                    K.op("dve", lambda e: e.tensor_reduce(out=dst[:], in_=eq[:], axis=AX.X, op=ALU.add), [eq], [dst])
                    yield
                K.op("dve", lambda e: e.scalar_tensor_tensor(out=idxf[:], in0=isl[:].rearrange("p h k -> p (h k)"), scalar=128.0,
                                                             in1=jsl[:].rearrange("p h k -> p (h k)"), op0=ALU.mult, op1=ALU.add), [isl, jsl], [idxf])
                yield
                K.op("dve", lambda e: e.tensor_scalar(out=idxf[:], in0=idxf[:], scalar1=0.0, scalar2=16383.0, op0=ALU.max, op1=ALU.min), [idxf], [idxf])
                yield
                K.op("dve", lambda e: e.tensor_copy(out=idx[:], in_=idxf[:]), [idxf], [idx])
                yield
                K.op("dve", lambda e: e.tensor_tensor(out=gate[:], in0=bs[:], in1=bs[:, :, 0:1].to_broadcast([128, 8, 16]), op=ALU.subtract), [bs], [gate])
                yield
                K.op("act", lambda e: e.activation(out=gate[:], in_=gate[:], func=AF.Exp), [gate], [gate])
                yield
                K.op("dve", lambda e: e.tensor_reduce(out=gsm[:, 0:8], in_=gate[:], axis=AX.X, op=ALU.add), [gate], [gsm])
                yield
                K.op("dve", lambda e: e.reciprocal(out=gsm[:, 8:16], in_=gsm[:, 0:8]), [gsm], [gsm])
                yield
                K.op("dve", lambda e: e.tensor_tensor(out=gate[:], in0=gate[:], in1=gsm[:, 8:16].unsqueeze(2).to_broadcast([128, 8, 16]), op=ALU.mult),
                     [gate, gsm], [gate])
                yield
                yield

            def drain(g, n=None):
                k = 0
                while g is not None and (n is None or k < n):
                    try:
                        next(g)
                    except StopIteration:
                        return None
                    k += 1
                return g

            gen = drain(front(0))
            for c in range(NOWN):
                h_ = ht[c % 2]
                idx = idxs[c % 2]
                for s in range(128):
                    rb = gather(pub_d, s, idx); rbv = _RB(rb)
                    K.op("dve", lambda e: e.scalar_tensor_tensor(out=junkf[:], in0=u2[:], scalar=1.0, in1=rbv[:], op0=ALU.mult, op1=ALU.mult,
                                                                 accum_out=dots[:, s:s + 1]), [u2, rb], [junkf, dots])
                K.op("act", lambda e: e.activation(out=wsl[:], in_=dots[:], func=AF.Gelu), [dots], [wsl])
                K.op("dve", lambda e: e.tensor_tensor(out=wsl[:], in0=wsl[:], in1=gate[:].rearrange("p h k -> p (h k)"), op=ALU.mult), [wsl, gate], [wsl])
                gen = front(c + 1) if c + 1 < NOWN else None
                for s in range(128):
                    rb = gather(pvb_d, s, idx); rbv = _RB(rb)
                    tb = tmpr[s % 3]
                    K.op("act", lambda e: e.activation(out=tb[:], in_=rbv[:], func=AF.Copy, scale=wsl[:, s:s + 1]), [rb, wsl], [tb])
                    for nh in range(2):
                        K.op("pe", lambda e: e.matmul(P[6 + nh][:, :], lhsT=identb[:], rhs=tb[:, nh * 512:(nh + 1) * 512],
                                                      start=(s == 0), stop=(s == 127)), [identb, tb], [P[6 + nh]])
                    gen = drain(gen, 2)
                gen = drain(gen)
                for nh in range(2):
                    K.op("dve", lambda e: e.tensor_tensor(out=acc[:, nh * 512:(nh + 1) * 512], in0=P[6 + nh][:, :], in1=G2[:, nh * 512:(nh + 1) * 512],
                                                          op=ALU.mult), [P[6 + nh], G2], [acc])
                K.op("pool", lambda e: e.tensor_tensor(out=acc[:], in0=acc[:], in1=h_[:], op=ALU.add), [acc, h_], [acc])
                rmsnorm_mod(acc, yt, NF, None, (junk, ssq, rstd))
                K.dma("sp", lambda e: e.dma_start(out=y[c * 128:(c + 1) * 128, :], in_=yt[:]), y, [yt])
            K.finish([y])
    return nc


SEQ_FULL = 16384
ONLY = None
DBG = None
_cache = {}


def _consts():
    ident = np.eye(128, dtype=np.float32)
    tri = np.triu(np.ones((128, 128), np.float32))
    sel = np.zeros((4, 4, 128), np.float32)
    for h in range(4):
        sel[h, h, :] = 1.0
    i4 = np.eye(4, dtype=np.float32)
    iota16 = np.tile(np.arange(16, dtype=np.float32)[None, :], (128, 1))
    qi = np.arange(128)[:, None]; ki = np.arange(256)[None, :]
    rel = qi + 128 - ki
    ok = (rel >= 0) & (rel < 128)
    maskN = np.where(ok, 0.0, NEG).astype(np.float32)
    mask0 = np.where(ok & (ki >= 128), 0.0, NEG).astype(np.float32)
    return dict(ident=ident, tri=tri, sel=sel, i4=i4, iota16=iota16, maskN=maskN, mask0=mask0)


def run(inputs, S, ncore_per_seq, NOWN, stage=2):
    x = np.asarray(inputs["x"], np.float32)
    B = x.shape[0]
    NPRE = (ncore_per_seq - 1) * NOWN
    key = (NPRE, NOWN, stage)
    if key not in _cache:
        _cache[key] = build(NPRE, NOWN, stage)
    nc = _cache[key]
    f = lambda k: np.ascontiguousarray(np.asarray(inputs[k], np.float32))
    cst = _consts()
    half = 32
    inv_freq = (np.float32(10000.0) ** (-np.arange(half, dtype=np.float32) * np.float32(2.0) / np.float32(64))).astype(np.float32)
    shared = dict(
        w_ada=f("w_ada")[0], b_ada=f("b_ada")[0], norm1_w=f("norm1_w")[0], norm2_w=f("norm2_w")[0], norm_f_w=f("norm_f_w"),
        w_in=f("w_in")[0],
        conv_w=np.ascontiguousarray(f("conv_w")[0].reshape(4, 8, 128).transpose(2, 1, 0)),
        att_sinks=f("att_sinks")[0], i_bias=f("i_bias")[0].reshape(4, 1), f_bias=f("f_bias")[0].reshape(4, 1),
        mlstm_norm_w=f("mlstm_norm_w")[0], w_att=f("w_att_branch")[0], w_ml=f("w_mlstm_branch")[0], w_out=f("w_out")[0],
        wq=f("peer_w_query")[0], subkT=np.ascontiguousarray(f("peer_sub_keys")[0].transpose(2, 0, 1)),
        pu=f("peer_u")[0], pv=f("peer_v")[0],
        ident=cst["ident"], tri=cst["tri"], sel=cst["sel"], i4=cst["i4"], iota16=cst["iota16"], maskN=cst["maskN"],
    )
    c = f("c")
    in_maps = []
    T = NOWN * 128
    for b in range(B):
        for j in range(ncore_per_seq):
            start = j * T
            xo = np.ascontiguousarray(x[b, start:start + T])
            NP1 = max(NPRE, 1)
            xp = np.zeros((NP1 * 128, D), np.float32)
            valid = np.zeros((128, NP1), np.float32)
            if NPRE > 0 and start > 0:
                xp[NPRE * 128 - start:] = x[b, :start]
                valid[:, NPRE - start // 128:] = 1.0
            pos = (np.arange(start - 128, start + T, dtype=np.float32))
            ang = (pos[:, None] * inv_freq[None, :]).astype(np.float32)
            m = dict(shared)
            m.update(xo=xo, xp=xp, valid=valid, mask0=(cst["mask0"] if j == 0 else cst["maskN"]),
                     cosd=np.cos(ang).astype(np.float32), sind=np.sin(ang).astype(np.float32),
                     cT=np.ascontiguousarray(c[b].reshape(8, 128).T))
            in_maps.append(m)
    n = len(in_maps)
    res = run_bass_kernel_spmd(nc, in_maps, core_ids=list(range(n)))
    out = np.zeros((B, S, D), np.float32)
    k = 0
    for b in range(B):
        for j in range(ncore_per_seq):
            out[b, j * T:(j + 1) * T] = res.results[k]["y"]
            k += 1
    return out


def kernel(**inputs):
    return run(inputs, SEQ_FULL, 4, 32)
```
